# Optimizing a Trainium2 kernel written in Bass

```python
import math
import jax, jax.numpy as jnp
from jax import lax
import numpy as np

D_MODEL = 1024
BATCH = 4
SEQ = 4096
DEPTH = 2

CHUNK = 64
Q_BLOCK = 128
NORM_EPS = 1e-6

DIFF_HEADS = 4
DIFF_HEAD_DIM = 64
DIFF_V_DIM = 2 * DIFF_HEAD_DIM
DIFF_WIDTH = DIFF_HEADS * DIFF_V_DIM
DIFF_QK_COLS = DIFF_HEADS * 2 * DIFF_HEAD_DIM
CHK_HEADS = 8
CHK_HEAD_DIM = 64
CHK_WIDTH = CHK_HEADS * CHK_HEAD_DIM
CHK_LEFT_CHUNKS = 8
CHK_BAND = (CHK_LEFT_CHUNKS + 1) * CHUNK
REL_CLIP = 256
MLA_HEADS = 8
MLA_Q_LORA = 384
MLA_KV_LORA = 256
MLA_NOPE = 64
MLA_ROPE = 32
MLA_V = 64
MLA_WIDTH = MLA_HEADS * MLA_V
ROPE_THETA = 10000.0
N_BRANCHES = 3
IN_SPLITS = (DIFF_QK_COLS, DIFF_QK_COLS, DIFF_WIDTH,
             CHK_WIDTH, CHK_WIDTH, CHK_WIDTH,
             MLA_Q_LORA, MLA_KV_LORA + MLA_ROPE,
             N_BRANCHES * D_MODEL)
IN_WIDTH = sum(IN_SPLITS)
N_EXPERTS = 16
N_GROUPS = 4
EXPERTS_PER_GROUP = N_EXPERTS // N_GROUPS
TOP_K = 2
MOE_D_FF = 512

kernel_name = "hybrid_chunk_causal_diff_chunk_mla_grouped_moe"


def rms_norm(x, g, eps=NORM_EPS):
    xf = x.astype(jnp.float32)
    y = xf * lax.rsqrt(jnp.mean(xf * xf, axis=-1, keepdims=True) + eps)
    return (y * g.astype(jnp.float32)).astype(x.dtype)


def apply_rope(x, cos, sin):
    xf = x.astype(jnp.float32)
    x1, x2 = jnp.split(xf, 2, axis=-1)
    return jnp.concatenate([x1 * cos - x2 * sin, x1 * sin + x2 * cos], axis=-1).astype(x.dtype)


def to_blocks(t):
    b, s = t.shape[:2]
    return jnp.moveaxis(t.reshape(b, s // Q_BLOCK, Q_BLOCK, *t.shape[2:]), 1, 0)


def from_blocks(t):
    nb, b, qb = t.shape[:3]
    return jnp.moveaxis(t, 0, 1).reshape(b, nb * qb, *t.shape[3:])


def chunk_causal_mask(blk, seq):
    q_pos = blk * Q_BLOCK + jnp.arange(Q_BLOCK)
    k_pos = jnp.arange(seq)
    allowed = (k_pos[None, :] // CHUNK) <= (q_pos[:, None] // CHUNK)
    return allowed, q_pos, k_pos


def diff_attention(q, k, v, lam, slopes):
    seq = q.shape[1]
    scale = DIFF_HEAD_DIM ** -0.5

    def one_block(args):
        qb, blk = args
        allowed, q_pos, k_pos = chunk_causal_mask(blk, seq)
        dist = jnp.abs(q_pos[:, None] - k_pos[None, :]).astype(jnp.float32)
        alibi = -slopes[:, None, None] * dist[None]
        s = jnp.einsum('bqhcd,bkhcd->bhcqk', qb, k).astype(jnp.float32) * scale
        s = jnp.where(allowed, s + alibi[None, :, None], -jnp.inf)
        p = jax.nn.softmax(s, axis=-1)
        a = p[:, :, 0] - lam * p[:, :, 1]
        return jnp.einsum('bhqk,bkhe->bqhe', a.astype(v.dtype), v)

    out = lax.map(one_block, (to_blocks(q), jnp.arange(seq // Q_BLOCK)))
    return from_blocks(out)


def chunked_band_attention(q, k, v, rel_table):
    b, seq, h, d = q.shape
    nc = seq // CHUNK
    qc = q.reshape(b, nc, CHUNK, h, d)
    pad = ((0, 0), (CHK_LEFT_CHUNKS, 0), (0, 0), (0, 0), (0, 0))
    kp = jnp.pad(k.reshape(b, nc, CHUNK, h, d), pad)
    vp = jnp.pad(v.reshape(b, nc, CHUNK, h, d), pad)
    band_idx = jnp.arange(nc)[:, None] + jnp.arange(CHK_LEFT_CHUNKS + 1)[None, :]
    kb = kp[:, band_idx].reshape(b, nc, CHK_BAND, h, d)
    vb = vp[:, band_idx].reshape(b, nc, CHK_BAND, h, d)
    q_i = jnp.arange(CHUNK)
    k_i = jnp.arange(CHK_BAND)
    rel = jnp.clip(q_i[:, None] + CHK_LEFT_CHUNKS * CHUNK - k_i[None, :], -REL_CLIP, REL_CLIP) + REL_CLIP
    bias = rel_table[:, rel].astype(jnp.float32)
    src_chunk = jnp.arange(nc)[:, None] - CHK_LEFT_CHUNKS + (k_i // CHUNK)[None, :]
    valid = src_chunk >= 0
    s = jnp.einsum('bnqhd,bnkhd->bhnqk', qc, kb).astype(jnp.float32) * (d ** -0.5)
    s = jnp.where(valid[None, None, :, None, :], s + bias[None, :, None], -jnp.inf)
    p = jax.nn.softmax(s, axis=-1)
    o = jnp.einsum('bhnqk,bnkhd->bnqhd', p.astype(v.dtype), vb)
    return o.reshape(b, seq, h, d)


def mla_attention(q_nope, q_rope, k_nope, k_rope, v):
    seq = q_nope.shape[1]
    scale = (MLA_NOPE + MLA_ROPE) ** -0.5

    def one_block(args):
        qn, qr, blk = args
        allowed, _, _ = chunk_causal_mask(blk, seq)
        s = (jnp.einsum('bqhd,bkhd->bhqk', qn, k_nope)
             + jnp.einsum('bqhr,bkr->bhqk', qr, k_rope)).astype(jnp.float32) * scale
        s = jnp.where(allowed, s, -jnp.inf)
        p = jax.nn.softmax(s, axis=-1)
        return jnp.einsum('bhqk,bkhd->bqhd', p.astype(v.dtype), v)

    out = lax.map(one_block, (to_blocks(q_nope), to_blocks(q_rope), jnp.arange(seq // Q_BLOCK)))
    return from_blocks(out)


def token_mixer(h, layer, cos, sin, w_in, lq1, lk1, lq2, lk2, subln_g, rel_table,
                q_norm_g, w_q_b, kv_norm_g, w_kv_b, w_br_diff, w_br_chunk, w_br_mla, w_out):
    b, seq, _ = h.shape
    f32 = jnp.float32
    split_points = np.cumsum(IN_SPLITS)[:-1].tolist()
    dq, dk, dv, cq, ck, cv, mq, mkv, gate_logits = jnp.split(h @ w_in, split_points, axis=-1)

    lam_init = 0.8 - 0.6 * math.exp(-0.3 * layer)
    lam = (jnp.exp(jnp.sum(lq1.astype(f32) * lk1.astype(f32)))
           - jnp.exp(jnp.sum(lq2.astype(f32) * lk2.astype(f32))) + lam_init)
    slopes = jnp.exp2(-8.0 / DIFF_HEADS * jnp.arange(1, DIFF_HEADS + 1, dtype=f32))
    y_a = diff_attention(dq.reshape(b, seq, DIFF_HEADS, 2, DIFF_HEAD_DIM),
                         dk.reshape(b, seq, DIFF_HEADS, 2, DIFF_HEAD_DIM),
                         dv.reshape(b, seq, DIFF_HEADS, DIFF_V_DIM), lam, slopes)
    y_a = (rms_norm(y_a, subln_g) * (1.0 - lam_init)).reshape(b, seq, DIFF_WIDTH)

    y_b = chunked_band_attention(cq.reshape(b, seq, CHK_HEADS, CHK_HEAD_DIM),
                                 ck.reshape(b, seq, CHK_HEADS, CHK_HEAD_DIM),
                                 cv.reshape(b, seq, CHK_HEADS, CHK_HEAD_DIM), rel_table)
    y_b = y_b.reshape(b, seq, CHK_WIDTH)

    q_full = (rms_norm(mq, q_norm_g) @ w_q_b).reshape(b, seq, MLA_HEADS, MLA_NOPE + MLA_ROPE)
    q_nope, q_rope = jnp.split(q_full, [MLA_NOPE], axis=-1)
    c_kv, k_rope = jnp.split(mkv, [MLA_KV_LORA], axis=-1)
    kv = (rms_norm(c_kv, kv_norm_g) @ w_kv_b).reshape(b, seq, MLA_HEADS, MLA_NOPE + MLA_V)
    k_nope, v_c = jnp.split(kv, [MLA_NOPE], axis=-1)
    q_rope = apply_rope(q_rope, cos[None, :, None, :], sin[None, :, None, :])
    k_rope = apply_rope(k_rope, cos[None], sin[None])
    y_c = mla_attention(q_nope, q_rope, k_nope, k_rope, v_c).reshape(b, seq, MLA_WIDTH)

    g = jax.nn.sigmoid(gate_logits.astype(f32)).astype(h.dtype).reshape(b, seq, N_BRANCHES, D_MODEL)
    merged = (g[:, :, 0] * (y_a @ w_br_diff) + g[:, :, 1] * (y_b @ w_br_chunk)
              + g[:, :, 2] * (y_c @ w_br_mla))
    return merged @ w_out


def grouped_moe(h, w_router, router_bias, w_gate, w_up, w_down):
    b, seq, d = h.shape
    t = h.reshape(-1, d)
    scores = jax.nn.sigmoid((t @ w_router).astype(jnp.float32))
    sel = scores + router_bias.astype(jnp.float32)
    grouped = sel.reshape(-1, N_GROUPS, EXPERTS_PER_GROUP)
    group_score = lax.top_k(grouped, TOP_K)[0].sum(-1)
    best_group = jnp.argmax(group_score, axis=-1)
    in_group = (jnp.arange(N_EXPERTS) // EXPERTS_PER_GROUP)[None, :] == best_group[:, None]
    _, idx = lax.top_k(jnp.where(in_group, sel, -jnp.inf), TOP_K)
    w = jnp.take_along_axis(scores, idx, axis=-1)
    w = w / jnp.sum(w, axis=-1, keepdims=True)
    combine = jnp.sum(jax.nn.one_hot(idx, N_EXPERTS, dtype=jnp.float32) * w[..., None], axis=1)
    combine = combine.astype(t.dtype)
    out = jnp.zeros(t.shape, t.dtype)
    for e in range(N_EXPERTS):
        he = jax.nn.silu(t @ w_gate[e]) * (t @ w_up[e])
        out = out + combine[:, e:e + 1] * (he @ w_down[e])
    return out.reshape(b, seq, d)


def setup_inputs(seed: int = 0) -> dict:
    key = jax.random.key(seed)
    keys = iter(jax.random.split(key, 40))
    f32 = jnp.float32

    def nrm(shape, scale):
        return scale * jax.random.normal(next(keys), shape, f32)

    def gain(shape):
        return 1.0 + nrm(shape, 0.05)

    L = DEPTH
    return {
        "x": nrm((BATCH, SEQ, D_MODEL), 1.0),
        "c": nrm((BATCH, D_MODEL), 1.0),
        "w_mod": nrm((L, D_MODEL, 6 * D_MODEL), 0.5 * D_MODEL ** -0.5),
        "b_mod": nrm((L, 6 * D_MODEL), 0.02),
        "g_norm_mix": gain((L, D_MODEL)),
        "g_norm_ffn": gain((L, D_MODEL)),
        "w_in": nrm((L, D_MODEL, IN_WIDTH), D_MODEL ** -0.5),
        "diff_lambda_q1": nrm((L, DIFF_HEAD_DIM), 0.1),
        "diff_lambda_k1": nrm((L, DIFF_HEAD_DIM), 0.1),
        "diff_lambda_q2": nrm((L, DIFF_HEAD_DIM), 0.1),
        "diff_lambda_k2": nrm((L, DIFF_HEAD_DIM), 0.1),
        "diff_subln_g": gain((L, DIFF_V_DIM)),
        "chunk_rel_bias": nrm((L, CHK_HEADS, 2 * REL_CLIP + 1), 0.2),
        "mla_q_norm_g": gain((L, MLA_Q_LORA)),
        "mla_w_q_b": nrm((L, MLA_Q_LORA, MLA_HEADS * (MLA_NOPE + MLA_ROPE)), MLA_Q_LORA ** -0.5),
        "mla_kv_norm_g": gain((L, MLA_KV_LORA)),
        "mla_w_kv_b": nrm((L, MLA_KV_LORA, MLA_HEADS * (MLA_NOPE + MLA_V)), MLA_KV_LORA ** -0.5),
        "w_branch_diff": nrm((L, DIFF_WIDTH, D_MODEL), DIFF_WIDTH ** -0.5),
        "w_branch_chunk": nrm((L, CHK_WIDTH, D_MODEL), CHK_WIDTH ** -0.5),
        "w_branch_mla": nrm((L, MLA_WIDTH, D_MODEL), MLA_WIDTH ** -0.5),
        "w_out": nrm((L, D_MODEL, D_MODEL), D_MODEL ** -0.5),
        "w_router": nrm((D_MODEL, N_EXPERTS), D_MODEL ** -0.5),
        "router_bias": nrm((N_EXPERTS,), 0.01),
        "w_exp_gate": nrm((L, N_EXPERTS, D_MODEL, MOE_D_FF), D_MODEL ** -0.5),
        "w_exp_up": nrm((L, N_EXPERTS, D_MODEL, MOE_D_FF), D_MODEL ** -0.5),
        "w_exp_down": nrm((L, N_EXPERTS, MOE_D_FF, D_MODEL), MOE_D_FF ** -0.5),
        "g_final": gain((D_MODEL,)),
    }


def reference(x, c, w_mod, b_mod, g_norm_mix, g_norm_ffn, w_in,
              diff_lambda_q1, diff_lambda_k1, diff_lambda_q2, diff_lambda_k2, diff_subln_g,
              chunk_rel_bias, mla_q_norm_g, mla_w_q_b, mla_kv_norm_g, mla_w_kv_b,
              w_branch_diff, w_branch_chunk, w_branch_mla, w_out,
              w_router, router_bias, w_exp_gate, w_exp_up, w_exp_down, g_final):
    seq = x.shape[1]
    pos = jnp.arange(seq, dtype=jnp.float32)
    inv_freq = 1.0 / (ROPE_THETA ** (jnp.arange(0, MLA_ROPE, 2, dtype=jnp.float32) / MLA_ROPE))
    ang = pos[:, None] * inv_freq[None, :]
    cos, sin = jnp.cos(ang), jnp.sin(ang)
    c_act = jax.nn.silu(c)
    for l in range(DEPTH):
        mod = c_act @ w_mod[l] + b_mod[l]
        sh1, sc1, gt1, sh2, sc2, gt2 = jnp.split(mod[:, None, :], 6, axis=-1)
        h = rms_norm(x, g_norm_mix[l]) * (1 + sc1) + sh1
        y = token_mixer(h, l, cos, sin, w_in[l],
                        diff_lambda_q1[l], diff_lambda_k1[l], diff_lambda_q2[l], diff_lambda_k2[l],
                        diff_subln_g[l], chunk_rel_bias[l],
                        mla_q_norm_g[l], mla_w_q_b[l], mla_kv_norm_g[l], mla_w_kv_b[l],
                        w_branch_diff[l], w_branch_chunk[l], w_branch_mla[l], w_out[l])
        x = x + gt1 * y
        h = rms_norm(x, g_norm_ffn[l]) * (1 + sc2) + sh2
        x = x + gt2 * grouped_moe(h, w_router, router_bias, w_exp_gate[l], w_exp_up[l], w_exp_down[l])
    return rms_norm(x, g_final)
```

```python
import math
import numpy as np
import ml_dtypes
from contextlib import ExitStack
import concourse.bass as bass
import concourse.mybir as mybir
from concourse.bass_utils import run_bass_kernel_spmd

F32 = mybir.dt.float32
BF16 = mybir.dt.bfloat16
AF = mybir.ActivationFunctionType
ALU = mybir.AluOpType
AX = mybir.AxisListType

T = 4096
D = 1024
L = 2
NT = T // 128
NB = T // 512
EPS = 1e-6
INW = 6816
BIG = 30000.0
N1_LIMIT = NB


class Buf:
    __slots__ = ("t", "w", "r", "name")

    def __init__(self, t, name=""):
        self.t = t
        self.w = None
        self.r = {}
        self.name = name

    def __getitem__(self, k):
        return self.t[k]


class Sched:
    ENG = ("pe", "act", "dve", "pool", "sp")
    SAME_ENGINE_SYNC = True
    NO_SELF_SYNC = ("pe",)

    def __init__(self, nc):
        self.nc = nc
        self.prog = {e: [] for e in self.ENG}
        self.seq = {e: 0 for e in self.ENG}
        self.last = {e: 0 for e in self.ENG}
        self.dma_cnt = {}
        self.waited = {e: {} for e in self.ENG}
        self.fence = {e: {} for e in self.ENG}
        self.needed = {e: set() for e in self.ENG}

    def _deps(self, eng, reads, writes):
        d = dict(self.fence[eng])
        self.fence[eng] = {}

        def add(tok):
            k, v = tok
            if d.get(k, 0) < v:
                d[k] = v
        for b in reads:
            if b.w is not None:
                add(b.w)
        for b in writes:
            if b.w is not None:
                add(b.w)
            for k, v in b.r.items():
                add((k, v))
        out = []
        for k, v in d.items():
            if k == eng and (eng in self.NO_SELF_SYNC or not self.SAME_ENGINE_SYNC):
                continue
            if self.waited[eng].get(k, 0) >= v:
                continue
            self.waited[eng][k] = v
            out.append((k, v))
            if k in self.needed:
                self.needed[k].add(v)
        return out

    def _mark(self, tok, reads, writes):
        k, v = tok
        for b in reads:
            if b.r.get(k, 0) < v:
                b.r[k] = v
        for b in writes:
            b.w = tok
            b.r = {}

    def op(self, eng, fn, reads=(), writes=()):
        waits = self._deps(eng, reads, writes)
        self.seq[eng] += 1
        tok = (eng, self.seq[eng])
        self.last[eng] = self.seq[eng]
        self.prog[eng].append((waits, fn, tok, None))
        self._mark(tok, reads, writes)

    def dma(self, q, key, fn, reads=(), writes=()):
        waits = self._deps(q, reads, writes)
        n = self.dma_cnt.get(key, 0) + 1
        self.dma_cnt[key] = n
        tok = (key, n)
        self.prog[q].append((waits, fn, None, key))
        self._mark(tok, reads, writes)

    def barrier(self):
        toks = {}
        for e in self.ENG:
            if self.last[e] > 0:
                toks[e] = self.last[e]
        for k, n in self.dma_cnt.items():
            toks[k] = n
        for e in self.ENG:
            f = self.fence[e]
            for k, v in toks.items():
                if f.get(k, 0) < v:
                    f[k] = v

    def emit(self):
        nc = self.nc
        self.barrier()
        for e in self.ENG:
            waits = self._deps(e, (), ())
            if waits:
                self.prog[e].append((waits, None, None, None))
        sems = {}
        handles = []
        for e in self.ENG:
            _UID[0] += 1
            sems[e] = nc.alloc_semaphore(name="s%d_%s" % (_UID[0], e))
            handles.append(sems[e])
        for i, k in enumerate(self.dma_cnt):
            _UID[0] += 1
            sems[k] = nc.alloc_semaphore(name="d%d_%d" % (_UID[0], i))
            handles.append(sems[k])
        rank = {e: {v: i + 1 for i, v in enumerate(sorted(self.needed[e]))}
                for e in self.ENG}
        ENGSET = set(self.ENG)

        def run(ename, e):
            rk = rank[ename]
            for (waits, fn, tok, key) in self.prog[ename]:
                for (k, v) in waits:
                    e.wait_ge(sems[k], rank[k][v] if k in ENGSET else 16 * v)
                if fn is None:
                    continue
                ins = fn(e)
                if key is not None:
                    ins.then_inc(sems[key], 16)
                elif tok[1] in rk:
                    ins.then_inc(sems[ename], 1)

        for h in handles:
            nc.gpsimd.sem_clear(h)
        nc.all_engine_barrier()
        with nc.Block() as block:
            block.tensor(lambda e: run("pe", e))
            block.scalar(lambda e: run("act", e))
            block.vector(lambda e: run("dve", e))
            block.gpsimd(lambda e: run("pool", e))
            block.sync(lambda e: run("sp", e))
        nc.clear_and_free_semaphores(handles)
        nc.all_engine_barrier()


class Ring:
    def __init__(self, bufs):
        self.bufs = bufs
        self.i = 0

    def next(self):
        b = self.bufs[self.i % len(self.bufs)]
        self.i += 1
        return b


_UID = [0]


class Phase:
    def __init__(self, nc):
        self.nc = nc
        self.es = ExitStack()
        self.S = Sched(nc)
        self.n = 0

    def sb(self, shape, dt, name=None):
        _UID[0] += 1
        name = "%s_%d" % (name or "t", _UID[0])
        return Buf(self.es.enter_context(self.nc.sbuf_tensor(name, shape, dt)), name)

    def ps(self, shape, dt, name=None):
        _UID[0] += 1
        name = "%s_%d" % (name or "p", _UID[0])
        return Buf(self.es.enter_context(self.nc.psum_tensor(name, shape, dt)), name)

    def ring(self, n, shape, dt, name, psum=False):
        return Ring([(self.ps if psum else self.sb)(shape, dt, name) for _ in range(n)])

    def close(self):
        self.S.emit()
        self.es.close()


def build_program(stop=None, debug=False):
    nc = bass.Bass("TRN2", target_bir_lowering=False)

    def din(name, shape, dt=F32):
        return nc.dram_tensor(name, list(shape), dt, kind="ExternalInput").ap()

    def dscr(name, shape, dt=BF16):
        if debug:
            return nc.dram_tensor(name, list(shape), dt, kind="ExternalOutput").ap()
        return nc.dram_tensor(name, list(shape), dt).ap()

    I = {}
    I["x"] = din("x", [T, D])
    I["c"] = din("c", [128, 8])
    I["w_mod"] = din("w_mod", [L, D, 6 * D])
    I["b_mod"] = din("b_mod", [L, 6 * D])
    I["g_norm_mix"] = din("g_norm_mix", [L, D])
    I["g_norm_ffn"] = din("g_norm_ffn", [L, D])
    I["w_in"] = din("w_in", [L, D, INW])
    for nm in ("diff_lambda_q1", "diff_lambda_k1", "diff_lambda_q2", "diff_lambda_k2"):
        I[nm] = din(nm, [L, 64])
    I["diff_subln_g"] = din("diff_subln_g", [L, 128])
    I["chunk_rel_bias"] = din("chunk_rel_bias", [L, 8, 513])
    I["mla_q_norm_g"] = din("mla_q_norm_g", [L, 384])
    I["mla_w_q_b"] = din("mla_w_q_b", [L, 384, 768])
    I["mla_kv_norm_g"] = din("mla_kv_norm_g", [L, 256])
    I["mla_w_kv_b"] = din("mla_w_kv_b", [L, 256, 1024])
    I["w_branch_diff"] = din("w_branch_diff", [L, 512, D])
    I["w_branch_chunk"] = din("w_branch_chunk", [L, 512, D])
    I["w_branch_mla"] = din("w_branch_mla", [L, 512, D])
    I["w_out"] = din("w_out", [L, D, D])
    I["w_router"] = din("w_router", [D, 16])
    I["router_bias"] = din("router_bias", [1, 16])
    I["w_exp_gate"] = din("w_exp_gate", [L, 16, D, 512])
    I["w_exp_up"] = din("w_exp_up", [L, 16, D, 512])
    I["w_exp_down"] = din("w_exp_down", [L, 16 * 512, D])
    I["g_final"] = din("g_final", [1, D])
    C_ident = din("k_ident", [128, 128], BF16)
    C_identf = din("k_identf", [128, 128], F32)
    C_anti = din("k_anti", [128, 128], BF16)
    C_sel = din("k_sel", [16, 16 * 128], F32)
    C_cos = din("k_cos", [128, T], F32)
    C_sin = din("k_sin", [128, T], F32)
    C_augq = din("k_augq", [4, 4, T], BF16)
    C_augk = din("k_augk", [4, 4, T], BF16)
    C_corr = din("k_corr", [4, 128, 64], F32)
    out_d = nc.dram_tensor("out", [T, D], F32, kind="ExternalOutput").ap()

    xs = dscr("xs", [T, D], F32)
    modB_d = dscr("modB", [L, 128, 6 * D], F32)
    hT_d = dscr("hT", [8, 128, T])
    qA = dscr("qA", [8, 64, T]); kA = dscr("kA", [8, 64, T]); vA = dscr("vA", [T, 512])
    qB = dscr("qB", [8, 64, T]); kB = dscr("kB", [8, 64, T]); vB = dscr("vB", [T, 512])
    qC = dscr("qC", [8, 96, T]); kC = dscr("kC", [8, 96, T]); vC = dscr("vC", [T, 512])
    yT_d = dscr("yT", [12, 128, T])
    h2T_d = dscr("h2T", [8, 128, T])
    combT_d = dscr("combT", [16, T], F32)
    HE_d = dscr("HE", [NT, 128, 64, 128])
    E_d = dscr("Ebias", [8, 768], F32)
    lam_d = dscr("lam", [1, 4], F32)

    def rstd_from_ssq(S, P, st, n, lnexp=False):
        S.op("dve", lambda e: e.tensor_scalar(out=st[:, 1:2], in0=st[:, 0:1], scalar1=1.0 / n, scalar2=EPS,
                                              op0=ALU.mult, op1=ALU.add), reads=[st], writes=[st])
        if lnexp:
            S.op("act", lambda e: e.activation(out=st[:, 1:2], in_=st[:, 1:2], func=AF.Ln), reads=[st], writes=[st])
            S.op("act", lambda e: e.activation(out=st[:, 1:2], in_=st[:, 1:2], func=AF.Exp, scale=-0.5), reads=[st], writes=[st])
            return
        S.op("act", lambda e: e.activation(out=st[:, 1:2], in_=st[:, 1:2], func=AF.Sqrt), reads=[st], writes=[st])
        S.op("dve", lambda e: e.reciprocal(out=st[:, 1:2], in_=st[:, 1:2]), reads=[st], writes=[st])

    def norm_tile(S, xt, AB, BB, junk, st, tmp):
        S.op("act", lambda e: e.activation(out=junk[:], in_=xt[:], func=AF.Square, accum_out=st[:, 0:1]),
             reads=[xt], writes=[junk, st])
        rstd_from_ssq(S, None, st, D)
        S.op("dve", lambda e: e.scalar_tensor_tensor(out=tmp[:], in0=xt[:], scalar=st[:, 1:2], in1=AB[:],
                                                     op0=ALU.mult, op1=ALU.mult), reads=[xt, st, AB], writes=[tmp])

    def phase_mod(l):
        P = Phase(nc); S = P.S
        cl = P.sb([128, 8], F32, "cl"); cact = P.sb([128, 8], F32, "cact")
        cB = P.sb([128, 8, 128], F32, "cB")
        wr = P.ring(3, [128, 8, 512], F32, "wmod")
        bb = P.ring(2, [128, 512], F32, "bb")
        gb = P.ring(2, [128, 512], F32, "gb")
        ob = P.ring(2, [128, 512], F32, "ob")
        pp = P.ring(2, [128, 512], F32, "pm", psum=True)
        dmod = Buf(modB_d, "modB_d")
        S.dma("sp", "cl", lambda e: e.dma_start(out=cl[:], in_=I["c"]), writes=[cl])
        S.op("act", lambda e: e.activation(out=cact[:], in_=cl[:], func=AF.Silu), reads=[cl], writes=[cact])
        for k in range(8):
            S.op("dve", lambda e, k=k: e.tensor_copy(out=cB[:, k, :], in_=cact[:, k:k + 1].to_broadcast([128, 128])),
                 reads=[cact], writes=[cB])
        for j in range(12):
            w = wr.next(); b = bb.next(); o = ob.next(); p = pp.next()
            for hk in range(2):
                S.dma("sp" if hk == 0 else "act", w.name, lambda e, w=w, j=j, hk=hk: e.dma_start(
                    out=w[:, hk * 4:(hk + 1) * 4, :], in_=I["w_mod"][l][hk * 512:(hk + 1) * 512, j * 512:(j + 1) * 512].rearrange("(k p) n -> p k n", p=128)), writes=[w])
            S.dma("sp", b.name, lambda e, b=b, j=j: e.dma_start(
                out=b[:], in_=I["b_mod"][l, j * 512:(j + 1) * 512].partition_broadcast(128)), writes=[b])
            for k in range(8):
                S.op("pe", lambda e, w=w, p=p, k=k: e.matmul(p[:], lhsT=cB[:, k, :], rhs=w[:, k, :],
                                                               start=(k == 0), stop=(k == 7)), reads=[cB, w], writes=[p])
            S.op("dve", lambda e, o=o, p=p, b=b: e.tensor_tensor(out=o[:], in0=p[:], in1=b[:], op=ALU.add),
                 reads=[p, b], writes=[o])
            if j // 2 in (1, 4):
                g = gb.next()
                gsrc = I["g_norm_mix"] if j // 2 == 1 else I["g_norm_ffn"]
                c0 = (j % 2) * 512
                S.dma("sp", g.name, lambda e, g=g, gsrc=gsrc, c0=c0: e.dma_start(
                    out=g[:], in_=gsrc[l, c0:c0 + 512].partition_broadcast(128)), writes=[g])
                S.op("dve", lambda e, o=o, g=g: e.scalar_tensor_tensor(out=o[:], in0=o[:], scalar=1.0, in1=g[:],
                                                                        op0=ALU.add, op1=ALU.mult), reads=[o, g], writes=[o])
            S.dma("sp", "st" + o.name, lambda e, o=o, j=j: e.dma_start(out=modB_d[l][:, j * 512:(j + 1) * 512], in_=o[:]),
                  reads=[o], writes=[dmod])
        P.close()

    def phase_lam(l):
        P = Phase(nc); S = P.S
        a = P.sb([1, 4, 64], F32, "lama"); pr = P.sb([1, 2, 64], F32, "lampr"); s2 = P.sb([1, 4], F32, "lams")
        for i, nm in enumerate(("diff_lambda_q1", "diff_lambda_k1", "diff_lambda_q2", "diff_lambda_k2")):
            S.dma("sp", "la%d" % i, lambda e, i=i, nm=nm: e.dma_start(out=a[:, i, :], in_=I[nm][l:l + 1, :]), writes=[a])
        S.op("dve", lambda e: e.tensor_tensor(out=pr[:, 0, :], in0=a[:, 0, :], in1=a[:, 1, :], op=ALU.mult), reads=[a], writes=[pr])
        S.op("dve", lambda e: e.tensor_tensor(out=pr[:, 1, :], in0=a[:, 2, :], in1=a[:, 3, :], op=ALU.mult), reads=[a, pr], writes=[pr])
        S.op("dve", lambda e: e.tensor_reduce(out=s2[:, 0:2], in_=pr[:], axis=AX.X, op=ALU.add), reads=[pr], writes=[s2])
        S.op("act", lambda e: e.activation(out=s2[:, 0:2], in_=s2[:, 0:2], func=AF.Exp), reads=[s2], writes=[s2])
        lam_init = 0.8 - 0.6 * math.exp(-0.3 * l)
        S.op("dve", lambda e: e.tensor_tensor(out=s2[:, 2:3], in0=s2[:, 0:1], in1=s2[:, 1:2], op=ALU.subtract), reads=[s2], writes=[s2])
        S.op("dve", lambda e: e.tensor_scalar(out=s2[:, 3:4], in0=s2[:, 2:3], scalar1=lam_init, scalar2=-1.0,
                                              op0=ALU.add, op1=ALU.mult), reads=[s2], writes=[s2])
        dl = Buf(lam_d, "lam_d")
        S.dma("sp", "lst", lambda e: e.dma_start(out=lam_d[0:1, l:l + 1], in_=s2[:, 3:4]), reads=[s2], writes=[dl])
        P.close()

    def transpose_store(S, P, hb, pT, hst, col, ident):
        for k in range(8):
            S.op("pe", lambda e, k=k: e.transpose(pT[:, k * 128:(k + 1) * 128], hb[:, k * 128:(k + 1) * 128], ident[:]),
                 reads=[hb, ident], writes=[pT])
        S.op("act", lambda e: e.activation(out=hst[:, :, col:col + 128],
                                           in_=pT[:].rearrange("p (k t) -> p k t", k=8), func=AF.Copy),
             reads=[pT], writes=[hst])

    def phase_n1(l, src):
        P = Phase(nc); S = P.S
        ident = P.sb([128, 128], BF16, "ident")
        AB = P.sb([128, D], F32, "AB"); BB = P.sb([128, D], F32, "BB")
        xr = P.ring(2, [128, D], F32, "x")
        junk = P.sb([128, D], BF16, "junk"); st = P.ring(2, [128, 2], F32, "st")
        tmp = P.ring(2, [128, D], F32, "tmp"); hbr = P.ring(2, [128, D], BF16, "hb")
        pT = P.ring(2, [128, 1024], BF16, "pT", psum=True)
        hst = P.ring(2, [128, 8, 512], BF16, "hst")
        dh = Buf(hT_d, "hT_d")
        S.dma("sp", "id", lambda e: e.dma_start(out=ident[:], in_=C_ident), writes=[ident])
        S.dma("sp", "AB", lambda e: e.dma_start(out=AB[:], in_=modB_d[l][:, 1024:2048]), writes=[AB])
        S.dma("sp", "BB", lambda e: e.dma_start(out=BB[:], in_=modB_d[l][:, 0:1024]), writes=[BB])
        for tb in range(N1_LIMIT):
            hs = hst.next()
            for tt in range(4):
                t = tb * 4 + tt
                xt = xr.next(); s_ = st.next(); tm = tmp.next(); hb = hbr.next(); p = pT.next()
                S.dma("sp", xt.name, lambda e, xt=xt, t=t: e.dma_start(out=xt[:], in_=src[t * 128:(t + 1) * 128, :]), writes=[xt])
                norm_tile(S, xt, AB, BB, junk, s_, tm)
                S.op("pool", lambda e, hb=hb, tm=tm: e.tensor_tensor(out=hb[:], in0=tm[:], in1=BB[:], op=ALU.add),
                     reads=[tm, BB], writes=[hb])
                transpose_store(S, P, hb, p, hs, tt * 128, ident)
            S.dma("sp", "st" + hs.name, lambda e, hs=hs, tb=tb: e.dma_start(
                out=hT_d[:, :, tb * 512:(tb + 1) * 512].rearrange("k p t -> p k t"), in_=hs[:]), reads=[hs], writes=[dh])
        P.close()

    def phase_prj(l):
        P = Phase(nc); S = P.S
        win = I["w_in"][l]
        hT = P.sb([128, 8, T], BF16, "hT")
        for k in range(8):
            S.dma("sp", "hT%d" % k, lambda e, k=k: e.dma_start(out=hT[:, k, :], in_=hT_d[k]), writes=[hT])
        wr = P.ring(2, [128, 8, 512], BF16, "w")
        pp = P.ring(4, [128, 512], F32, "pp", psum=True)
        stg = P.ring(2, [128, T], BF16, "stg")
        stv = P.ring(2, [128, 4, 512], BF16, "stv")
        evq = [0]

        def evac(dst_ap, p, dstbuf):
            evq[0] += 1
            if evq[0] % 2:
                S.op("act", lambda e: e.activation(out=dst_ap, in_=p[:], func=AF.Copy), reads=[p], writes=[dstbuf])
            else:
                S.op("dve", lambda e: e.tensor_copy(out=dst_ap, in_=p[:]), reads=[p], writes=[dstbuf])

        def load_w(c0, n):
            w = wr.next()
            S.dma("pool", w.name, lambda e: e.dma_start(
                out=w[:, :, 0:n], in_=win[:, c0:c0 + n].rearrange("(k p) n -> p k n", p=128)), writes=[w])
            return w

        def fm_group(w, wc, dsts):
            sg = stg.next()
            for tb in range(NB):
                p = pp.next()
                for k in range(8):
                    S.op("pe", lambda e, p=p, k=k, tb=tb: e.matmul(p[:], lhsT=w[:, k, wc:wc + 128],
                                                                   rhs=hT[:, k, tb * 512:(tb + 1) * 512],
                                                                   start=(k == 0), stop=(k == 7)), reads=[w, hT], writes=[p])
                evac(sg[:, tb * 512:(tb + 1) * 512], p, sg)
            for i, (dap, r0, nr) in enumerate(dsts):
                S.dma("sp", "st%s_%d" % (sg.name, i), lambda e, dap=dap, r0=r0, nr=nr: e.dma_start(out=dap, in_=sg[r0:r0 + nr, :]),
                      reads=[sg])

        def tm_group(w, dst, hsrc, nk):
            for t4 in range(NT // 4):
                sv = stv.next()
                for tt in range(4):
                    t = t4 * 4 + tt
                    p = pp.next()
                    for k in range(nk):
                        S.op("pe", lambda e, p=p, k=k, t=t: e.matmul(p[:], lhsT=hsrc[:, k, t * 128:(t + 1) * 128], rhs=w[:, k, 0:512],
                                                                     start=(k == 0), stop=(k == nk - 1)), reads=[w, hsrc], writes=[p])
                    evac(sv[:, tt, :], p, sv)
                S.dma("sp", "st" + sv.name, lambda e, sv=sv, t4=t4: e.dma_start(
                    out=dst[t4 * 512:(t4 + 1) * 512, :].rearrange("(a p) n -> p a n", p=128), in_=sv[:]), reads=[sv])

        for base, dst in ((0, qA), (512, kA)):
            w = load_w(base, 512)
            for g in range(4):
                fm_group(w, g * 128, [(dst[2 * g], 0, 64), (dst[2 * g + 1], 64, 64)])
        w = load_w(1024, 512); tm_group(w, vA, hT, 8)
        for base, dst in ((1536, qB), (2048, kB)):
            w = load_w(base, 512)
            for g in range(4):
                fm_group(w, g * 128, [(dst[2 * g], 0, 64), (dst[2 * g + 1], 64, 64)])
        w = load_w(2560, 512); tm_group(w, vB, hT, 8)

        onesf = P.sb([128, 128], F32, "onesf")
        S.op("pool", lambda e: e.memset(onesf[:], 1.0), writes=[onesf])
        mqn = P.sb([128, 3, T], BF16, "mqn"); ckvn = P.sb([128, 2, T], BF16, "ckvn")
        gq = P.sb([128, 3], F32, "gq"); gkv = P.sb([128, 2], F32, "gkv")
        S.dma("sp", "gq", lambda e: e.dma_start(out=gq[:], in_=I["mla_q_norm_g"][l].rearrange("(c p) -> p c", p=128),
                                                allow_slow_non_contiguous=True), writes=[gq])
        S.dma("sp", "gkv", lambda e: e.dma_start(out=gkv[:], in_=I["mla_kv_norm_g"][l].rearrange("(c p) -> p c", p=128),
                                                 allow_slow_non_contiguous=True), writes=[gkv])
        latf = P.ring(1, [128, 3, 512], F32, "latf"); latsq = P.ring(1, [128, 3, 512], F32, "latsq")
        rsB = P.ring(2, [128, 512], F32, "rsB")

        def latent(c0, nch, gvec, dstn):
            w = load_w(c0, nch * 128)
            for tb in range(NB):
                lf = latf.next(); lq = latsq.next(); rs = rsB.next()
                for c in range(nch):
                    p = pp.next()
                    for k in range(8):
                        S.op("pe", lambda e, p=p, k=k, c=c, tb=tb: e.matmul(p[:], lhsT=w[:, k, c * 128:(c + 1) * 128],
                                                                            rhs=hT[:, k, tb * 512:(tb + 1) * 512],
                                                                            start=(k == 0), stop=(k == 7)), reads=[w, hT], writes=[p])
                    S.op("act", lambda e, p=p, c=c, lf=lf: e.activation(out=lf[:, c, :], in_=p[:], func=AF.Copy), reads=[p], writes=[lf])
                    S.op("dve", lambda e, c=c, lf=lf, lq=lq: e.tensor_tensor(out=lq[:, c, :], in0=lf[:, c, :], in1=lf[:, c, :], op=ALU.mult),
                         reads=[lf], writes=[lq])
                p = pp.next()
                for c in range(nch):
                    S.op("pe", lambda e, p=p, c=c, lq=lq: e.matmul(p[:], lhsT=onesf[:], rhs=lq[:, c, :], start=(c == 0), stop=(c == nch - 1)),
                         reads=[onesf, lq], writes=[p])
                S.op("dve", lambda e, p=p, rs=rs: e.tensor_scalar(out=rs[:], in0=p[:], scalar1=1.0 / (nch * 128), scalar2=EPS,
                                                                  op0=ALU.mult, op1=ALU.add), reads=[p], writes=[rs])
                S.op("act", lambda e, rs=rs: e.activation(out=rs[:], in_=rs[:], func=AF.Sqrt), reads=[rs], writes=[rs])
                S.op("dve", lambda e, rs=rs: e.reciprocal(out=rs[:], in_=rs[:]), reads=[rs], writes=[rs])
                for c in range(nch):
                    S.op("dve", lambda e, c=c, lf=lf, rs=rs, tb=tb: e.scalar_tensor_tensor(
                        out=dstn[:, c, tb * 512:(tb + 1) * 512], in0=lf[:, c, :], scalar=gvec[:, c:c + 1], in1=rs[:],
                        op0=ALU.mult, op1=ALU.mult), reads=[lf, rs, gvec], writes=[dstn])

        latent(3072, 3, gq, mqn)
        latent(3456, 2, gkv, ckvn)

        cosr = P.ring(2, [128, 512], F32, "cos"); sinr = P.ring(2, [128, 512], F32, "sin")
        wkr = P.sb([128, 8, 64], BF16, "wkr")
        kr0 = 3456 + 256
        for (d0, s0, n) in ((0, kr0, 32), (32, kr0 + 16, 16), (48, kr0, 16)):
            S.dma("pool", "wkr%d" % d0, lambda e, d0=d0, s0=s0, n=n: e.dma_start(
                out=wkr[:, :, d0:d0 + n], in_=win[:, s0:s0 + n].rearrange("(k p) n -> p k n", p=128)), writes=[wkr])
        wqb = I["mla_w_q_b"][l]
        wkvb = I["mla_w_kv_b"][l]
        wqn = P.sb([128, 3, 512], BF16, "wqn"); wqr = P.sb([128, 3, 256], BF16, "wqr"); wqs = P.sb([128, 3, 256], BF16, "wqs")
        wkn = P.sb([128, 2, 512], BF16, "wkn"); wvc = P.sb([128, 2, 512], BF16, "wvc")

        def wsrc(ap2, c0, n):
            return ap2[:, c0:c0 + n].rearrange("(k p) n -> p k n", p=128)
        for h in range(8):
            S.dma("pool", "wqn", lambda e, h=h: e.dma_start(out=wqn[:, :, h * 64:(h + 1) * 64], in_=wsrc(wqb, h * 96, 64)), writes=[wqn])
            S.dma("pool", "wqr", lambda e, h=h: e.dma_start(out=wqr[:, :, h * 32:(h + 1) * 32], in_=wsrc(wqb, h * 96 + 64, 32)), writes=[wqr])
            S.dma("pool", "wqs", lambda e, h=h: e.dma_start(out=wqs[:, :, h * 32:h * 32 + 16], in_=wsrc(wqb, h * 96 + 80, 16)), writes=[wqs])
            S.dma("pool", "wqs", lambda e, h=h: e.dma_start(out=wqs[:, :, h * 32 + 16:h * 32 + 32], in_=wsrc(wqb, h * 96 + 64, 16)), writes=[wqs])
            S.dma("pool", "wkn", lambda e, h=h: e.dma_start(out=wkn[:, :, h * 64:(h + 1) * 64], in_=wsrc(wkvb, h * 128, 64)), writes=[wkn])
            S.dma("pool", "wvc", lambda e, h=h: e.dma_start(out=wvc[:, :, h * 64:(h + 1) * 64], in_=wsrc(wkvb, h * 128 + 64, 64)), writes=[wvc])

        t1r = P.ring(2, [128, 512], F32, "t1"); t2r = P.ring(2, [128, 512], F32, "t2")

        def rope_group(lhs_r, lhs_s, src, nk, nrows, sg):
            for tb in range(NB):
                co = cosr.next(); si = sinr.next()
                S.dma("sp", co.name, lambda e, co=co, tb=tb: e.dma_start(out=co[:], in_=C_cos[:, tb * 512:(tb + 1) * 512]), writes=[co])
                S.dma("sp", si.name, lambda e, si=si, tb=tb: e.dma_start(out=si[:], in_=C_sin[:, tb * 512:(tb + 1) * 512]), writes=[si])
                pr = pp.next(); ps_ = pp.next()
                for k in range(nk):
                    S.op("pe", lambda e, k=k, tb=tb, pr=pr: e.matmul(pr[0:nrows, :], lhsT=lhs_r(k), rhs=src[:, k, tb * 512:(tb + 1) * 512],
                                                                     start=(k == 0), stop=(k == nk - 1)), reads=[src, wkr, wqr], writes=[pr])
                for k in range(nk):
                    S.op("pe", lambda e, k=k, tb=tb, ps_=ps_: e.matmul(ps_[0:nrows, :], lhsT=lhs_s(k), rhs=src[:, k, tb * 512:(tb + 1) * 512],
                                                                       start=(k == 0), stop=(k == nk - 1)), reads=[src, wkr, wqs], writes=[ps_])
                t1 = t1r.next(); t2 = t2r.next()
                S.op("dve", lambda e, t1=t1, pr=pr, co=co: e.tensor_tensor(out=t1[0:nrows, :], in0=pr[0:nrows, :], in1=co[0:nrows, :], op=ALU.mult),
                     reads=[pr, co], writes=[t1])
                S.op("dve", lambda e, t2=t2, ps_=ps_, si=si: e.tensor_tensor(out=t2[0:nrows, :], in0=ps_[0:nrows, :], in1=si[0:nrows, :], op=ALU.mult),
                     reads=[ps_, si], writes=[t2])
                S.op("pool", lambda e, t1=t1, t2=t2, tb=tb: e.tensor_tensor(out=sg[0:nrows, tb * 512:(tb + 1) * 512], in0=t1[0:nrows, :],
                                                                           in1=t2[0:nrows, :], op=ALU.add), reads=[t1, t2], writes=[sg])

        sg = stg.next()
        rope_group(lambda k: wkr[:, k, 0:32], lambda k: wkr[:, k, 32:64], hT, 8, 32, sg)
        for h in range(8):
            S.dma("sp", "stkr%d" % h, lambda e, h=h, sg=sg: e.dma_start(out=kC[h, 0:32, :], in_=sg[0:32, :]), reads=[sg])
        for g in range(2):
            sg = stg.next()
            rope_group(lambda k, g=g: wqr[:, k, g * 128:(g + 1) * 128], lambda k, g=g: wqs[:, k, g * 128:(g + 1) * 128], mqn, 3, 128, sg)
            for i in range(4):
                S.dma("sp", "stqr%d" % i, lambda e, g=g, i=i, sg=sg: e.dma_start(out=qC[4 * g + i, 0:32, :], in_=sg[32 * i:32 * i + 32, :]), reads=[sg])

        def fm_group2(w, wc, src, nk, dsts):
            sg = stg.next()
            for tb in range(NB):
                p = pp.next()
                for k in range(nk):
                    S.op("pe", lambda e, p=p, k=k, tb=tb: e.matmul(p[:], lhsT=w[:, k, wc:wc + 128], rhs=src[:, k, tb * 512:(tb + 1) * 512],
                                                                   start=(k == 0), stop=(k == nk - 1)), reads=[w, src], writes=[p])
                evac(sg[:, tb * 512:(tb + 1) * 512], p, sg)
            for i, (dap, r0, nr) in enumerate(dsts):
                S.dma("sp", "st%s_%d" % (sg.name, i), lambda e, dap=dap, r0=r0, nr=nr, sg=sg: e.dma_start(out=dap, in_=sg[r0:r0 + nr, :]), reads=[sg])
        for g in range(4):
            fm_group2(wqn, g * 128, mqn, 3, [(qC[2 * g, 32:96, :], 0, 64), (qC[2 * g + 1, 32:96, :], 64, 64)])
        for g in range(4):
            fm_group2(wkn, g * 128, ckvn, 2, [(kC[2 * g, 32:96, :], 0, 64), (kC[2 * g + 1, 32:96, :], 64, 64)])
        tm_group(wvc, vC, ckvn, 2)
        P.close()

    def phase_att(l, kind):
        P = Phase(nc); S = P.S
        ident = P.sb([128, 128], BF16, "ident")
        S.dma("sp", "id", lambda e: e.dma_start(out=ident[:], in_=C_ident), writes=[ident])
        if kind == "A":
            KR, DV, NH, NM = 68, 128, 4, 2
            qd, kd, vd = qA, kA, vA
            scale = 0.125
        elif kind == "B":
            KR, DV, NH, NM = 64, 64, 8, 1
            qd, kd, vd = qB, kB, vB
            scale = 0.125
        else:
            KR, DV, NH, NM = 96, 64, 8, 1
            qd, kd, vd = qC, kC, vC
            scale = 96.0 ** -0.5
        NMAP = NH * NM
        qr = P.ring(4, [128, T], BF16, "qT"); kr = P.ring(4, [128, T], BF16, "kT")
        vr = P.ring(3, [128, NT, DV + 1], BF16, "v")
        for v in vr.bufs:
            S.op("pool", lambda e, v=v: e.memset(v[:, :, DV:DV + 1], 1.0), writes=[v])
        pS = P.ring(3, [128, 512], F32, "pS", psum=True)
        pO = [P.ps([128, 512], F32, "pO") for _ in range(4)]
        pTt = P.ps([128, 1024], BF16, "pTt")
        ptr = P.ring(4, [128, 512], BF16, "pt")
        st = P.ring(4, [128, 4], F32, "st")
        o1n = P.sb([128, 4, 128], F32, "o1n")
        af = P.ring(2, [128, 128], F32, "af")
        yb = P.ring(12, [128, 128], BF16, "yb")
        junk = P.sb([128, 128], BF16, "junk")
        yst = P.ring(2, [128, 512], BF16, "yst")
        dy = Buf(yT_d, "yT_d")
        if kind == "A":
            neglam = P.sb([128, 1], F32, "neglam")
            S.dma("sp", "nl", lambda e: e.dma_start(out=neglam[:], in_=lam_d[0, l:l + 1].partition_broadcast(128)), writes=[neglam])
            gsub = P.sb([128, 128], F32, "gsub")
            S.dma("sp", "gs", lambda e: e.dma_start(out=gsub[:], in_=I["diff_subln_g"][l, :].partition_broadcast(128)), writes=[gsub])
            lam_init = 0.8 - 0.6 * math.exp(-0.3 * l)
            S.op("dve", lambda e: e.tensor_scalar(out=gsub[:], in0=gsub[:], scalar1=1.0 - lam_init, scalar2=None, op0=ALU.mult),
                 reads=[gsub], writes=[gsub])
            corr = P.sb([128, 4, 64], F32, "corr")
            S.dma("sp", "corr", lambda e: e.dma_start(out=corr[:], in_=C_corr.rearrange("h p q -> p h q")), writes=[corr])
        if kind == "B":
            anti = P.sb([128, 128], BF16, "anti")
            S.dma("sp", "anti", lambda e: e.dma_start(out=anti[:], in_=C_anti), writes=[anti])
            dE = Buf(E_d, "E_d")
            rel = I["chunk_rel_bias"][l]
            S.dma("sp", "E1", lambda e: e.dma_start(out=E_d[:, 0:384], in_=rel[:, 129:513]), writes=[dE])
            e1 = P.sb([8, 1], F32, "e1"); e2 = P.sb([8, 384], F32, "e2")
            S.dma("sp", "E2a", lambda e: e.dma_start(out=e1[:], in_=rel[:, 512:513], allow_slow_non_contiguous=True), writes=[e1])
            S.op("dve", lambda e: e.tensor_copy(out=e2[:], in_=e1[:, 0:1].to_broadcast([8, 384])), reads=[e1], writes=[e2])
            S.dma("sp", "E2", lambda e: e.dma_start(out=E_d[:, 384:768], in_=e2[:]), reads=[e2], writes=[dE])
            bf = P.sb([128, 640], F32, "bf")
            bhi = P.sb([128, 8, 640], BF16, "bhi"); blo = P.sb([128, 8, 640], BF16, "blo")
            bh32 = P.sb([128, 640], F32, "bh32")
            for h in range(8):
                S.dma("sp", "bf", lambda e, h=h: e.dma_start(out=bf[:], in_=bass.AP(E_d.tensor, h * 768, [[1, 128], [1, 640]])),
                      reads=[dE], writes=[bf])
                S.op("dve", lambda e: e.tensor_scalar(out=bf[:], in0=bf[:], scalar1=1.0 / scale, scalar2=None, op0=ALU.mult), reads=[bf], writes=[bf])
                S.op("dve", lambda e, h=h: e.tensor_copy(out=bhi[:, h, :], in_=bf[:]), reads=[bf], writes=[bhi])
                S.op("dve", lambda e, h=h: e.tensor_copy(out=bh32[:], in_=bhi[:, h, :]), reads=[bhi], writes=[bh32])
                S.op("dve", lambda e, h=h: e.tensor_tensor(out=blo[:, h, :], in0=bf[:], in1=bh32[:], op=ALU.subtract), reads=[bf, bh32], writes=[blo])

        def load_map(m):
            h = m // NM
            q = qr.next(); k = kr.next()
            nd = 64 if kind != "C" else 96
            S.dma("sp", q.name, lambda e: e.dma_start(out=q[0:nd, :], in_=qd[m]), writes=[q])
            S.dma("sp", k.name, lambda e: e.dma_start(out=k[0:nd, :], in_=kd[m]), writes=[k])
            if kind == "A":
                S.dma("sp", q.name, lambda e: e.dma_start(out=q[64:68, :], in_=C_augq[h]), writes=[q])
                S.dma("sp", k.name, lambda e: e.dma_start(out=k[64:68, :], in_=C_augk[h]), writes=[k])
            return q, k

        def load_v(h):
            v = vr.next()
            S.dma("sp", v.name, lambda e: e.dma_start(out=v[:, :, 0:DV], in_=vd[:, h * DV:(h + 1) * DV].rearrange("(t p) d -> p t d", p=128)),
                  writes=[v])
            return v

        LA = 2
        jobs = []
        for h in range(NH):
            for Qb in range(NB):
                for c in range(NM):
                    if kind == "B":
                        kts = list(range(max(0, 4 * Qb - 4), 4 * Qb + 4))
                    else:
                        kts = list(range(0, 4 * Qb + 4))
                    for kt in kts:
                        jobs.append((h, Qb, c, kt, kt == kts[-1]))
        cur = {"h": None, "v": None, "maps": None}
        pre = {}

        def stage1(job):
            h, Qb, c, kt, last = job
            if cur["h"] != h:
                cur["h"] = h
                if h not in pre:
                    pre[h] = (load_v(h), [load_map(h * NM + cc) for cc in range(NM)])
                cur["v"], cur["maps"] = pre.pop(h)
                if h + 1 < NH:
                    pre[h + 1] = (load_v(h + 1), [load_map((h + 1) * NM + cc) for cc in range(NM)])
            v = cur["v"]
            q, k = cur["maps"][c]
            if kind == "B":
                jlo = max(kt, 4 * Qb); jhi = min(kt + 4, 4 * Qb + 3)
            else:
                jlo = max(kt, 4 * Qb); jhi = 4 * Qb + 3
            c0 = (jlo - 4 * Qb) * 128; c1 = (jhi - 4 * Qb + 1) * 128
            ps_ = pS.next(); pt = ptr.next()
            lastmm = (kind != "B")
            S.op("pe", lambda e: e.matmul(ps_[:, c0:c1], lhsT=k[0:KR, kt * 128:(kt + 1) * 128], rhs=q[0:KR, Qb * 512 + c0:Qb * 512 + c1],
                                          start=True, stop=lastmm), reads=[q, k], writes=[ps_])
            if kind == "B":
                b0 = (jlo - kt) * 128; b1 = (jhi - kt + 1) * 128
                S.op("pe", lambda e: e.matmul(ps_[:, c0:c1], lhsT=anti[:], rhs=bhi[:, h, b0:b1], start=False, stop=False), reads=[anti, bhi], writes=[ps_])
                S.op("pe", lambda e: e.matmul(ps_[:, c0:c1], lhsT=anti[:], rhs=blo[:, h, b0:b1], start=False, stop=True), reads=[anti, blo], writes=[ps_])
            S.op("act", lambda e: e.activation(out=pt[:, c0:c1], in_=ps_[:, c0:c1], func=AF.Exp, scale=scale), reads=[ps_], writes=[pt])
            if kt >= 4 * Qb:
                m0 = (kt - 4 * Qb) * 128
                if kind == "A":
                    S.op("pool", lambda e: e.tensor_tensor(out=pt[0:64, m0:m0 + 64], in0=pt[0:64, m0:m0 + 64], in1=corr[0:64, h, :], op=ALU.mult),
                         reads=[pt, corr], writes=[pt])
                    S.op("pool", lambda e: e.tensor_tensor(out=pt[64:128, m0 + 64:m0 + 128], in0=pt[64:128, m0 + 64:m0 + 128], in1=corr[64:128, h, :], op=ALU.mult),
                         reads=[pt, corr], writes=[pt])
                S.op("pool", lambda e: e.memset(pt[64:128, m0:m0 + 64], 0.0), writes=[pt])
            if kind == "B" and kt + 4 <= 4 * Qb + 3:
                m1 = (kt + 4 - 4 * Qb) * 128 + 64
                S.op("pool", lambda e: e.memset(pt[0:64, m1:m1 + 64], 0.0), writes=[pt])
            return (job, pt, v, jlo, jhi)

        def stage2(rec):
            (h, Qb, c, kt, last), pt, v, jlo, jhi = rec
            for j in range(jlo, jhi + 1):
                jj = j - 4 * Qb
                first = (kt == (max(0, j - 4) if kind == "B" else 0))
                S.op("pe", lambda e, jj=jj, first=first, j=j: e.matmul(
                    pO[jj][:, 0:DV + 1], lhsT=pt[:, jj * 128:(jj + 1) * 128], rhs=v[:, kt, :], start=first, stop=(kt == j)),
                    reads=[pt, v], writes=[pO[jj]])
            if not last:
                return None
            ys = None
            defer = []
            for jj in range(4):
                s_ = st.next()
                S.op("dve", lambda e, s_=s_, jj=jj: e.reciprocal(out=s_[:, 0:1], in_=pO[jj][:, DV:DV + 1]), reads=[pO[jj]], writes=[s_])
                if kind == "A":
                    if c == 0:
                        S.op("dve", lambda e, s_=s_, jj=jj: e.tensor_scalar(out=o1n[:, jj, :], in0=pO[jj][:, 0:DV], scalar1=s_[:, 0:1], scalar2=None,
                                                                            op0=ALU.mult), reads=[pO[jj], s_], writes=[o1n])
                        continue
                    a = af.next(); y = yb.next()
                    S.op("dve", lambda e, s_=s_: e.tensor_tensor(out=s_[:, 1:2], in0=s_[:, 0:1], in1=neglam[:], op=ALU.mult), reads=[s_, neglam], writes=[s_])
                    S.op("dve", lambda e, s_=s_, jj=jj, a=a: e.scalar_tensor_tensor(out=a[:], in0=pO[jj][:, 0:DV], scalar=s_[:, 1:2], in1=o1n[:, jj, :],
                                                                                    op0=ALU.mult, op1=ALU.add), reads=[pO[jj], s_, o1n], writes=[a])
                    s2 = st.next()
                    S.op("act", lambda e, a=a, s2=s2: e.activation(out=junk[:], in_=a[:], func=AF.Square, accum_out=s2[:, 0:1]), reads=[a], writes=[junk, s2])
                    rstd_from_ssq(S, P, s2, 128, lnexp=True)
                    S.op("dve", lambda e, a=a, s2=s2, y=y: e.scalar_tensor_tensor(out=y[:], in0=a[:], scalar=s2[:, 1:2], in1=gsub[:],
                                                                                  op0=ALU.mult, op1=ALU.mult), reads=[a, s2, gsub], writes=[y])
                    defer.append(lambda y=y, jj=jj: S.op("pe", lambda e: e.transpose(pTt[:, jj * 128:(jj + 1) * 128], y[:], ident[:]), reads=[y, ident], writes=[pTt]))
                else:
                    y = yb.next()
                    S.op("dve", lambda e, s_=s_, jj=jj, y=y: e.tensor_scalar(out=y[:, 0:DV], in0=pO[jj][:, 0:DV], scalar1=s_[:, 0:1], scalar2=None,
                                                                             op0=ALU.mult), reads=[pO[jj], s_], writes=[y])
                    defer.append(lambda y=y, jj=jj: S.op("pe", lambda e: e.transpose(pTt[0:DV, jj * 128:(jj + 1) * 128], y[:, 0:DV], ident[:]), reads=[y, ident], writes=[pTt]))
            if kind == "A":
                if c == 1:
                    def fin_a():
                        ys = yst.next()
                        S.op("act", lambda e: e.activation(out=ys[:], in_=pTt[:, 0:512], func=AF.Copy), reads=[pTt], writes=[ys])
                        S.dma("sp", "st" + ys.name, lambda e: e.dma_start(out=yT_d[h, :, Qb * 512:(Qb + 1) * 512], in_=ys[:]),
                              reads=[ys], writes=[dy])
                    defer.append(fin_a)
            else:
                def fin_bc():
                    ys2 = yst.next()
                    S.op("act", lambda e: e.activation(out=ys2[0:DV, :], in_=pTt[0:DV, 0:512], func=AF.Copy), reads=[pTt], writes=[ys2])
                    ch = (4 if kind == "B" else 8) + h // 2
                    r0 = (h % 2) * 64
                    S.dma("sp", "st" + ys2.name, lambda e: e.dma_start(
                        out=yT_d[ch, r0:r0 + 64, Qb * 512:(Qb + 1) * 512], in_=ys2[0:64, :]), reads=[ys2], writes=[dy])
                defer.append(fin_bc)
            return defer

        pend = []
        dq = []

        def tick():
            for d in dq:
                d[0] -= 1
            while dq and dq[0][0] <= 0:
                for fn in dq.pop(0)[1]:
                    fn()

        def run2(rec):
            d = stage2(rec)
            tick()
            if d:
                dq.append([3, d])

        for job in jobs:
            pend.append(stage1(job))
            if len(pend) > LA:
                run2(pend.pop(0))
        while pend:
            run2(pend.pop(0))
        while dq:
            for fn in dq.pop(0)[1]:
                fn()
        P.close()

    def phase_mrg(l, xsrc):
        P = Phase(nc); S = P.S
        ident = P.sb([128, 128], F32, "identf")
        S.dma("sp", "id", lambda e: e.dma_start(out=ident[:], in_=C_identf), writes=[ident])
        wbr = P.sb([128, 12, D], BF16, "wbr")
        for i, nm in enumerate(("w_branch_diff", "w_branch_chunk", "w_branch_mla")):
            S.dma("pool", "wbr", lambda e, i=i, nm=nm: e.dma_start(out=wbr[:, 4 * i:4 * i + 4, :], in_=I[nm][l].rearrange("(k p) n -> p k n", p=128)), writes=[wbr])
        wg = P.sb([128, 8, 3 * D], BF16, "wg")
        for i in range(6):
            S.dma("pool", "wg", lambda e, i=i: e.dma_start(out=wg[:, :, i * 512:(i + 1) * 512],
                                                            in_=I["w_in"][l][:, 3744 + i * 512:3744 + (i + 1) * 512].rearrange("(k p) n -> p k n", p=128)), writes=[wg])
        wo = P.sb([128, 8, D], BF16, "wo")
        S.dma("pool", "wo", lambda e: e.dma_start(out=wo[:], in_=I["w_out"][l].rearrange("(k p) n -> p k n", p=128)), writes=[wo])
        wrt = P.sb([128, 8, 16], F32, "wrt")
        S.dma("sp", "wrt", lambda e: e.dma_start(out=wrt[:], in_=I["w_router"].rearrange("(k p) n -> p k n", p=128)), writes=[wrt])
        rbias = P.sb([128, 16], F32, "rbias")
        S.dma("sp", "rb", lambda e: e.dma_start(out=rbias[:], in_=I["router_bias"][0, :].partition_broadcast(128)), writes=[rbias])
        gtB = P.sb([128, D], F32, "gtB"); A2 = P.sb([128, D], F32, "A2"); B2 = P.sb([128, D], F32, "B2")
        S.dma("sp", "gtB", lambda e: e.dma_start(out=gtB[:], in_=modB_d[l][:, 2048:3072]), writes=[gtB])
        S.dma("sp", "A2", lambda e: e.dma_start(out=A2[:], in_=modB_d[l][:, 4096:5120]), writes=[A2])
        S.dma("sp", "B2", lambda e: e.dma_start(out=B2[:], in_=modB_d[l][:, 3072:4096]), writes=[B2])
        yr = P.ring(2, [128, 12, 512], BF16, "y"); hr = P.ring(2, [128, 8, 512], BF16, "h")
        pz = P.ring(2, [128, 512], F32, "pz", psum=True); pg = P.ring(2, [128, 512], F32, "pg", psum=True)
        py = [P.ps([128, 512], F32, "py") for _ in range(2)]
        pT2 = [P.ps([128, 512], F32, "pT2") for _ in range(2)]
        sgr = P.ring(2, [128, 512], F32, "sg"); mr = P.ring(2, [128, 512], F32, "m"); tr = P.ring(2, [128, 512], F32, "t")
        mT = P.ring(1, [128, 8, 512], BF16, "mT")
        xr = P.ring(2, [128, D], F32, "x"); tmp = P.ring(1, [128, D], F32, "tmp")
        h2f = P.ring(2, [128, D], F32, "h2f")
        junk = P.sb([128, D], BF16, "junk"); st = P.ring(2, [128, 2], F32, "st")
        h2Tf = P.ring(2, [128, 8, 128], F32, "h2Tf")
        hst = P.ring(1, [128, 8, 512], BF16, "hst")
        rt = P.ring(2, [128, 8, 16], F32, "rt")
        cst = P.ring(2, [16, 512], F32, "cst")
        dxs = Buf(xs, "xs"); dh2 = Buf(h2T_d, "h2T_d"); dcb = Buf(combT_d, "combT_d")
        yh = {}
        xl = {}

        def load_yh(tb):
            y = yr.next(); h = hr.next()
            S.dma("sp", y.name, lambda e: e.dma_start(out=y[:], in_=yT_d[:, :, tb * 512:(tb + 1) * 512].rearrange("k p t -> p k t")), writes=[y])
            S.dma("sp", h.name, lambda e: e.dma_start(out=h[:], in_=hT_d[:, :, tb * 512:(tb + 1) * 512].rearrange("k p t -> p k t")), writes=[h])
            yh[tb] = (y, h)

        def load_x(t_):
            xt = xr.next()
            S.dma("sp", xt.name, lambda e: e.dma_start(out=xt[:], in_=xsrc[t_ * 128:(t_ + 1) * 128, :]), writes=[xt])
            xl[t_] = xt

        load_yh(0)
        for tb in range(NB):
            y, h = yh.pop(tb)
            m_T = mT.next(); hs = hst.next(); cs = cst.next()
            for f in range(8):
                m = mr.next()
                for br in range(3):
                    z = pz.next(); g = pg.next(); sg = sgr.next()
                    for c4 in range(4):
                        S.op("pe", lambda e, z=z, c4=c4, br=br, f=f, y=y: e.matmul(z[:], lhsT=wbr[:, br * 4 + c4, f * 128:(f + 1) * 128], rhs=y[:, br * 4 + c4, :],
                                                                                 start=(c4 == 0), stop=(c4 == 3)), reads=[wbr, y], writes=[z])
                    for k in range(8):
                        S.op("pe", lambda e, g=g, k=k, br=br, f=f, h=h: e.matmul(g[:], lhsT=wg[:, k, br * D + f * 128:br * D + (f + 1) * 128], rhs=h[:, k, :],
                                                                               start=(k == 0), stop=(k == 7)), reads=[wg, h], writes=[g])
                    S.op("act", lambda e, g=g, sg=sg: e.activation(out=sg[:], in_=g[:], func=AF.Sigmoid), reads=[g], writes=[sg])
                    if br == 0:
                        S.op("dve", lambda e, z=z, sg=sg, m=m: e.tensor_tensor(out=m[:], in0=z[:], in1=sg[:], op=ALU.mult), reads=[z, sg], writes=[m])
                    else:
                        t = tr.next()
                        S.op("dve", lambda e, z=z, sg=sg, t=t: e.tensor_tensor(out=t[:], in0=z[:], in1=sg[:], op=ALU.mult), reads=[z, sg], writes=[t])
                        if br == 1:
                            S.op("pool", lambda e, m=m, t=t: e.tensor_tensor(out=m[:], in0=m[:], in1=t[:], op=ALU.add), reads=[m, t], writes=[m])
                        else:
                            S.op("pool", lambda e, m=m, t=t, f=f, m_T=m_T: e.tensor_tensor(out=m_T[:, f, :], in0=m[:], in1=t[:], op=ALU.add),
                                 reads=[m, t], writes=[m_T])
            def stageA(tt, tb=tb, m_T=m_T):
                t_ = tb * 4 + tt
                if t_ not in xl:
                    load_x(t_)
                xt = xl.pop(t_)
                if t_ + 1 < NT:
                    load_x(t_ + 1)
                tm = tmp.next(); s_ = st.next(); hf = h2f.next()
                for half in range(2):
                    for k in range(8):
                        S.op("pe", lambda e, half=half, k=k: e.matmul(py[half][:], lhsT=m_T[:, k, tt * 128:(tt + 1) * 128],
                                                                      rhs=wo[:, k, half * 512:(half + 1) * 512], start=(k == 0), stop=(k == 7)),
                             reads=[m_T, wo], writes=[py[half]])
                    S.op("dve", lambda e, half=half: e.tensor_tensor(out=tm[:, half * 512:(half + 1) * 512], in0=py[half][:],
                                                                     in1=gtB[:, half * 512:(half + 1) * 512], op=ALU.mult), reads=[py[half], gtB], writes=[tm])
                S.op("pool", lambda e: e.tensor_tensor(out=xt[:], in0=tm[:], in1=xt[:], op=ALU.add), reads=[tm, xt], writes=[xt])
                S.dma("sp", "st" + xt.name, lambda e: e.dma_start(out=xs[t_ * 128:(t_ + 1) * 128, :], in_=xt[:]), reads=[xt], writes=[dxs])
                norm_tile(S, xt, A2, B2, junk, s_, tm)
                S.op("pool", lambda e: e.tensor_tensor(out=hf[:], in0=tm[:], in1=B2[:], op=ALU.add), reads=[tm, B2], writes=[hf])
                return hf

            def stageB(tt, hf, hs=hs, cs=cs):
                hTf = h2Tf.next(); r = rt.next()
                for k in range(8):
                    S.op("pe", lambda e, k=k: e.matmul(pT2[k // 4][:, (k % 4) * 128:(k % 4 + 1) * 128], lhsT=hf[:, k * 128:(k + 1) * 128], rhs=ident[:],
                                                       start=True, stop=True),
                         reads=[hf, ident], writes=[pT2[k // 4]])
                for hh in range(2):
                    S.op("dve", lambda e, hh=hh: e.tensor_copy(out=hTf[:, hh * 4:(hh + 1) * 4, :], in_=pT2[hh][:].rearrange("p (k t) -> p k t", k=4)),
                         reads=[pT2[hh]], writes=[hTf])
                    S.op("act", lambda e, hh=hh: e.activation(out=hs[:, hh * 4:(hh + 1) * 4, tt * 128:(tt + 1) * 128],
                                                              in_=hTf[:, hh * 4:(hh + 1) * 4, :], func=AF.Copy),
                         reads=[hTf], writes=[hs])
                pl = pz.next()
                for k in range(8):
                    S.op("pe", lambda e, k=k: e.matmul(pl[:, 0:16], lhsT=hTf[:, k, :], rhs=wrt[:, k, :], start=(k == 0), stop=(k == 7)),
                         reads=[hTf, wrt], writes=[pl])
                sc = r[:, 0, :]; sel = r[:, 1, :]; w1 = r[:, 2, :]; w2 = r[:, 3, :]; w3 = r[:, 4, :]; msk = r[:, 5, :]; w4 = r[:, 6, :]; sm = r[:, 7, :]

                def V(fn, rd=()):
                    S.op("dve", fn, reads=[r] + list(rd), writes=[r])
                g4 = lambda ap: ap.rearrange("p (g i) -> p g i", g=4)
                S.op("act", lambda e: e.activation(out=sc, in_=pl[:, 0:16], func=AF.Sigmoid), reads=[pl], writes=[r])
                V(lambda e: e.tensor_tensor(out=sel, in0=sc, in1=rbias[:], op=ALU.add), [rbias])
                V(lambda e: e.tensor_reduce(out=sm[:, 0:4], in_=g4(sel), axis=AX.X, op=ALU.max))
                V(lambda e: e.tensor_tensor(out=g4(w1), in0=g4(sel), in1=sm[:, 0:4].unsqueeze(2).to_broadcast([128, 4, 4]), op=ALU.is_equal))
                V(lambda e: e.scalar_tensor_tensor(out=w2, in0=w1, scalar=-BIG, in1=sel, op0=ALU.mult, op1=ALU.add))
                V(lambda e: e.tensor_reduce(out=sm[:, 4:8], in_=g4(w2), axis=AX.X, op=ALU.max))
                V(lambda e: e.tensor_tensor(out=sm[:, 8:12], in0=sm[:, 0:4], in1=sm[:, 4:8], op=ALU.add))
                V(lambda e: e.tensor_reduce(out=sm[:, 12:13], in_=sm[:, 8:12], axis=AX.X, op=ALU.max))
                V(lambda e: e.tensor_tensor(out=sm[:, 4:8], in0=sm[:, 8:12], in1=sm[:, 12:13].to_broadcast([128, 4]), op=ALU.is_equal))
                V(lambda e: e.tensor_scalar(out=g4(w1), in0=sm[:, 4:8].unsqueeze(2).to_broadcast([128, 4, 4]), scalar1=-1.0, scalar2=BIG,
                                            op0=ALU.add, op1=ALU.mult))
                V(lambda e: e.tensor_tensor(out=w2, in0=w1, in1=sel, op=ALU.add))
                V(lambda e: e.tensor_reduce(out=sm[:, 13:14], in_=w2, axis=AX.X, op=ALU.max))
                V(lambda e: e.tensor_tensor(out=w3, in0=w2, in1=sm[:, 13:14].to_broadcast([128, 16]), op=ALU.is_equal))
                V(lambda e: e.scalar_tensor_tensor(out=w4, in0=w3, scalar=-BIG, in1=w2, op0=ALU.mult, op1=ALU.add))
                V(lambda e: e.tensor_reduce(out=sm[:, 14:15], in_=w4, axis=AX.X, op=ALU.max))
                V(lambda e: e.tensor_tensor(out=msk, in0=w4, in1=sm[:, 14:15].to_broadcast([128, 16]), op=ALU.is_equal))
                V(lambda e: e.tensor_tensor(out=msk, in0=msk, in1=w3, op=ALU.add))
                V(lambda e: e.tensor_tensor(out=w1, in0=sc, in1=msk, op=ALU.mult))
                V(lambda e: e.tensor_reduce(out=sm[:, 15:16], in_=w1, axis=AX.X, op=ALU.add))
                V(lambda e: e.reciprocal(out=sm[:, 15:16], in_=sm[:, 15:16]))
                V(lambda e: e.tensor_scalar(out=w2, in0=w1, scalar1=sm[:, 15:16], scalar2=None, op0=ALU.mult))
                pc = pg.next()
                S.op("pe", lambda e: e.matmul(pc[0:16, 0:128], lhsT=w2, rhs=ident[:], start=True, stop=True), reads=[r, ident], writes=[pc])
                S.op("act", lambda e: e.activation(out=cs[:, tt * 128:(tt + 1) * 128], in_=pc[0:16, 0:128], func=AF.Copy), reads=[pc], writes=[cs])

            if tb + 1 < NB:
                load_yh(tb + 1)
            hf0 = stageA(0)
            hf1 = stageA(1)
            stageB(0, hf0)
            hf2 = stageA(2)
            stageB(1, hf1)
            hf3 = stageA(3)
            stageB(2, hf2)
            stageB(3, hf3)
            S.dma("sp", "st" + hs.name, lambda e, hs=hs, tb=tb: e.dma_start(out=h2T_d[:, :, tb * 512:(tb + 1) * 512].rearrange("k p t -> p k t"), in_=hs[:]),
                  reads=[hs], writes=[dh2])
            S.dma("sp", "st" + cs.name, lambda e, cs=cs, tb=tb: e.dma_start(out=combT_d[:, tb * 512:(tb + 1) * 512], in_=cs[:]), reads=[cs], writes=[dcb])
        P.close()

    def phase_moe1(l):
        P = Phase(nc); S = P.S
        h2T = P.sb([128, 8, T], BF16, "h2T")
        for k in range(8):
            S.dma("sp", "h2T%d" % k, lambda e, k=k: e.dma_start(out=h2T[:, k, :], in_=h2T_d[k]), writes=[h2T])
        combT = P.sb([16, T], F32, "combT")
        S.dma("sp", "cT", lambda e: e.dma_start(out=combT[:], in_=combT_d), writes=[combT])
        sel = P.sb([16, 16 * 128], F32, "sel")
        S.dma("sp", "sel", lambda e: e.dma_start(out=sel[:], in_=C_sel), writes=[sel])
        wgr = P.ring(2, [128, 8, 512], BF16, "wg"); wur = P.ring(2, [128, 8, 512], BF16, "wu")
        pg = P.ring(2, [128, 512], F32, "pg", psum=True); pu = P.ring(2, [128, 512], F32, "pu", psum=True)
        pc = P.ring(2, [128, 512], F32, "pc", psum=True)
        cbr = P.ring(2, [128, 512], F32, "cb"); sgr = P.ring(2, [128, 512], F32, "sg"); tr = P.ring(2, [128, 512], F32, "t")
        her = P.ring(2, [128, 4, 512], BF16, "he")
        dhe = Buf(HE_d, "HE_d")
        for ex in range(16):
            wg_ = wgr.next(); wu_ = wur.next()
            S.dma("pool", wg_.name, lambda e, wg_=wg_, ex=ex: e.dma_start(out=wg_[:], in_=I["w_exp_gate"][l, ex].rearrange("(k p) n -> p k n", p=128)), writes=[wg_])
            S.dma("pool", wu_.name, lambda e, wu_=wu_, ex=ex: e.dma_start(out=wu_[:], in_=I["w_exp_up"][l, ex].rearrange("(k p) n -> p k n", p=128)), writes=[wu_])
            for tb in range(NB):
                p_c = pc.next(); cb = cbr.next(); he = her.next()
                S.op("pe", lambda e, p_c=p_c, ex=ex, tb=tb: e.matmul(p_c[:], lhsT=sel[:, ex * 128:(ex + 1) * 128], rhs=combT[:, tb * 512:(tb + 1) * 512],
                                                                      start=True, stop=True), reads=[sel, combT], writes=[p_c])
                S.op("act", lambda e, p_c=p_c, cb=cb: e.activation(out=cb[:], in_=p_c[:], func=AF.Copy), reads=[p_c], writes=[cb])
                for fc in range(4):
                    g = pg.next(); u = pu.next(); sg = sgr.next(); t = tr.next()
                    for k in range(8):
                        S.op("pe", lambda e, g=g, k=k, fc=fc, tb=tb, wg_=wg_: e.matmul(g[:], lhsT=wg_[:, k, fc * 128:(fc + 1) * 128], rhs=h2T[:, k, tb * 512:(tb + 1) * 512],
                                                                                     start=(k == 0), stop=(k == 7)), reads=[wg_, h2T], writes=[g])
                    for k in range(8):
                        S.op("pe", lambda e, u=u, k=k, fc=fc, tb=tb, wu_=wu_: e.matmul(u[:], lhsT=wu_[:, k, fc * 128:(fc + 1) * 128], rhs=h2T[:, k, tb * 512:(tb + 1) * 512],
                                                                                     start=(k == 0), stop=(k == 7)), reads=[wu_, h2T], writes=[u])
                    S.op("act", lambda e, g=g, sg=sg: e.activation(out=sg[:], in_=g[:], func=AF.Silu), reads=[g], writes=[sg])
                    S.op("dve", lambda e, u=u, sg=sg, t=t: e.tensor_tensor(out=t[:], in0=u[:], in1=sg[:], op=ALU.mult), reads=[u, sg], writes=[t])
                    S.op("dve", lambda e, t=t, cb=cb, he=he, fc=fc: e.tensor_tensor(out=he[:, fc, :], in0=t[:], in1=cb[:], op=ALU.mult), reads=[t, cb], writes=[he])
                for a4 in range(4):
                    S.dma("sp", "st" + he.name, lambda e, he=he, ex=ex, tb=tb, a4=a4: e.dma_start(
                        out=HE_d[tb * 4 + a4, :, ex * 4:(ex + 1) * 4, :],
                        in_=he[:, :, a4 * 128:(a4 + 1) * 128]), reads=[he], writes=[dhe])
        P.close()

    def phase_moe2(l):
        P = Phase(nc); S = P.S
        last = (l == L - 1)
        ident = P.sb([128, 128], BF16, "ident")
        S.dma("sp", "id", lambda e: e.dma_start(out=ident[:], in_=C_ident), writes=[ident])
        wd = P.sb([128, 64, D], BF16, "wd")
        for i in range(16):
            S.dma("pool", "wd", lambda e, i=i: e.dma_start(out=wd[:, 4 * i:4 * i + 4, :],
                                                            in_=I["w_exp_down"][l][i * 512:(i + 1) * 512, :].rearrange("(k p) n -> p k n", p=128)), writes=[wd])
        gtB = P.sb([128, D], F32, "gtB")
        S.dma("sp", "gtB", lambda e: e.dma_start(out=gtB[:], in_=modB_d[l][:, 5120:6144]), writes=[gtB])
        AB = P.sb([128, D], F32, "AB")
        if last:
            S.dma("sp", "AB", lambda e: e.dma_start(out=AB[:], in_=I["g_final"][0, :].partition_broadcast(128)), writes=[AB])
            BB = None
        else:
            BB = P.sb([128, D], F32, "BB")
            S.dma("sp", "AB", lambda e: e.dma_start(out=AB[:], in_=modB_d[l + 1][:, 1024:2048]), writes=[AB])
            S.dma("sp", "BB", lambda e: e.dma_start(out=BB[:], in_=modB_d[l + 1][:, 0:1024]), writes=[BB])
        her = P.ring(2, [128, 64, 128], BF16, "he")
        po = [P.ps([128, 512], F32, "po") for _ in range(4)]
        pT = P.ring(2, [128, 1024], BF16, "pT", psum=True)
        xr = P.ring(2, [128, D], F32, "x"); tmp = P.ring(2, [128, D], F32, "tmp")
        junk = P.sb([128, D], BF16, "junk"); st = P.ring(2, [128, 2], F32, "st")
        hbr = P.ring(2, [128, D], BF16, "hb")
        hst = P.ring(1, [128, 8, 512], BF16, "hst")
        dxs = Buf(xs, "xs"); dh = Buf(hT_d, "hT_d")
        hcur = {"hs": None}

        loaded = {}

        def loads(t_):
            he = her.next(); xt = xr.next()
            S.dma("sp", he.name, lambda e: e.dma_start(out=he[:], in_=HE_d[t_]), writes=[he])
            S.dma("sp", xt.name, lambda e: e.dma_start(out=xt[:], in_=xs[t_ * 128:(t_ + 1) * 128, :]), writes=[xt])
            loaded[t_] = (he, xt)

        def stageA(t_):
            if t_ not in loaded:
                loads(t_)
            he, xt = loaded.pop(t_)
            if t_ + 1 < NT:
                loads(t_ + 1)
            tm = tmp.next(); s_ = st.next()
            for half in range(2):
                pp_ = po[(t_ % 2) * 2 + half]
                for c in range(64):
                    S.op("pe", lambda e, pp_=pp_, c=c, half=half: e.matmul(pp_[:], lhsT=he[:, c, :], rhs=wd[:, c, half * 512:(half + 1) * 512],
                                                                           start=(c == 0), stop=(c == 63)), reads=[he, wd], writes=[pp_])
                S.op("dve", lambda e, pp_=pp_, half=half: e.tensor_tensor(out=tm[:, half * 512:(half + 1) * 512], in0=pp_[:],
                                                                          in1=gtB[:, half * 512:(half + 1) * 512], op=ALU.mult), reads=[pp_, gtB], writes=[tm])
            S.op("pool", lambda e: e.tensor_tensor(out=xt[:], in0=tm[:], in1=xt[:], op=ALU.add), reads=[tm, xt], writes=[xt])
            if not last:
                S.dma("sp", "st" + xt.name, lambda e: e.dma_start(out=xs[t_ * 128:(t_ + 1) * 128, :], in_=xt[:]), reads=[xt], writes=[dxs])
            norm_tile(S, xt, AB, BB, junk, s_, tm)
            if last:
                S.dma("sp", "st" + tm.name, lambda e: e.dma_start(out=out_d[t_ * 128:(t_ + 1) * 128, :], in_=tm[:]), reads=[tm])
                return None
            hb = hbr.next()
            S.op("pool", lambda e: e.tensor_tensor(out=hb[:], in0=tm[:], in1=BB[:], op=ALU.add), reads=[tm, BB], writes=[hb])
            return hb

        def stageB(t_, hb):
            if hb is None:
                return
            if t_ % 4 == 0:
                hcur["hs"] = hst.next()
            hs = hcur["hs"]
            p = pT.next()
            transpose_store(S, P, hb, p, hs, (t_ % 4) * 128, ident)
            if t_ % 4 == 3:
                tb = t_ // 4
                S.dma("sp", "st" + hs.name, lambda e: e.dma_start(
                    out=hT_d[:, :, tb * 512:(tb + 1) * 512].rearrange("k p t -> p k t"), in_=hs[:]), reads=[hs], writes=[dh])

        prev = None
        for t_ in range(NT):
            hb = stageA(t_)
            if prev is not None:
                stageB(*prev)
            prev = (t_, hb)
        stageB(*prev)
        P.close()

    plist = []
    for l in range(L):
        plist.append(lambda l=l: phase_mod(l))
        plist.append(lambda l=l: phase_lam(l))
    plist.append(lambda: phase_n1(0, I["x"]))
    for l in range(L):
        plist.append(lambda l=l: phase_prj(l))
        for kind in ("A", "B", "C"):
            plist.append(lambda l=l, kind=kind: phase_att(l, kind))
        plist.append(lambda l=l: phase_mrg(l, I["x"] if l == 0 else xs))
        plist.append(lambda l=l: phase_moe1(l))
        plist.append(lambda l=l: phase_moe2(l))
    for i, f in enumerate(plist):
        if stop is not None and i >= stop:
            break
        f()
    return nc


def _consts():
    bf = ml_dtypes.bfloat16
    ident = np.eye(128, dtype=np.float32)
    anti = ident[::-1].copy()
    sel = np.zeros((16, 16, 128), np.float32)
    for e in range(16):
        sel[e, e, :] = 1.0
    pos = np.arange(T, dtype=np.float32)
    inv_freq = (1.0 / (10000.0 ** (np.arange(0, 32, 2, dtype=np.float32) / 32.0))).astype(np.float32)
    ang = (pos[:, None] * inv_freq[None, :]).astype(np.float32)
    cos = np.cos(ang.astype(np.float64)).astype(np.float32); sin = np.sin(ang.astype(np.float64)).astype(np.float32)
    cos32 = np.concatenate([cos, cos], axis=1).T
    sin32 = np.concatenate([-sin, sin], axis=1).T
    cos128 = np.tile(cos32, (4, 1)).astype(np.float32)
    sin128 = np.tile(sin32, (4, 1)).astype(np.float32)
    slopes = np.exp2(-8.0 / 4 * np.arange(1, 5, dtype=np.float64))
    ipos = np.arange(T)
    a_hi = (ipos // 64) * 64.0
    a_lo = (ipos % 64) * 1.0
    augq = np.zeros((4, 4, T), np.float64); augk = np.zeros((4, 4, T), np.float64)
    for h in range(4):
        s8 = slopes[h] * 8.0
        augq[h, 0] = -s8 * a_hi; augq[h, 1] = -s8 * a_lo; augq[h, 2] = 1.0; augq[h, 3] = 1.0
        augk[h, 0] = 1.0; augk[h, 1] = 1.0; augk[h, 2] = s8 * a_hi; augk[h, 3] = s8 * a_lo
    kk = np.arange(64)[:, None]; qq = np.arange(64)[None, :]
    corr = np.zeros((4, 128, 64), np.float64)
    for h in range(4):
        c = np.exp(-2.0 * slopes[h] * np.maximum(kk - qq, 0))
        corr[h, 0:64] = c; corr[h, 64:128] = c
    return {
        "k_ident": ident.astype(bf), "k_identf": ident, "k_anti": anti.astype(bf),
        "k_sel": sel.reshape(16, 16 * 128), "k_cos": np.ascontiguousarray(cos128), "k_sin": np.ascontiguousarray(sin128),
        "k_augq": augq.astype(np.float32).astype(bf), "k_augk": augk.astype(np.float32).astype(bf),
        "k_corr": corr.astype(np.float32),
    }


_NC_CACHE = {}


def kernel(**inputs):
    if "nc" not in _NC_CACHE:
        _NC_CACHE["nc"] = build_program()
    nc = _NC_CACHE["nc"]
    consts = _consts()
    shared = {}
    for k, v in inputs.items():
        if k in ("x", "c"):
            continue
        a = np.ascontiguousarray(np.asarray(v, dtype=np.float32))
        if k == "router_bias":
            a = a.reshape(1, 16)
        elif k == "g_final":
            a = a.reshape(1, D)
        elif k == "w_exp_down":
            a = a.reshape(L, 16 * 512, D)
        shared[k] = a
    shared.update(consts)
    x = np.asarray(inputs["x"], dtype=np.float32)
    c = np.asarray(inputs["c"], dtype=np.float32)
    in_maps = []
    for core in range(8):
        b = core % 4
        m = dict(shared)
        m["x"] = np.ascontiguousarray(x[b])
        m["c"] = np.ascontiguousarray(c[b].reshape(8, 128).T)
        in_maps.append(m)
    res = run_bass_kernel_spmd(nc, in_maps, core_ids=list(range(8)))
    out = np.stack([np.asarray(res.results[b]["out"], dtype=np.float32) for b in range(4)], axis=0)
    return out
```

```python
import math
import numpy as np
import ml_dtypes
from contextlib import ExitStack
import concourse.bass as bass
import concourse.mybir as mybir
from concourse.bass_utils import run_bass_kernel_spmd

F32 = mybir.dt.float32
BF16 = mybir.dt.bfloat16
AF = mybir.ActivationFunctionType
ALU = mybir.AluOpType
AX = mybir.AxisListType

T = 4096
D = 1024
L = 2
NT = T // 128
NB = T // 512
EPS = 1e-6
INW = 6816
BIG = 30000.0
N1_LIMIT = NB


class Buf:
    __slots__ = ("t", "w", "r", "name")

    def __init__(self, t, name=""):
        self.t = t
        self.w = None
        self.r = {}
        self.name = name

    def __getitem__(self, k):
        return self.t[k]


class Sched:
    ENG = ("pe", "act", "dve", "pool", "sp")
    SAME_ENGINE_SYNC = True
    NO_SELF_SYNC = ("pe",)

    def __init__(self, nc):
        self.nc = nc
        self.prog = {e: [] for e in self.ENG}
        self.seq = {e: 0 for e in self.ENG}
        self.last = {e: 0 for e in self.ENG}
        self.dma_cnt = {}
        self.waited = {e: {} for e in self.ENG}
        self.fence = {e: {} for e in self.ENG}
        self.needed = {e: set() for e in self.ENG}

    def _deps(self, eng, reads, writes):
        d = dict(self.fence[eng])
        self.fence[eng] = {}

        def add(tok):
            k, v = tok
            if d.get(k, 0) < v:
                d[k] = v
        for b in reads:
            if b.w is not None:
                add(b.w)
        for b in writes:
            if b.w is not None:
                add(b.w)
            for k, v in b.r.items():
                add((k, v))
        out = []
        for k, v in d.items():
            if k == eng and (eng in self.NO_SELF_SYNC or not self.SAME_ENGINE_SYNC):
                continue
            if self.waited[eng].get(k, 0) >= v:
                continue
            self.waited[eng][k] = v
            out.append((k, v))
            if k in self.needed:
                self.needed[k].add(v)
        return out

    def _mark(self, tok, reads, writes):
        k, v = tok
        for b in reads:
            if b.r.get(k, 0) < v:
                b.r[k] = v
        for b in writes:
            b.w = tok
            b.r = {}

    def op(self, eng, fn, reads=(), writes=()):
        waits = self._deps(eng, reads, writes)
        self.seq[eng] += 1
        tok = (eng, self.seq[eng])
        self.last[eng] = self.seq[eng]
        self.prog[eng].append((waits, fn, tok, None))
        self._mark(tok, reads, writes)

    def dma(self, q, key, fn, reads=(), writes=()):
        waits = self._deps(q, reads, writes)
        n = self.dma_cnt.get(key, 0) + 1
        self.dma_cnt[key] = n
        tok = (key, n)
        self.prog[q].append((waits, fn, None, key))
        self._mark(tok, reads, writes)

    def barrier(self):
        toks = {}
        for e in self.ENG:
            if self.last[e] > 0:
                toks[e] = self.last[e]
        for k, n in self.dma_cnt.items():
            toks[k] = n
        for e in self.ENG:
            f = self.fence[e]
            for k, v in toks.items():
                if f.get(k, 0) < v:
                    f[k] = v

    def emit(self):
        nc = self.nc
        self.barrier()
        for e in self.ENG:
            waits = self._deps(e, (), ())
            if waits:
                self.prog[e].append((waits, None, None, None))
        sems = {}
        handles = []
        for e in self.ENG:
            _UID[0] += 1
            sems[e] = nc.alloc_semaphore(name="s%d_%s" % (_UID[0], e))
            handles.append(sems[e])
        for i, k in enumerate(self.dma_cnt):
            _UID[0] += 1
            sems[k] = nc.alloc_semaphore(name="d%d_%d" % (_UID[0], i))
            handles.append(sems[k])
        rank = {e: {v: i + 1 for i, v in enumerate(sorted(self.needed[e]))}
                for e in self.ENG}
        ENGSET = set(self.ENG)

        def run(ename, e):
            rk = rank[ename]
            for (waits, fn, tok, key) in self.prog[ename]:
                for (k, v) in waits:
                    e.wait_ge(sems[k], rank[k][v] if k in ENGSET else 16 * v)
                if fn is None:
                    continue
                ins = fn(e)
                if key is not None:
                    ins.then_inc(sems[key], 16)
                elif tok[1] in rk:
                    ins.then_inc(sems[ename], 1)

        for h in handles:
            nc.gpsimd.sem_clear(h)
        nc.all_engine_barrier()
        with nc.Block() as block:
            block.tensor(lambda e: run("pe", e))
            block.scalar(lambda e: run("act", e))
            block.vector(lambda e: run("dve", e))
            block.gpsimd(lambda e: run("pool", e))
            block.sync(lambda e: run("sp", e))
        nc.clear_and_free_semaphores(handles)
        nc.all_engine_barrier()


class Ring:
    def __init__(self, bufs):
        self.bufs = bufs
        self.i = 0

    def next(self):
        b = self.bufs[self.i % len(self.bufs)]
        self.i += 1
        return b


_UID = [0]


class Phase:
    def __init__(self, nc):
        self.nc = nc
        self.es = ExitStack()
        self.S = Sched(nc)
        self.n = 0

    def sb(self, shape, dt, name=None):
        _UID[0] += 1
        name = "%s_%d" % (name or "t", _UID[0])
        return Buf(self.es.enter_context(self.nc.sbuf_tensor(name, shape, dt)), name)

    def ps(self, shape, dt, name=None):
        _UID[0] += 1
        name = "%s_%d" % (name or "p", _UID[0])
        return Buf(self.es.enter_context(self.nc.psum_tensor(name, shape, dt)), name)

    def ring(self, n, shape, dt, name, psum=False):
        return Ring([(self.ps if psum else self.sb)(shape, dt, name) for _ in range(n)])

    def close(self):
        self.S.emit()
        self.es.close()


def build_program(stop=None, debug=False):
    nc = bass.Bass("TRN2", target_bir_lowering=False)

    def din(name, shape, dt=F32):
        return nc.dram_tensor(name, list(shape), dt, kind="ExternalInput").ap()

    def dscr(name, shape, dt=BF16):
        if debug:
            return nc.dram_tensor(name, list(shape), dt, kind="ExternalOutput").ap()
        return nc.dram_tensor(name, list(shape), dt).ap()

    I = {}
    I["x"] = din("x", [T, D])
    I["c"] = din("c", [128, 8])
    I["w_mod"] = din("w_mod", [L, D, 6 * D])
    I["b_mod"] = din("b_mod", [L, 6 * D])
    I["g_norm_mix"] = din("g_norm_mix", [L, D])
    I["g_norm_ffn"] = din("g_norm_ffn", [L, D])
    I["w_in"] = din("w_in", [L, D, INW])
    for nm in ("diff_lambda_q1", "diff_lambda_k1", "diff_lambda_q2", "diff_lambda_k2"):
        I[nm] = din(nm, [L, 64])
    I["diff_subln_g"] = din("diff_subln_g", [L, 128])
    I["chunk_rel_bias"] = din("chunk_rel_bias", [L, 8, 513])
    I["mla_q_norm_g"] = din("mla_q_norm_g", [L, 384])
    I["mla_w_q_b"] = din("mla_w_q_b", [L, 384, 768])
    I["mla_kv_norm_g"] = din("mla_kv_norm_g", [L, 256])
    I["mla_w_kv_b"] = din("mla_w_kv_b", [L, 256, 1024])
    I["w_branch_diff"] = din("w_branch_diff", [L, 512, D])
    I["w_branch_chunk"] = din("w_branch_chunk", [L, 512, D])
    I["w_branch_mla"] = din("w_branch_mla", [L, 512, D])
    I["w_out"] = din("w_out", [L, D, D])
    I["w_router"] = din("w_router", [D, 16])
    I["router_bias"] = din("router_bias", [1, 16])
    I["w_exp_gate"] = din("w_exp_gate", [L, 16, D, 512])
    I["w_exp_up"] = din("w_exp_up", [L, 16, D, 512])
    I["w_exp_down"] = din("w_exp_down", [L, 16 * 512, D])
    I["g_final"] = din("g_final", [1, D])
    C_ident = din("k_ident", [128, 128], BF16)
    C_identf = din("k_identf", [128, 128], F32)
    C_anti = din("k_anti", [128, 128], BF16)
    C_sel = din("k_sel", [16, 16 * 128], F32)
    C_cos = din("k_cos", [128, T], F32)
    C_sin = din("k_sin", [128, T], F32)
    C_augq = din("k_augq", [4, 4, T], BF16)
    C_augk = din("k_augk", [4, 4, T], BF16)
    C_corr = din("k_corr", [4, 128, 64], F32)
    out_d = nc.dram_tensor("out", [T, D], F32, kind="ExternalOutput").ap()

    xs = dscr("xs", [T, D], F32)
    modB_d = dscr("modB", [L, 128, 6 * D], F32)
    hT_d = dscr("hT", [8, 128, T])
    qA = dscr("qA", [8, 64, T]); kA = dscr("kA", [8, 64, T]); vA = dscr("vA", [T, 512])
    qB = dscr("qB", [8, 64, T]); kB = dscr("kB", [8, 64, T]); vB = dscr("vB", [T, 512])
    qC = dscr("qC", [8, 96, T]); kC = dscr("kC", [8, 96, T]); vC = dscr("vC", [T, 512])
    yT_d = dscr("yT", [12, 128, T])
    h2T_d = dscr("h2T", [8, 128, T])
    combT_d = dscr("combT", [16, T], F32)
    HE_d = dscr("HE", [NT, 128, 64, 128])
    E_d = dscr("Ebias", [8, 768], F32)
    lam_d = dscr("lam", [1, 4], F32)

    def rstd_from_ssq(S, P, st, n, lnexp=False):
        S.op("dve", lambda e: e.tensor_scalar(out=st[:, 1:2], in0=st[:, 0:1], scalar1=1.0 / n, scalar2=EPS,
                                              op0=ALU.mult, op1=ALU.add), reads=[st], writes=[st])
        if lnexp:
            S.op("act", lambda e: e.activation(out=st[:, 1:2], in_=st[:, 1:2], func=AF.Ln), reads=[st], writes=[st])
            S.op("act", lambda e: e.activation(out=st[:, 1:2], in_=st[:, 1:2], func=AF.Exp, scale=-0.5), reads=[st], writes=[st])
            return
        S.op("act", lambda e: e.activation(out=st[:, 1:2], in_=st[:, 1:2], func=AF.Sqrt), reads=[st], writes=[st])
        S.op("dve", lambda e: e.reciprocal(out=st[:, 1:2], in_=st[:, 1:2]), reads=[st], writes=[st])

    def norm_tile(S, xt, AB, BB, junk, st, tmp):
        S.op("act", lambda e: e.activation(out=junk[:], in_=xt[:], func=AF.Square, accum_out=st[:, 0:1]),
             reads=[xt], writes=[junk, st])
        rstd_from_ssq(S, None, st, D)
        S.op("dve", lambda e: e.scalar_tensor_tensor(out=tmp[:], in0=xt[:], scalar=st[:, 1:2], in1=AB[:],
                                                     op0=ALU.mult, op1=ALU.mult), reads=[xt, st, AB], writes=[tmp])

    def phase_mod(l):
        P = Phase(nc); S = P.S
        cl = P.sb([128, 8], F32, "cl"); cact = P.sb([128, 8], F32, "cact")
        cB = P.sb([128, 8, 128], F32, "cB")
        wr = P.ring(3, [128, 8, 512], F32, "wmod")
        bb = P.ring(2, [128, 512], F32, "bb")
        gb = P.ring(2, [128, 512], F32, "gb")
        ob = P.ring(2, [128, 512], F32, "ob")
        pp = P.ring(2, [128, 512], F32, "pm", psum=True)
        dmod = Buf(modB_d, "modB_d")
        S.dma("sp", "cl", lambda e: e.dma_start(out=cl[:], in_=I["c"]), writes=[cl])
        S.op("act", lambda e: e.activation(out=cact[:], in_=cl[:], func=AF.Silu), reads=[cl], writes=[cact])
        for k in range(8):
            S.op("dve", lambda e, k=k: e.tensor_copy(out=cB[:, k, :], in_=cact[:, k:k + 1].to_broadcast([128, 128])),
                 reads=[cact], writes=[cB])
        for j in range(12):
            w = wr.next(); b = bb.next(); o = ob.next(); p = pp.next()
            S.dma("sp", w.name, lambda e, w=w, j=j: e.dma_start(
                out=w[:], in_=I["w_mod"][l][:, j * 512:(j + 1) * 512].rearrange("(k p) n -> p k n", p=128)), writes=[w])
            S.dma("sp", b.name, lambda e, b=b, j=j: e.dma_start(
                out=b[:], in_=I["b_mod"][l, j * 512:(j + 1) * 512].partition_broadcast(128)), writes=[b])
            for k in range(8):
                S.op("pe", lambda e, w=w, p=p, k=k: e.matmul(p[:], lhsT=cB[:, k, :], rhs=w[:, k, :],
                                                               start=(k == 0), stop=(k == 7)), reads=[cB, w], writes=[p])
            S.op("dve", lambda e, o=o, p=p, b=b: e.tensor_tensor(out=o[:], in0=p[:], in1=b[:], op=ALU.add),
                 reads=[p, b], writes=[o])
            if j // 2 in (1, 4):
                g = gb.next()
                gsrc = I["g_norm_mix"] if j // 2 == 1 else I["g_norm_ffn"]
                c0 = (j % 2) * 512
                S.dma("sp", g.name, lambda e, g=g, gsrc=gsrc, c0=c0: e.dma_start(
                    out=g[:], in_=gsrc[l, c0:c0 + 512].partition_broadcast(128)), writes=[g])
                S.op("dve", lambda e, o=o, g=g: e.scalar_tensor_tensor(out=o[:], in0=o[:], scalar=1.0, in1=g[:],
                                                                        op0=ALU.add, op1=ALU.mult), reads=[o, g], writes=[o])
            S.dma("sp", "st" + o.name, lambda e, o=o, j=j: e.dma_start(out=modB_d[l][:, j * 512:(j + 1) * 512], in_=o[:]),
                  reads=[o], writes=[dmod])
        P.close()

    def phase_lam(l):
        P = Phase(nc); S = P.S
        a = P.sb([1, 4, 64], F32, "lama"); pr = P.sb([1, 2, 64], F32, "lampr"); s2 = P.sb([1, 4], F32, "lams")
        for i, nm in enumerate(("diff_lambda_q1", "diff_lambda_k1", "diff_lambda_q2", "diff_lambda_k2")):
            S.dma("sp", "la%d" % i, lambda e, i=i, nm=nm: e.dma_start(out=a[:, i, :], in_=I[nm][l:l + 1, :]), writes=[a])
        S.op("dve", lambda e: e.tensor_tensor(out=pr[:, 0, :], in0=a[:, 0, :], in1=a[:, 1, :], op=ALU.mult), reads=[a], writes=[pr])
        S.op("dve", lambda e: e.tensor_tensor(out=pr[:, 1, :], in0=a[:, 2, :], in1=a[:, 3, :], op=ALU.mult), reads=[a, pr], writes=[pr])
        S.op("dve", lambda e: e.tensor_reduce(out=s2[:, 0:2], in_=pr[:], axis=AX.X, op=ALU.add), reads=[pr], writes=[s2])
        S.op("act", lambda e: e.activation(out=s2[:, 0:2], in_=s2[:, 0:2], func=AF.Exp), reads=[s2], writes=[s2])
        lam_init = 0.8 - 0.6 * math.exp(-0.3 * l)
        S.op("dve", lambda e: e.tensor_tensor(out=s2[:, 2:3], in0=s2[:, 0:1], in1=s2[:, 1:2], op=ALU.subtract), reads=[s2], writes=[s2])
        S.op("dve", lambda e: e.tensor_scalar(out=s2[:, 3:4], in0=s2[:, 2:3], scalar1=lam_init, scalar2=-1.0,
                                              op0=ALU.add, op1=ALU.mult), reads=[s2], writes=[s2])
        dl = Buf(lam_d, "lam_d")
        S.dma("sp", "lst", lambda e: e.dma_start(out=lam_d[0:1, l:l + 1], in_=s2[:, 3:4]), reads=[s2], writes=[dl])
        P.close()

    def transpose_store(S, P, hb, pT, hst, col, ident):
        for k in range(8):
            S.op("pe", lambda e, k=k: e.transpose(pT[:, k * 128:(k + 1) * 128], hb[:, k * 128:(k + 1) * 128], ident[:]),
                 reads=[hb, ident], writes=[pT])
        S.op("act", lambda e: e.activation(out=hst[:, :, col:col + 128],
                                           in_=pT[:].rearrange("p (k t) -> p k t", k=8), func=AF.Copy),
             reads=[pT], writes=[hst])

    def phase_n1(l, src):
        P = Phase(nc); S = P.S
        ident = P.sb([128, 128], BF16, "ident")
        AB = P.sb([128, D], F32, "AB"); BB = P.sb([128, D], F32, "BB")
        xr = P.ring(2, [128, D], F32, "x")
        junk = P.sb([128, D], BF16, "junk"); st = P.ring(2, [128, 2], F32, "st")
        tmp = P.ring(2, [128, D], F32, "tmp"); hbr = P.ring(2, [128, D], BF16, "hb")
        pT = P.ring(2, [128, 1024], BF16, "pT", psum=True)
        hst = P.ring(2, [128, 8, 512], BF16, "hst")
        dh = Buf(hT_d, "hT_d")
        S.dma("sp", "id", lambda e: e.dma_start(out=ident[:], in_=C_ident), writes=[ident])
        S.dma("sp", "AB", lambda e: e.dma_start(out=AB[:], in_=modB_d[l][:, 1024:2048]), writes=[AB])
        S.dma("sp", "BB", lambda e: e.dma_start(out=BB[:], in_=modB_d[l][:, 0:1024]), writes=[BB])
        for tb in range(N1_LIMIT):
            hs = hst.next()
            for tt in range(4):
                t = tb * 4 + tt
                xt = xr.next(); s_ = st.next(); tm = tmp.next(); hb = hbr.next(); p = pT.next()
                S.dma("sp", xt.name, lambda e, xt=xt, t=t: e.dma_start(out=xt[:], in_=src[t * 128:(t + 1) * 128, :]), writes=[xt])
                norm_tile(S, xt, AB, BB, junk, s_, tm)
                S.op("pool", lambda e, hb=hb, tm=tm: e.tensor_tensor(out=hb[:], in0=tm[:], in1=BB[:], op=ALU.add),
                     reads=[tm, BB], writes=[hb])
                transpose_store(S, P, hb, p, hs, tt * 128, ident)
            S.dma("sp", "st" + hs.name, lambda e, hs=hs, tb=tb: e.dma_start(
                out=hT_d[:, :, tb * 512:(tb + 1) * 512].rearrange("k p t -> p k t"), in_=hs[:]), reads=[hs], writes=[dh])
        P.close()

    def phase_prj(l):
        P = Phase(nc); S = P.S
        win = I["w_in"][l]
        hT = P.sb([128, 8, T], BF16, "hT")
        for k in range(8):
            S.dma("sp", "hT%d" % k, lambda e, k=k: e.dma_start(out=hT[:, k, :], in_=hT_d[k]), writes=[hT])
        wr = P.ring(2, [128, 8, 512], BF16, "w")
        pp = P.ring(8, [128, 512], F32, "pp", psum=True)
        stg = P.ring(2, [128, T], BF16, "stg")
        stv = P.ring(2, [128, 4, 512], BF16, "stv")
        evq = [0]

        def evac(dst_ap, p, dstbuf):
            evq[0] += 1
            if evq[0] % 2:
                S.op("act", lambda e: e.activation(out=dst_ap, in_=p[:], func=AF.Copy), reads=[p], writes=[dstbuf])
            else:
                S.op("dve", lambda e: e.tensor_copy(out=dst_ap, in_=p[:]), reads=[p], writes=[dstbuf])

        def load_w(c0, n):
            w = wr.next()
            S.dma("pool", w.name, lambda e: e.dma_start(
                out=w[:, :, 0:n], in_=win[:, c0:c0 + n].rearrange("(k p) n -> p k n", p=128)), writes=[w])
            return w

        def fm_group(w, wc, dsts):
            sg = stg.next()
            for tb in range(NB):
                p = pp.next()
                for k in range(8):
                    S.op("pe", lambda e, p=p, k=k, tb=tb: e.matmul(p[:], lhsT=w[:, k, wc:wc + 128],
                                                                   rhs=hT[:, k, tb * 512:(tb + 1) * 512],
                                                                   start=(k == 0), stop=(k == 7)), reads=[w, hT], writes=[p])
                evac(sg[:, tb * 512:(tb + 1) * 512], p, sg)
            for i, (dap, r0, nr) in enumerate(dsts):
                S.dma("sp", "st%s_%d" % (sg.name, i), lambda e, dap=dap, r0=r0, nr=nr: e.dma_start(out=dap, in_=sg[r0:r0 + nr, :]),
                      reads=[sg])

        def tm_group(w, dst, hsrc, nk):
            for t4 in range(NT // 4):
                sv = stv.next()
                for tt in range(4):
                    t = t4 * 4 + tt
                    p = pp.next()
                    for k in range(nk):
                        S.op("pe", lambda e, p=p, k=k, t=t: e.matmul(p[:], lhsT=hsrc[:, k, t * 128:(t + 1) * 128], rhs=w[:, k, 0:512],
                                                                     start=(k == 0), stop=(k == nk - 1)), reads=[w, hsrc], writes=[p])
                    evac(sv[:, tt, :], p, sv)
                S.dma("sp", "st" + sv.name, lambda e, sv=sv, t4=t4: e.dma_start(
                    out=dst[t4 * 512:(t4 + 1) * 512, :].rearrange("(a p) n -> p a n", p=128), in_=sv[:]), reads=[sv])

        for base, dst in ((0, qA), (512, kA)):
            w = load_w(base, 512)
            for g in range(4):
                fm_group(w, g * 128, [(dst[2 * g], 0, 64), (dst[2 * g + 1], 64, 64)])
        w = load_w(1024, 512); tm_group(w, vA, hT, 8)
        for base, dst in ((1536, qB), (2048, kB)):
            w = load_w(base, 512)
            for g in range(4):
                fm_group(w, g * 128, [(dst[2 * g], 0, 64), (dst[2 * g + 1], 64, 64)])
        w = load_w(2560, 512); tm_group(w, vB, hT, 8)

        onesf = P.sb([128, 128], F32, "onesf")
        S.op("pool", lambda e: e.memset(onesf[:], 1.0), writes=[onesf])
        mqn = P.sb([128, 3, T], BF16, "mqn"); ckvn = P.sb([128, 2, T], BF16, "ckvn")
        gq = P.sb([128, 3], F32, "gq"); gkv = P.sb([128, 2], F32, "gkv")
        S.dma("sp", "gq", lambda e: e.dma_start(out=gq[:], in_=I["mla_q_norm_g"][l].rearrange("(c p) -> p c", p=128),
                                                allow_slow_non_contiguous=True), writes=[gq])
        S.dma("sp", "gkv", lambda e: e.dma_start(out=gkv[:], in_=I["mla_kv_norm_g"][l].rearrange("(c p) -> p c", p=128),
                                                 allow_slow_non_contiguous=True), writes=[gkv])
        latf = P.ring(1, [128, 3, 512], F32, "latf"); latsq = P.ring(1, [128, 3, 512], F32, "latsq")
        rsB = P.ring(2, [128, 512], F32, "rsB")

        def latent(c0, nch, gvec, dstn):
            w = load_w(c0, nch * 128)
            for tb in range(NB):
                lf = latf.next(); lq = latsq.next(); rs = rsB.next()
                for c in range(nch):
                    p = pp.next()
                    for k in range(8):
                        S.op("pe", lambda e, p=p, k=k, c=c, tb=tb: e.matmul(p[:], lhsT=w[:, k, c * 128:(c + 1) * 128],
                                                                            rhs=hT[:, k, tb * 512:(tb + 1) * 512],
                                                                            start=(k == 0), stop=(k == 7)), reads=[w, hT], writes=[p])
                    S.op("act", lambda e, p=p, c=c, lf=lf: e.activation(out=lf[:, c, :], in_=p[:], func=AF.Copy), reads=[p], writes=[lf])
                    S.op("dve", lambda e, c=c, lf=lf, lq=lq: e.tensor_tensor(out=lq[:, c, :], in0=lf[:, c, :], in1=lf[:, c, :], op=ALU.mult),
                         reads=[lf], writes=[lq])
                p = pp.next()
                for c in range(nch):
                    S.op("pe", lambda e, p=p, c=c, lq=lq: e.matmul(p[:], lhsT=onesf[:], rhs=lq[:, c, :], start=(c == 0), stop=(c == nch - 1)),
                         reads=[onesf, lq], writes=[p])
                S.op("dve", lambda e, p=p, rs=rs: e.tensor_scalar(out=rs[:], in0=p[:], scalar1=1.0 / (nch * 128), scalar2=EPS,
                                                                  op0=ALU.mult, op1=ALU.add), reads=[p], writes=[rs])
                S.op("act", lambda e, rs=rs: e.activation(out=rs[:], in_=rs[:], func=AF.Sqrt), reads=[rs], writes=[rs])
                S.op("dve", lambda e, rs=rs: e.reciprocal(out=rs[:], in_=rs[:]), reads=[rs], writes=[rs])
                for c in range(nch):
                    S.op("dve", lambda e, c=c, lf=lf, rs=rs, tb=tb: e.scalar_tensor_tensor(
                        out=dstn[:, c, tb * 512:(tb + 1) * 512], in0=lf[:, c, :], scalar=gvec[:, c:c + 1], in1=rs[:],
                        op0=ALU.mult, op1=ALU.mult), reads=[lf, rs, gvec], writes=[dstn])

        latent(3072, 3, gq, mqn)
        latent(3456, 2, gkv, ckvn)

        cosr = P.ring(2, [128, 512], F32, "cos"); sinr = P.ring(2, [128, 512], F32, "sin")
        wkr = P.sb([128, 8, 64], BF16, "wkr")
        kr0 = 3456 + 256
        for (d0, s0, n) in ((0, kr0, 32), (32, kr0 + 16, 16), (48, kr0, 16)):
            S.dma("pool", "wkr%d" % d0, lambda e, d0=d0, s0=s0, n=n: e.dma_start(
                out=wkr[:, :, d0:d0 + n], in_=win[:, s0:s0 + n].rearrange("(k p) n -> p k n", p=128)), writes=[wkr])
        wqb = I["mla_w_q_b"][l]
        wkvb = I["mla_w_kv_b"][l]
        wqn = P.sb([128, 3, 512], BF16, "wqn"); wqr = P.sb([128, 3, 256], BF16, "wqr"); wqs = P.sb([128, 3, 256], BF16, "wqs")
        wkn = P.sb([128, 2, 512], BF16, "wkn"); wvc = P.sb([128, 2, 512], BF16, "wvc")

        def wsrc(ap2, c0, n):
            return ap2[:, c0:c0 + n].rearrange("(k p) n -> p k n", p=128)
        for h in range(8):
            S.dma("pool", "wqn", lambda e, h=h: e.dma_start(out=wqn[:, :, h * 64:(h + 1) * 64], in_=wsrc(wqb, h * 96, 64)), writes=[wqn])
            S.dma("pool", "wqr", lambda e, h=h: e.dma_start(out=wqr[:, :, h * 32:(h + 1) * 32], in_=wsrc(wqb, h * 96 + 64, 32)), writes=[wqr])
            S.dma("pool", "wqs", lambda e, h=h: e.dma_start(out=wqs[:, :, h * 32:h * 32 + 16], in_=wsrc(wqb, h * 96 + 80, 16)), writes=[wqs])
            S.dma("pool", "wqs", lambda e, h=h: e.dma_start(out=wqs[:, :, h * 32 + 16:h * 32 + 32], in_=wsrc(wqb, h * 96 + 64, 16)), writes=[wqs])
            S.dma("pool", "wkn", lambda e, h=h: e.dma_start(out=wkn[:, :, h * 64:(h + 1) * 64], in_=wsrc(wkvb, h * 128, 64)), writes=[wkn])
            S.dma("pool", "wvc", lambda e, h=h: e.dma_start(out=wvc[:, :, h * 64:(h + 1) * 64], in_=wsrc(wkvb, h * 128 + 64, 64)), writes=[wvc])

        t1r = P.ring(2, [128, 512], F32, "t1"); t2r = P.ring(2, [128, 512], F32, "t2")

        def rope_group(lhs_r, lhs_s, src, nk, nrows, sg):
            for tb in range(NB):
                co = cosr.next(); si = sinr.next()
                S.dma("sp", co.name, lambda e, co=co, tb=tb: e.dma_start(out=co[:], in_=C_cos[:, tb * 512:(tb + 1) * 512]), writes=[co])
                S.dma("sp", si.name, lambda e, si=si, tb=tb: e.dma_start(out=si[:], in_=C_sin[:, tb * 512:(tb + 1) * 512]), writes=[si])
                pr = pp.next(); ps_ = pp.next()
                for k in range(nk):
                    S.op("pe", lambda e, k=k, tb=tb, pr=pr: e.matmul(pr[0:nrows, :], lhsT=lhs_r(k), rhs=src[:, k, tb * 512:(tb + 1) * 512],
                                                                     start=(k == 0), stop=(k == nk - 1)), reads=[src, wkr, wqr], writes=[pr])
                for k in range(nk):
                    S.op("pe", lambda e, k=k, tb=tb, ps_=ps_: e.matmul(ps_[0:nrows, :], lhsT=lhs_s(k), rhs=src[:, k, tb * 512:(tb + 1) * 512],
                                                                       start=(k == 0), stop=(k == nk - 1)), reads=[src, wkr, wqs], writes=[ps_])
                t1 = t1r.next(); t2 = t2r.next()
                S.op("dve", lambda e, t1=t1, pr=pr, co=co: e.tensor_tensor(out=t1[0:nrows, :], in0=pr[0:nrows, :], in1=co[0:nrows, :], op=ALU.mult),
                     reads=[pr, co], writes=[t1])
                S.op("dve", lambda e, t2=t2, ps_=ps_, si=si: e.tensor_tensor(out=t2[0:nrows, :], in0=ps_[0:nrows, :], in1=si[0:nrows, :], op=ALU.mult),
                     reads=[ps_, si], writes=[t2])
                S.op("pool", lambda e, t1=t1, t2=t2, tb=tb: e.tensor_tensor(out=sg[0:nrows, tb * 512:(tb + 1) * 512], in0=t1[0:nrows, :],
                                                                           in1=t2[0:nrows, :], op=ALU.add), reads=[t1, t2], writes=[sg])

        sg = stg.next()
        rope_group(lambda k: wkr[:, k, 0:32], lambda k: wkr[:, k, 32:64], hT, 8, 32, sg)
        for h in range(8):
            S.dma("sp", "stkr%d" % h, lambda e, h=h, sg=sg: e.dma_start(out=kC[h, 0:32, :], in_=sg[0:32, :]), reads=[sg])
        for g in range(2):
            sg = stg.next()
            rope_group(lambda k, g=g: wqr[:, k, g * 128:(g + 1) * 128], lambda k, g=g: wqs[:, k, g * 128:(g + 1) * 128], mqn, 3, 128, sg)
            for i in range(4):
                S.dma("sp", "stqr%d" % i, lambda e, g=g, i=i, sg=sg: e.dma_start(out=qC[4 * g + i, 0:32, :], in_=sg[32 * i:32 * i + 32, :]), reads=[sg])

        def fm_group2(w, wc, src, nk, dsts):
            sg = stg.next()
            for tb in range(NB):
                p = pp.next()
                for k in range(nk):
                    S.op("pe", lambda e, p=p, k=k, tb=tb: e.matmul(p[:], lhsT=w[:, k, wc:wc + 128], rhs=src[:, k, tb * 512:(tb + 1) * 512],
                                                                   start=(k == 0), stop=(k == nk - 1)), reads=[w, src], writes=[p])
                evac(sg[:, tb * 512:(tb + 1) * 512], p, sg)
            for i, (dap, r0, nr) in enumerate(dsts):
                S.dma("sp", "st%s_%d" % (sg.name, i), lambda e, dap=dap, r0=r0, nr=nr, sg=sg: e.dma_start(out=dap, in_=sg[r0:r0 + nr, :]), reads=[sg])
        for g in range(4):
            fm_group2(wqn, g * 128, mqn, 3, [(qC[2 * g, 32:96, :], 0, 64), (qC[2 * g + 1, 32:96, :], 64, 64)])
        for g in range(4):
            fm_group2(wkn, g * 128, ckvn, 2, [(kC[2 * g, 32:96, :], 0, 64), (kC[2 * g + 1, 32:96, :], 64, 64)])
        tm_group(wvc, vC, ckvn, 2)
        P.close()

    def phase_att(l, kind):
        P = Phase(nc); S = P.S
        ident = P.sb([128, 128], BF16, "ident")
        S.dma("sp", "id", lambda e: e.dma_start(out=ident[:], in_=C_ident), writes=[ident])
        if kind == "A":
            KR, DV, NH, NM = 68, 128, 4, 2
            qd, kd, vd = qA, kA, vA
            scale = 0.125
        elif kind == "B":
            KR, DV, NH, NM = 64, 64, 8, 1
            qd, kd, vd = qB, kB, vB
            scale = 0.125
        else:
            KR, DV, NH, NM = 96, 64, 8, 1
            qd, kd, vd = qC, kC, vC
            scale = 96.0 ** -0.5
        NMAP = NH * NM
        qr = P.ring(4, [128, T], BF16, "qT"); kr = P.ring(4, [128, T], BF16, "kT")
        vr = P.ring(3, [128, NT, DV + 1], BF16, "v")
        for v in vr.bufs:
            S.op("pool", lambda e, v=v: e.memset(v[:, :, DV:DV + 1], 1.0), writes=[v])
        pS = P.ring(3, [128, 512], F32, "pS", psum=True)
        pO = [P.ps([128, 512], F32, "pO") for _ in range(4)]
        pTt = P.ps([128, 1024], BF16, "pTt")
        ptr = P.ring(4, [128, 512], BF16, "pt")
        st = P.ring(4, [128, 4], F32, "st")
        o1n = P.sb([128, 4, 128], F32, "o1n")
        af = P.ring(2, [128, 128], F32, "af")
        yb = P.ring(12, [128, 128], BF16, "yb")
        junk = P.sb([128, 128], BF16, "junk")
        yst = P.ring(2, [128, 512], BF16, "yst")
        dy = Buf(yT_d, "yT_d")
        if kind == "A":
            neglam = P.sb([128, 1], F32, "neglam")
            S.dma("sp", "nl", lambda e: e.dma_start(out=neglam[:], in_=lam_d[0, l:l + 1].partition_broadcast(128)), writes=[neglam])
            gsub = P.sb([128, 128], F32, "gsub")
            S.dma("sp", "gs", lambda e: e.dma_start(out=gsub[:], in_=I["diff_subln_g"][l, :].partition_broadcast(128)), writes=[gsub])
            lam_init = 0.8 - 0.6 * math.exp(-0.3 * l)
            S.op("dve", lambda e: e.tensor_scalar(out=gsub[:], in0=gsub[:], scalar1=1.0 - lam_init, scalar2=None, op0=ALU.mult),
                 reads=[gsub], writes=[gsub])
            corr = P.sb([128, 4, 64], F32, "corr")
            S.dma("sp", "corr", lambda e: e.dma_start(out=corr[:], in_=C_corr.rearrange("h p q -> p h q")), writes=[corr])
        if kind == "B":
            anti = P.sb([128, 128], BF16, "anti")
            S.dma("sp", "anti", lambda e: e.dma_start(out=anti[:], in_=C_anti), writes=[anti])
            dE = Buf(E_d, "E_d")
            rel = I["chunk_rel_bias"][l]
            S.dma("sp", "E1", lambda e: e.dma_start(out=E_d[:, 0:384], in_=rel[:, 129:513]), writes=[dE])
            e1 = P.sb([8, 1], F32, "e1"); e2 = P.sb([8, 384], F32, "e2")
            S.dma("sp", "E2a", lambda e: e.dma_start(out=e1[:], in_=rel[:, 512:513], allow_slow_non_contiguous=True), writes=[e1])
            S.op("dve", lambda e: e.tensor_copy(out=e2[:], in_=e1[:, 0:1].to_broadcast([8, 384])), reads=[e1], writes=[e2])
            S.dma("sp", "E2", lambda e: e.dma_start(out=E_d[:, 384:768], in_=e2[:]), reads=[e2], writes=[dE])
            bf = P.sb([128, 640], F32, "bf")
            bhi = P.sb([128, 8, 640], BF16, "bhi"); blo = P.sb([128, 8, 640], BF16, "blo")
            bh32 = P.sb([128, 640], F32, "bh32")
            for h in range(8):
                S.dma("sp", "bf", lambda e, h=h: e.dma_start(out=bf[:], in_=bass.AP(E_d.tensor, h * 768, [[1, 128], [1, 640]])),
                      reads=[dE], writes=[bf])
                S.op("dve", lambda e: e.tensor_scalar(out=bf[:], in0=bf[:], scalar1=1.0 / scale, scalar2=None, op0=ALU.mult), reads=[bf], writes=[bf])
                S.op("dve", lambda e, h=h: e.tensor_copy(out=bhi[:, h, :], in_=bf[:]), reads=[bf], writes=[bhi])
                S.op("dve", lambda e, h=h: e.tensor_copy(out=bh32[:], in_=bhi[:, h, :]), reads=[bhi], writes=[bh32])
                S.op("dve", lambda e, h=h: e.tensor_tensor(out=blo[:, h, :], in0=bf[:], in1=bh32[:], op=ALU.subtract), reads=[bf, bh32], writes=[blo])

        def load_map(m):
            h = m // NM
            q = qr.next(); k = kr.next()
            nd = 64 if kind != "C" else 96
            S.dma("sp", q.name, lambda e: e.dma_start(out=q[0:nd, :], in_=qd[m]), writes=[q])
            S.dma("sp", k.name, lambda e: e.dma_start(out=k[0:nd, :], in_=kd[m]), writes=[k])
            if kind == "A":
                S.dma("sp", q.name, lambda e: e.dma_start(out=q[64:68, :], in_=C_augq[h]), writes=[q])
                S.dma("sp", k.name, lambda e: e.dma_start(out=k[64:68, :], in_=C_augk[h]), writes=[k])
            return q, k

        def load_v(h):
            v = vr.next()
            S.dma("sp", v.name, lambda e: e.dma_start(out=v[:, :, 0:DV], in_=vd[:, h * DV:(h + 1) * DV].rearrange("(t p) d -> p t d", p=128)),
                  writes=[v])
            return v

        LA = 2
        jobs = []
        for h in range(NH):
            for Qb in range(NB):
                for c in range(NM):
                    if kind == "B":
                        kts = list(range(max(0, 4 * Qb - 4), 4 * Qb + 4))
                    else:
                        kts = list(range(0, 4 * Qb + 4))
                    for kt in kts:
                        jobs.append((h, Qb, c, kt, kt == kts[-1]))
        cur = {"h": None, "v": None, "maps": None}
        pre = {}

        def stage1(job):
            h, Qb, c, kt, last = job
            if cur["h"] != h:
                cur["h"] = h
                if h not in pre:
                    pre[h] = (load_v(h), [load_map(h * NM + cc) for cc in range(NM)])
                cur["v"], cur["maps"] = pre.pop(h)
                if h + 1 < NH:
                    pre[h + 1] = (load_v(h + 1), [load_map((h + 1) * NM + cc) for cc in range(NM)])
            v = cur["v"]
            q, k = cur["maps"][c]
            if kind == "B":
                jlo = max(kt, 4 * Qb); jhi = min(kt + 4, 4 * Qb + 3)
            else:
                jlo = max(kt, 4 * Qb); jhi = 4 * Qb + 3
            c0 = (jlo - 4 * Qb) * 128; c1 = (jhi - 4 * Qb + 1) * 128
            ps_ = pS.next(); pt = ptr.next()
            lastmm = (kind != "B")
            S.op("pe", lambda e: e.matmul(ps_[:, c0:c1], lhsT=k[0:KR, kt * 128:(kt + 1) * 128], rhs=q[0:KR, Qb * 512 + c0:Qb * 512 + c1],
                                          start=True, stop=lastmm), reads=[q, k], writes=[ps_])
            if kind == "B":
                b0 = (jlo - kt) * 128; b1 = (jhi - kt + 1) * 128
                S.op("pe", lambda e: e.matmul(ps_[:, c0:c1], lhsT=anti[:], rhs=bhi[:, h, b0:b1], start=False, stop=False), reads=[anti, bhi], writes=[ps_])
                S.op("pe", lambda e: e.matmul(ps_[:, c0:c1], lhsT=anti[:], rhs=blo[:, h, b0:b1], start=False, stop=True), reads=[anti, blo], writes=[ps_])
            S.op("act", lambda e: e.activation(out=pt[:, c0:c1], in_=ps_[:, c0:c1], func=AF.Exp, scale=scale), reads=[ps_], writes=[pt])
            if kt >= 4 * Qb:
                m0 = (kt - 4 * Qb) * 128
                if kind == "A":
                    S.op("pool", lambda e: e.tensor_tensor(out=pt[0:64, m0:m0 + 64], in0=pt[0:64, m0:m0 + 64], in1=corr[0:64, h, :], op=ALU.mult),
                         reads=[pt, corr], writes=[pt])
                    S.op("pool", lambda e: e.tensor_tensor(out=pt[64:128, m0 + 64:m0 + 128], in0=pt[64:128, m0 + 64:m0 + 128], in1=corr[64:128, h, :], op=ALU.mult),
                         reads=[pt, corr], writes=[pt])
                S.op("pool", lambda e: e.memset(pt[64:128, m0:m0 + 64], 0.0), writes=[pt])
            if kind == "B" and kt + 4 <= 4 * Qb + 3:
                m1 = (kt + 4 - 4 * Qb) * 128 + 64
                S.op("pool", lambda e: e.memset(pt[0:64, m1:m1 + 64], 0.0), writes=[pt])
            return (job, pt, v, jlo, jhi)

        def stage2(rec):
            (h, Qb, c, kt, last), pt, v, jlo, jhi = rec
            for j in range(jlo, jhi + 1):
                jj = j - 4 * Qb
                first = (kt == (max(0, j - 4) if kind == "B" else 0))
                S.op("pe", lambda e, jj=jj, first=first, j=j: e.matmul(
                    pO[jj][:, 0:DV + 1], lhsT=pt[:, jj * 128:(jj + 1) * 128], rhs=v[:, kt, :], start=first, stop=(kt == j)),
                    reads=[pt, v], writes=[pO[jj]])
            if not last:
                return None
            ys = None
            defer = []
            for jj in range(4):
                s_ = st.next()
                S.op("dve", lambda e, s_=s_, jj=jj: e.reciprocal(out=s_[:, 0:1], in_=pO[jj][:, DV:DV + 1]), reads=[pO[jj]], writes=[s_])
                if kind == "A":
                    if c == 0:
                        S.op("dve", lambda e, s_=s_, jj=jj: e.tensor_scalar(out=o1n[:, jj, :], in0=pO[jj][:, 0:DV], scalar1=s_[:, 0:1], scalar2=None,
                                                                            op0=ALU.mult), reads=[pO[jj], s_], writes=[o1n])
                        continue
                    a = af.next(); y = yb.next()
                    S.op("dve", lambda e, s_=s_: e.tensor_tensor(out=s_[:, 1:2], in0=s_[:, 0:1], in1=neglam[:], op=ALU.mult), reads=[s_, neglam], writes=[s_])
                    S.op("dve", lambda e, s_=s_, jj=jj, a=a: e.scalar_tensor_tensor(out=a[:], in0=pO[jj][:, 0:DV], scalar=s_[:, 1:2], in1=o1n[:, jj, :],
                                                                                    op0=ALU.mult, op1=ALU.add), reads=[pO[jj], s_, o1n], writes=[a])
                    s2 = st.next()
                    S.op("act", lambda e, a=a, s2=s2: e.activation(out=junk[:], in_=a[:], func=AF.Square, accum_out=s2[:, 0:1]), reads=[a], writes=[junk, s2])
                    rstd_from_ssq(S, P, s2, 128, lnexp=True)
                    S.op("dve", lambda e, a=a, s2=s2, y=y: e.scalar_tensor_tensor(out=y[:], in0=a[:], scalar=s2[:, 1:2], in1=gsub[:],
                                                                                  op0=ALU.mult, op1=ALU.mult), reads=[a, s2, gsub], writes=[y])
                    defer.append(lambda y=y, jj=jj: S.op("pe", lambda e: e.transpose(pTt[:, jj * 128:(jj + 1) * 128], y[:], ident[:]), reads=[y, ident], writes=[pTt]))
                else:
                    y = yb.next()
                    S.op("dve", lambda e, s_=s_, jj=jj, y=y: e.tensor_scalar(out=y[:, 0:DV], in0=pO[jj][:, 0:DV], scalar1=s_[:, 0:1], scalar2=None,
                                                                             op0=ALU.mult), reads=[pO[jj], s_], writes=[y])
                    defer.append(lambda y=y, jj=jj: S.op("pe", lambda e: e.transpose(pTt[0:DV, jj * 128:(jj + 1) * 128], y[:, 0:DV], ident[:]), reads=[y, ident], writes=[pTt]))
            if kind == "A":
                if c == 1:
                    def fin_a():
                        ys = yst.next()
                        S.op("act", lambda e: e.activation(out=ys[:], in_=pTt[:, 0:512], func=AF.Copy), reads=[pTt], writes=[ys])
                        S.dma("sp", "st" + ys.name, lambda e: e.dma_start(out=yT_d[h, :, Qb * 512:(Qb + 1) * 512], in_=ys[:]),
                              reads=[ys], writes=[dy])
                    defer.append(fin_a)
            else:
                def fin_bc():
                    ys2 = yst.next()
                    S.op("act", lambda e: e.activation(out=ys2[0:DV, :], in_=pTt[0:DV, 0:512], func=AF.Copy), reads=[pTt], writes=[ys2])
                    ch = (4 if kind == "B" else 8) + h // 2
                    r0 = (h % 2) * 64
                    S.dma("sp", "st" + ys2.name, lambda e: e.dma_start(
                        out=yT_d[ch, r0:r0 + 64, Qb * 512:(Qb + 1) * 512], in_=ys2[0:64, :]), reads=[ys2], writes=[dy])
                defer.append(fin_bc)
            return defer

        pend = []
        dq = []

        def tick():
            for d in dq:
                d[0] -= 1
            while dq and dq[0][0] <= 0:
                for fn in dq.pop(0)[1]:
                    fn()

        def run2(rec):
            d = stage2(rec)
            tick()
            if d:
                dq.append([3, d])

        for job in jobs:
            pend.append(stage1(job))
            if len(pend) > LA:
                run2(pend.pop(0))
        while pend:
            run2(pend.pop(0))
        while dq:
            for fn in dq.pop(0)[1]:
                fn()
        P.close()

    def phase_mrg(l, xsrc):
        P = Phase(nc); S = P.S
        ident = P.sb([128, 128], F32, "identf")
        S.dma("sp", "id", lambda e: e.dma_start(out=ident[:], in_=C_identf), writes=[ident])
        wbr = P.sb([128, 12, D], BF16, "wbr")
        for i, nm in enumerate(("w_branch_diff", "w_branch_chunk", "w_branch_mla")):
            S.dma("pool", "wbr", lambda e, i=i, nm=nm: e.dma_start(out=wbr[:, 4 * i:4 * i + 4, :], in_=I[nm][l].rearrange("(k p) n -> p k n", p=128)), writes=[wbr])
        wg = P.sb([128, 8, 3 * D], BF16, "wg")
        for i in range(6):
            S.dma("pool", "wg", lambda e, i=i: e.dma_start(out=wg[:, :, i * 512:(i + 1) * 512],
                                                            in_=I["w_in"][l][:, 3744 + i * 512:3744 + (i + 1) * 512].rearrange("(k p) n -> p k n", p=128)), writes=[wg])
        wo = P.sb([128, 8, D], BF16, "wo")
        S.dma("pool", "wo", lambda e: e.dma_start(out=wo[:], in_=I["w_out"][l].rearrange("(k p) n -> p k n", p=128)), writes=[wo])
        wrt = P.sb([128, 8, 16], F32, "wrt")
        S.dma("sp", "wrt", lambda e: e.dma_start(out=wrt[:], in_=I["w_router"].rearrange("(k p) n -> p k n", p=128)), writes=[wrt])
        rbias = P.sb([128, 16], F32, "rbias")
        S.dma("sp", "rb", lambda e: e.dma_start(out=rbias[:], in_=I["router_bias"][0, :].partition_broadcast(128)), writes=[rbias])
        gtB = P.sb([128, D], F32, "gtB"); A2 = P.sb([128, D], F32, "A2"); B2 = P.sb([128, D], F32, "B2")
        S.dma("sp", "gtB", lambda e: e.dma_start(out=gtB[:], in_=modB_d[l][:, 2048:3072]), writes=[gtB])
        S.dma("sp", "A2", lambda e: e.dma_start(out=A2[:], in_=modB_d[l][:, 4096:5120]), writes=[A2])
        S.dma("sp", "B2", lambda e: e.dma_start(out=B2[:], in_=modB_d[l][:, 3072:4096]), writes=[B2])
        yr = P.ring(2, [128, 12, 512], BF16, "y"); hr = P.ring(2, [128, 8, 512], BF16, "h")
        pz = P.ring(8, [128, 512], F32, "pz", psum=True); pg = pz
        sgr = P.ring(2, [128, 512], F32, "sg"); mr = P.ring(2, [128, 512], F32, "m"); tr = P.ring(2, [128, 512], F32, "t")
        mT = P.ring(1, [128, 8, 512], BF16, "mT")
        xr = P.ring(2, [128, D], F32, "x"); tmp = P.ring(1, [128, D], F32, "tmp")
        h2f = P.ring(2, [128, D], F32, "h2f")
        junk = P.sb([128, D], BF16, "junk"); st = P.ring(2, [128, 2], F32, "st")
        h2Tf = P.ring(2, [128, 8, 128], F32, "h2Tf")
        hst = P.ring(1, [128, 8, 512], BF16, "hst")
        rt = P.ring(2, [128, 8, 16], F32, "rt")
        cst = P.ring(2, [16, 512], F32, "cst")
        dxs = Buf(xs, "xs"); dh2 = Buf(h2T_d, "h2T_d"); dcb = Buf(combT_d, "combT_d")
        yh = {}
        xl = {}

        def load_yh(tb):
            y = yr.next(); h = hr.next()
            S.dma("sp", y.name, lambda e: e.dma_start(out=y[:], in_=yT_d[:, :, tb * 512:(tb + 1) * 512].rearrange("k p t -> p k t")), writes=[y])
            S.dma("sp", h.name, lambda e: e.dma_start(out=h[:], in_=hT_d[:, :, tb * 512:(tb + 1) * 512].rearrange("k p t -> p k t")), writes=[h])
            yh[tb] = (y, h)

        def load_x(t_):
            xt = xr.next()
            S.dma("sp", xt.name, lambda e: e.dma_start(out=xt[:], in_=xsrc[t_ * 128:(t_ + 1) * 128, :]), writes=[xt])
            xl[t_] = xt

        load_yh(0)
        for tb in range(NB):
            y, h = yh.pop(tb)
            m_T = mT.next(); hs = hst.next(); cs = cst.next()
            for f in range(8):
                m = mr.next()
                for br in range(3):
                    g = pg.next(); z = pz.next(); sg = sgr.next()
                    for k in range(8):
                        S.op("pe", lambda e, g=g, k=k, br=br, f=f, h=h: e.matmul(g[:], lhsT=wg[:, k, br * D + f * 128:br * D + (f + 1) * 128], rhs=h[:, k, :],
                                                                               start=(k == 0), stop=(k == 7)), reads=[wg, h], writes=[g])
                    for c4 in range(4):
                        S.op("pe", lambda e, z=z, c4=c4, br=br, f=f, y=y: e.matmul(z[:], lhsT=wbr[:, br * 4 + c4, f * 128:(f + 1) * 128], rhs=y[:, br * 4 + c4, :],
                                                                                 start=(c4 == 0), stop=(c4 == 3)), reads=[wbr, y], writes=[z])
                    S.op("act", lambda e, g=g, sg=sg: e.activation(out=sg[:], in_=g[:], func=AF.Sigmoid), reads=[g], writes=[sg])
                    if br == 0:
                        S.op("dve", lambda e, z=z, sg=sg, m=m: e.tensor_tensor(out=m[:], in0=z[:], in1=sg[:], op=ALU.mult), reads=[z, sg], writes=[m])
                    else:
                        t = tr.next()
                        S.op("dve", lambda e, z=z, sg=sg, t=t: e.tensor_tensor(out=t[:], in0=z[:], in1=sg[:], op=ALU.mult), reads=[z, sg], writes=[t])
                        if br == 1:
                            S.op("pool", lambda e, m=m, t=t: e.tensor_tensor(out=m[:], in0=m[:], in1=t[:], op=ALU.add), reads=[m, t], writes=[m])
                        else:
                            S.op("pool", lambda e, m=m, t=t, f=f, m_T=m_T: e.tensor_tensor(out=m_T[:, f, :], in0=m[:], in1=t[:], op=ALU.add),
                                 reads=[m, t], writes=[m_T])
            def stageA(tt, tb=tb, m_T=m_T):
                t_ = tb * 4 + tt
                if t_ not in xl:
                    load_x(t_)
                xt = xl.pop(t_)
                if t_ + 1 < NT:
                    load_x(t_ + 1)
                tm = tmp.next(); s_ = st.next(); hf = h2f.next()
                py = [pz.next(), pz.next()]
                for half in range(2):
                    for k in range(8):
                        S.op("pe", lambda e, half=half, k=k: e.matmul(py[half][:], lhsT=m_T[:, k, tt * 128:(tt + 1) * 128],
                                                                      rhs=wo[:, k, half * 512:(half + 1) * 512], start=(k == 0), stop=(k == 7)),
                             reads=[m_T, wo], writes=[py[half]])
                    S.op("dve", lambda e, half=half: e.tensor_tensor(out=tm[:, half * 512:(half + 1) * 512], in0=py[half][:],
                                                                     in1=gtB[:, half * 512:(half + 1) * 512], op=ALU.mult), reads=[py[half], gtB], writes=[tm])
                S.op("pool", lambda e: e.tensor_tensor(out=xt[:], in0=tm[:], in1=xt[:], op=ALU.add), reads=[tm, xt], writes=[xt])
                S.dma("sp", "st" + xt.name, lambda e: e.dma_start(out=xs[t_ * 128:(t_ + 1) * 128, :], in_=xt[:]), reads=[xt], writes=[dxs])
                norm_tile(S, xt, A2, B2, junk, s_, tm)
                S.op("pool", lambda e: e.tensor_tensor(out=hf[:], in0=tm[:], in1=B2[:], op=ALU.add), reads=[tm, B2], writes=[hf])
                return hf

            def stageB(tt, hf, hs=hs, cs=cs):
                hTf = h2Tf.next(); r = rt.next()
                pT2 = [pz.next(), pz.next()]
                for k in range(8):
                    S.op("pe", lambda e, k=k: e.matmul(pT2[k // 4][:, (k % 4) * 128:(k % 4 + 1) * 128], lhsT=hf[:, k * 128:(k + 1) * 128], rhs=ident[:],
                                                       start=True, stop=True),
                         reads=[hf, ident], writes=[pT2[k // 4]])
                for hh in range(2):
                    S.op("dve", lambda e, hh=hh: e.tensor_copy(out=hTf[:, hh * 4:(hh + 1) * 4, :], in_=pT2[hh][:].rearrange("p (k t) -> p k t", k=4)),
                         reads=[pT2[hh]], writes=[hTf])
                    S.op("act", lambda e, hh=hh: e.activation(out=hs[:, hh * 4:(hh + 1) * 4, tt * 128:(tt + 1) * 128],
                                                              in_=hTf[:, hh * 4:(hh + 1) * 4, :], func=AF.Copy),
                         reads=[hTf], writes=[hs])
                pl = pz.next()
                for k in range(8):
                    S.op("pe", lambda e, k=k: e.matmul(pl[:, 0:16], lhsT=hTf[:, k, :], rhs=wrt[:, k, :], start=(k == 0), stop=(k == 7)),
                         reads=[hTf, wrt], writes=[pl])
                sc = r[:, 0, :]; sel = r[:, 1, :]; w1 = r[:, 2, :]; w2 = r[:, 3, :]; w3 = r[:, 4, :]; msk = r[:, 5, :]; w4 = r[:, 6, :]; sm = r[:, 7, :]

                def V(fn, rd=()):
                    S.op("dve", fn, reads=[r] + list(rd), writes=[r])
                g4 = lambda ap: ap.rearrange("p (g i) -> p g i", g=4)
                S.op("act", lambda e: e.activation(out=sc, in_=pl[:, 0:16], func=AF.Sigmoid), reads=[pl], writes=[r])
                V(lambda e: e.tensor_tensor(out=sel, in0=sc, in1=rbias[:], op=ALU.add), [rbias])
                V(lambda e: e.tensor_reduce(out=sm[:, 0:4], in_=g4(sel), axis=AX.X, op=ALU.max))
                V(lambda e: e.tensor_tensor(out=g4(w1), in0=g4(sel), in1=sm[:, 0:4].unsqueeze(2).to_broadcast([128, 4, 4]), op=ALU.is_equal))
                V(lambda e: e.scalar_tensor_tensor(out=w2, in0=w1, scalar=-BIG, in1=sel, op0=ALU.mult, op1=ALU.add))
                V(lambda e: e.tensor_reduce(out=sm[:, 4:8], in_=g4(w2), axis=AX.X, op=ALU.max))
                V(lambda e: e.tensor_tensor(out=sm[:, 8:12], in0=sm[:, 0:4], in1=sm[:, 4:8], op=ALU.add))
                V(lambda e: e.tensor_reduce(out=sm[:, 12:13], in_=sm[:, 8:12], axis=AX.X, op=ALU.max))
                V(lambda e: e.tensor_tensor(out=sm[:, 4:8], in0=sm[:, 8:12], in1=sm[:, 12:13].to_broadcast([128, 4]), op=ALU.is_equal))
                V(lambda e: e.tensor_scalar(out=g4(w1), in0=sm[:, 4:8].unsqueeze(2).to_broadcast([128, 4, 4]), scalar1=-1.0, scalar2=BIG,
                                            op0=ALU.add, op1=ALU.mult))
                V(lambda e: e.tensor_tensor(out=w2, in0=w1, in1=sel, op=ALU.add))
                V(lambda e: e.tensor_reduce(out=sm[:, 13:14], in_=w2, axis=AX.X, op=ALU.max))
                V(lambda e: e.tensor_tensor(out=w3, in0=w2, in1=sm[:, 13:14].to_broadcast([128, 16]), op=ALU.is_equal))
                V(lambda e: e.scalar_tensor_tensor(out=w4, in0=w3, scalar=-BIG, in1=w2, op0=ALU.mult, op1=ALU.add))
                V(lambda e: e.tensor_reduce(out=sm[:, 14:15], in_=w4, axis=AX.X, op=ALU.max))
                V(lambda e: e.tensor_tensor(out=msk, in0=w4, in1=sm[:, 14:15].to_broadcast([128, 16]), op=ALU.is_equal))
                V(lambda e: e.tensor_tensor(out=msk, in0=msk, in1=w3, op=ALU.add))
                V(lambda e: e.tensor_tensor(out=w1, in0=sc, in1=msk, op=ALU.mult))
                V(lambda e: e.tensor_reduce(out=sm[:, 15:16], in_=w1, axis=AX.X, op=ALU.add))
                V(lambda e: e.reciprocal(out=sm[:, 15:16], in_=sm[:, 15:16]))
                V(lambda e: e.tensor_scalar(out=w2, in0=w1, scalar1=sm[:, 15:16], scalar2=None, op0=ALU.mult))
                pc = pg.next()
                S.op("pe", lambda e: e.matmul(pc[0:16, 0:128], lhsT=w2, rhs=ident[:], start=True, stop=True), reads=[r, ident], writes=[pc])
                S.op("act", lambda e: e.activation(out=cs[:, tt * 128:(tt + 1) * 128], in_=pc[0:16, 0:128], func=AF.Copy), reads=[pc], writes=[cs])

            if tb + 1 < NB:
                load_yh(tb + 1)
            hf0 = stageA(0)
            hf1 = stageA(1)
            stageB(0, hf0)
            hf2 = stageA(2)
            stageB(1, hf1)
            hf3 = stageA(3)
            stageB(2, hf2)
            stageB(3, hf3)
            S.dma("sp", "st" + hs.name, lambda e, hs=hs, tb=tb: e.dma_start(out=h2T_d[:, :, tb * 512:(tb + 1) * 512].rearrange("k p t -> p k t"), in_=hs[:]),
                  reads=[hs], writes=[dh2])
            S.dma("sp", "st" + cs.name, lambda e, cs=cs, tb=tb: e.dma_start(out=combT_d[:, tb * 512:(tb + 1) * 512], in_=cs[:]), reads=[cs], writes=[dcb])
        P.close()

    def phase_moe1(l):
        P = Phase(nc); S = P.S
        h2T = P.sb([128, 8, T], BF16, "h2T")
        for k in range(8):
            S.dma("sp", "h2T%d" % k, lambda e, k=k: e.dma_start(out=h2T[:, k, :], in_=h2T_d[k]), writes=[h2T])
        combT = P.sb([16, T], F32, "combT")
        S.dma("sp", "cT", lambda e: e.dma_start(out=combT[:], in_=combT_d), writes=[combT])
        sel = P.sb([16, 16 * 128], F32, "sel")
        S.dma("sp", "sel", lambda e: e.dma_start(out=sel[:], in_=C_sel), writes=[sel])
        wgr = P.ring(2, [128, 8, 512], BF16, "wg"); wur = P.ring(2, [128, 8, 512], BF16, "wu")
        pg = P.ring(3, [128, 512], F32, "pg", psum=True); pu = P.ring(3, [128, 512], F32, "pu", psum=True)
        pc = P.ring(2, [128, 512], F32, "pc", psum=True)
        cbr = P.ring(2, [128, 512], F32, "cb"); sgr = P.ring(3, [128, 512], F32, "sg"); tr = P.ring(3, [128, 512], F32, "t")
        her = P.ring(2, [128, 4, 512], BF16, "he")
        dhe = Buf(HE_d, "HE_d")
        for ex in range(16):
            wg_ = wgr.next(); wu_ = wur.next()
            S.dma("pool", wg_.name, lambda e, wg_=wg_, ex=ex: e.dma_start(out=wg_[:], in_=I["w_exp_gate"][l, ex].rearrange("(k p) n -> p k n", p=128)), writes=[wg_])
            S.dma("pool", wu_.name, lambda e, wu_=wu_, ex=ex: e.dma_start(out=wu_[:], in_=I["w_exp_up"][l, ex].rearrange("(k p) n -> p k n", p=128)), writes=[wu_])
            for tb in range(NB):
                p_c = pc.next(); cb = cbr.next(); he = her.next()
                S.op("pe", lambda e, p_c=p_c, ex=ex, tb=tb: e.matmul(p_c[:], lhsT=sel[:, ex * 128:(ex + 1) * 128], rhs=combT[:, tb * 512:(tb + 1) * 512],
                                                                      start=True, stop=True), reads=[sel, combT], writes=[p_c])
                S.op("act", lambda e, p_c=p_c, cb=cb: e.activation(out=cb[:], in_=p_c[:], func=AF.Copy), reads=[p_c], writes=[cb])
                for fc in range(4):
                    g = pg.next(); u = pu.next(); sg = sgr.next(); t = tr.next()
                    for k in range(8):
                        S.op("pe", lambda e, g=g, k=k, fc=fc, tb=tb, wg_=wg_: e.matmul(g[:], lhsT=wg_[:, k, fc * 128:(fc + 1) * 128], rhs=h2T[:, k, tb * 512:(tb + 1) * 512],
                                                                                     start=(k == 0), stop=(k == 7)), reads=[wg_, h2T], writes=[g])
                    for k in range(8):
                        S.op("pe", lambda e, u=u, k=k, fc=fc, tb=tb, wu_=wu_: e.matmul(u[:], lhsT=wu_[:, k, fc * 128:(fc + 1) * 128], rhs=h2T[:, k, tb * 512:(tb + 1) * 512],
                                                                                     start=(k == 0), stop=(k == 7)), reads=[wu_, h2T], writes=[u])
                    S.op("act", lambda e, g=g, sg=sg: e.activation(out=sg[:], in_=g[:], func=AF.Silu), reads=[g], writes=[sg])
                    S.op("dve", lambda e, u=u, sg=sg, t=t: e.tensor_tensor(out=t[:], in0=u[:], in1=sg[:], op=ALU.mult), reads=[u, sg], writes=[t])
                    S.op("dve", lambda e, t=t, cb=cb, he=he, fc=fc: e.tensor_tensor(out=he[:, fc, :], in0=t[:], in1=cb[:], op=ALU.mult), reads=[t, cb], writes=[he])
                for a4 in range(4):
                    S.dma("sp", "st" + he.name, lambda e, he=he, ex=ex, tb=tb, a4=a4: e.dma_start(
                        out=HE_d[tb * 4 + a4, :, ex * 4:(ex + 1) * 4, :],
                        in_=he[:, :, a4 * 128:(a4 + 1) * 128]), reads=[he], writes=[dhe])
        P.close()

    def phase_moe2(l):
        P = Phase(nc); S = P.S
        last = (l == L - 1)
        ident = P.sb([128, 128], BF16, "ident")
        S.dma("sp", "id", lambda e: e.dma_start(out=ident[:], in_=C_ident), writes=[ident])
        wd = P.sb([128, 64, D], BF16, "wd")
        for i in range(16):
            S.dma("pool", "wd", lambda e, i=i: e.dma_start(out=wd[:, 4 * i:4 * i + 4, :],
                                                            in_=I["w_exp_down"][l][i * 512:(i + 1) * 512, :].rearrange("(k p) n -> p k n", p=128)), writes=[wd])
        gtB = P.sb([128, D], F32, "gtB")
        S.dma("sp", "gtB", lambda e: e.dma_start(out=gtB[:], in_=modB_d[l][:, 5120:6144]), writes=[gtB])
        AB = P.sb([128, D], F32, "AB")
        if last:
            S.dma("sp", "AB", lambda e: e.dma_start(out=AB[:], in_=I["g_final"][0, :].partition_broadcast(128)), writes=[AB])
            BB = None
        else:
            BB = P.sb([128, D], F32, "BB")
            S.dma("sp", "AB", lambda e: e.dma_start(out=AB[:], in_=modB_d[l + 1][:, 1024:2048]), writes=[AB])
            S.dma("sp", "BB", lambda e: e.dma_start(out=BB[:], in_=modB_d[l + 1][:, 0:1024]), writes=[BB])
        her = P.ring(2, [128, 64, 128], BF16, "he")
        po = [P.ps([128, 512], F32, "po") for _ in range(4)]
        pT = P.ring(2, [128, 1024], BF16, "pT", psum=True)
        xr = P.ring(2, [128, D], F32, "x"); tmp = P.ring(2, [128, D], F32, "tmp")
        junk = P.sb([128, D], BF16, "junk"); st = P.ring(2, [128, 2], F32, "st")
        hbr = P.ring(2, [128, D], BF16, "hb")
        hst = P.ring(1, [128, 8, 512], BF16, "hst")
        dxs = Buf(xs, "xs"); dh = Buf(hT_d, "hT_d")
        hcur = {"hs": None}

        loaded = {}

        def loads(t_):
            he = her.next(); xt = xr.next()
            S.dma("sp", he.name, lambda e: e.dma_start(out=he[:], in_=HE_d[t_]), writes=[he])
            S.dma("sp", xt.name, lambda e: e.dma_start(out=xt[:], in_=xs[t_ * 128:(t_ + 1) * 128, :]), writes=[xt])
            loaded[t_] = (he, xt)

        def stageA(t_):
            if t_ not in loaded:
                loads(t_)
            he, xt = loaded.pop(t_)
            if t_ + 1 < NT:
                loads(t_ + 1)
            tm = tmp.next(); s_ = st.next()
            for half in range(2):
                pp_ = po[(t_ % 2) * 2 + half]
                for c in range(64):
                    S.op("pe", lambda e, pp_=pp_, c=c, half=half: e.matmul(pp_[:], lhsT=he[:, c, :], rhs=wd[:, c, half * 512:(half + 1) * 512],
                                                                           start=(c == 0), stop=(c == 63)), reads=[he, wd], writes=[pp_])
                S.op("dve", lambda e, pp_=pp_, half=half: e.tensor_tensor(out=tm[:, half * 512:(half + 1) * 512], in0=pp_[:],
                                                                          in1=gtB[:, half * 512:(half + 1) * 512], op=ALU.mult), reads=[pp_, gtB], writes=[tm])
            S.op("pool", lambda e: e.tensor_tensor(out=xt[:], in0=tm[:], in1=xt[:], op=ALU.add), reads=[tm, xt], writes=[xt])
            if not last:
                S.dma("sp", "st" + xt.name, lambda e: e.dma_start(out=xs[t_ * 128:(t_ + 1) * 128, :], in_=xt[:]), reads=[xt], writes=[dxs])
            norm_tile(S, xt, AB, BB, junk, s_, tm)
            if last:
                S.dma("sp", "st" + tm.name, lambda e: e.dma_start(out=out_d[t_ * 128:(t_ + 1) * 128, :], in_=tm[:]), reads=[tm])
                return None
            hb = hbr.next()
            S.op("pool", lambda e: e.tensor_tensor(out=hb[:], in0=tm[:], in1=BB[:], op=ALU.add), reads=[tm, BB], writes=[hb])
            return hb

        def stageB(t_, hb):
            if hb is None:
                return
            if t_ % 4 == 0:
                hcur["hs"] = hst.next()
            hs = hcur["hs"]
            p = pT.next()
            transpose_store(S, P, hb, p, hs, (t_ % 4) * 128, ident)
            if t_ % 4 == 3:
                tb = t_ // 4
                S.dma("sp", "st" + hs.name, lambda e: e.dma_start(
                    out=hT_d[:, :, tb * 512:(tb + 1) * 512].rearrange("k p t -> p k t"), in_=hs[:]), reads=[hs], writes=[dh])

        prev = None
        for t_ in range(NT):
            hb = stageA(t_)
            if prev is not None:
                stageB(*prev)
            prev = (t_, hb)
        stageB(*prev)
        P.close()

    plist = []
    for l in range(L):
        plist.append(lambda l=l: phase_mod(l))
        plist.append(lambda l=l: phase_lam(l))
    plist.append(lambda: phase_n1(0, I["x"]))
    for l in range(L):
        plist.append(lambda l=l: phase_prj(l))
        for kind in ("A", "B", "C"):
            plist.append(lambda l=l, kind=kind: phase_att(l, kind))
        plist.append(lambda l=l: phase_mrg(l, I["x"] if l == 0 else xs))
        plist.append(lambda l=l: phase_moe1(l))
        plist.append(lambda l=l: phase_moe2(l))
    for i, f in enumerate(plist):
        if stop is not None and i >= stop:
            break
        f()
    return nc


def _consts():
    bf = ml_dtypes.bfloat16
    ident = np.eye(128, dtype=np.float32)
    anti = ident[::-1].copy()
    sel = np.zeros((16, 16, 128), np.float32)
    for e in range(16):
        sel[e, e, :] = 1.0
    pos = np.arange(T, dtype=np.float32)
    inv_freq = (1.0 / (10000.0 ** (np.arange(0, 32, 2, dtype=np.float32) / 32.0))).astype(np.float32)
    ang = (pos[:, None] * inv_freq[None, :]).astype(np.float32)
    cos = np.cos(ang.astype(np.float64)).astype(np.float32); sin = np.sin(ang.astype(np.float64)).astype(np.float32)
    cos32 = np.concatenate([cos, cos], axis=1).T
    sin32 = np.concatenate([-sin, sin], axis=1).T
    cos128 = np.tile(cos32, (4, 1)).astype(np.float32)
    sin128 = np.tile(sin32, (4, 1)).astype(np.float32)
    slopes = np.exp2(-8.0 / 4 * np.arange(1, 5, dtype=np.float64))
    ipos = np.arange(T)
    a_hi = (ipos // 64) * 64.0
    a_lo = (ipos % 64) * 1.0
    augq = np.zeros((4, 4, T), np.float64); augk = np.zeros((4, 4, T), np.float64)
    for h in range(4):
        s8 = slopes[h] * 8.0
        augq[h, 0] = -s8 * a_hi; augq[h, 1] = -s8 * a_lo; augq[h, 2] = 1.0; augq[h, 3] = 1.0
        augk[h, 0] = 1.0; augk[h, 1] = 1.0; augk[h, 2] = s8 * a_hi; augk[h, 3] = s8 * a_lo
    kk = np.arange(64)[:, None]; qq = np.arange(64)[None, :]
    corr = np.zeros((4, 128, 64), np.float64)
    for h in range(4):
        c = np.exp(-2.0 * slopes[h] * np.maximum(kk - qq, 0))
        corr[h, 0:64] = c; corr[h, 64:128] = c
    return {
        "k_ident": ident.astype(bf), "k_identf": ident, "k_anti": anti.astype(bf),
        "k_sel": sel.reshape(16, 16 * 128), "k_cos": np.ascontiguousarray(cos128), "k_sin": np.ascontiguousarray(sin128),
        "k_augq": augq.astype(np.float32).astype(bf), "k_augk": augk.astype(np.float32).astype(bf),
        "k_corr": corr.astype(np.float32),
    }


_NC_CACHE = {}


def kernel(**inputs):
    if "nc" not in _NC_CACHE:
        _NC_CACHE["nc"] = build_program()
    nc = _NC_CACHE["nc"]
    consts = _consts()
    shared = {}
    for k, v in inputs.items():
        if k in ("x", "c"):
            continue
        a = np.ascontiguousarray(np.asarray(v, dtype=np.float32))
        if k == "router_bias":
            a = a.reshape(1, 16)
        elif k == "g_final":
            a = a.reshape(1, D)
        elif k == "w_exp_down":
            a = a.reshape(L, 16 * 512, D)
        shared[k] = a
    shared.update(consts)
    x = np.asarray(inputs["x"], dtype=np.float32)
    c = np.asarray(inputs["c"], dtype=np.float32)
    real_cores = [0, 1, 4, 5]
    idle = {k: np.zeros_like(v) for k, v in shared.items() if not k.startswith("k_")}
    idle.update(consts)
    idle["x"] = np.zeros((T, D), np.float32)
    idle["c"] = np.zeros((128, 8), np.float32)
    in_maps = []
    for core in range(8):
        if core in real_cores:
            b = real_cores.index(core)
            m = dict(shared)
            m["x"] = np.ascontiguousarray(x[b])
            m["c"] = np.ascontiguousarray(c[b].reshape(8, 128).T)
        else:
            m = idle
        in_maps.append(m)
    res = run_bass_kernel_spmd(nc, in_maps, core_ids=list(range(8)))
    out = np.stack([np.asarray(res.results[cid]["out"], dtype=np.float32) for cid in real_cores], axis=0)
    return out
```

```python
import math
import numpy as np
import ml_dtypes
from contextlib import ExitStack
import concourse.bass as bass
import concourse.mybir as mybir
from concourse.bass_utils import run_bass_kernel_spmd

F32 = mybir.dt.float32
BF16 = mybir.dt.bfloat16
AF = mybir.ActivationFunctionType
ALU = mybir.AluOpType
AX = mybir.AxisListType

T = 4096
D = 1024
L = 2
NT = T // 128
NB = T // 512
EPS = 1e-6
INW = 6816
BIG = 30000.0
N1_LIMIT = NB


class Buf:
    __slots__ = ("t", "w", "r", "name")

    def __init__(self, t, name=""):
        self.t = t
        self.w = None
        self.r = {}
        self.name = name

    def __getitem__(self, k):
        return self.t[k]


class Sched:
    ENG = ("pe", "act", "dve", "pool", "sp")
    SAME_ENGINE_SYNC = True
    NO_SELF_SYNC = ("pe",)

    def __init__(self, nc):
        self.nc = nc
        self.prog = {e: [] for e in self.ENG}
        self.seq = {e: 0 for e in self.ENG}
        self.last = {e: 0 for e in self.ENG}
        self.dma_cnt = {}
        self.waited = {e: {} for e in self.ENG}
        self.fence = {e: {} for e in self.ENG}
        self.needed = {e: set() for e in self.ENG}

    def _deps(self, eng, reads, writes):
        d = dict(self.fence[eng])
        self.fence[eng] = {}

        def add(tok):
            k, v = tok
            if d.get(k, 0) < v:
                d[k] = v
        for b in reads:
            if b.w is not None:
                add(b.w)
        for b in writes:
            if b.w is not None:
                add(b.w)
            for k, v in b.r.items():
                add((k, v))
        out = []
        for k, v in d.items():
            if k == eng and (eng in self.NO_SELF_SYNC or not self.SAME_ENGINE_SYNC):
                continue
            if self.waited[eng].get(k, 0) >= v:
                continue
            self.waited[eng][k] = v
            out.append((k, v))
            if k in self.needed:
                self.needed[k].add(v)
        return out

    def _mark(self, tok, reads, writes):
        k, v = tok
        for b in reads:
            if b.r.get(k, 0) < v:
                b.r[k] = v
        for b in writes:
            b.w = tok
            b.r = {}

    def op(self, eng, fn, reads=(), writes=()):
        waits = self._deps(eng, reads, writes)
        self.seq[eng] += 1
        tok = (eng, self.seq[eng])
        self.last[eng] = self.seq[eng]
        self.prog[eng].append((waits, fn, tok, None))
        self._mark(tok, reads, writes)

    def dma(self, q, key, fn, reads=(), writes=()):
        waits = self._deps(q, reads, writes)
        n = self.dma_cnt.get(key, 0) + 1
        self.dma_cnt[key] = n
        tok = (key, n)
        self.prog[q].append((waits, fn, None, key))
        self._mark(tok, reads, writes)

    def barrier(self):
        toks = {}
        for e in self.ENG:
            if self.last[e] > 0:
                toks[e] = self.last[e]
        for k, n in self.dma_cnt.items():
            toks[k] = n
        for e in self.ENG:
            f = self.fence[e]
            for k, v in toks.items():
                if f.get(k, 0) < v:
                    f[k] = v

    def emit(self):
        nc = self.nc
        self.barrier()
        for e in self.ENG:
            waits = self._deps(e, (), ())
            if waits:
                self.prog[e].append((waits, None, None, None))
        sems = {}
        handles = []
        for e in self.ENG:
            _UID[0] += 1
            sems[e] = nc.alloc_semaphore(name="s%d_%s" % (_UID[0], e))
            handles.append(sems[e])
        for i, k in enumerate(self.dma_cnt):
            _UID[0] += 1
            sems[k] = nc.alloc_semaphore(name="d%d_%d" % (_UID[0], i))
            handles.append(sems[k])
        rank = {e: {v: i + 1 for i, v in enumerate(sorted(self.needed[e]))}
                for e in self.ENG}
        ENGSET = set(self.ENG)

        def run(ename, e):
            rk = rank[ename]
            for (waits, fn, tok, key) in self.prog[ename]:
                for (k, v) in waits:
                    e.wait_ge(sems[k], rank[k][v] if k in ENGSET else 16 * v)
                if fn is None:
                    continue
                ins = fn(e)
                if key is not None:
                    ins.then_inc(sems[key], 16)
                elif tok[1] in rk:
                    ins.then_inc(sems[ename], 1)

        for h in handles:
            nc.gpsimd.sem_clear(h)
        nc.all_engine_barrier()
        with nc.Block() as block:
            block.tensor(lambda e: run("pe", e))
            block.scalar(lambda e: run("act", e))
            block.vector(lambda e: run("dve", e))
            block.gpsimd(lambda e: run("pool", e))
            block.sync(lambda e: run("sp", e))
        nc.clear_and_free_semaphores(handles)
        nc.all_engine_barrier()


class Ring:
    def __init__(self, bufs):
        self.bufs = bufs
        self.i = 0

    def next(self):
        b = self.bufs[self.i % len(self.bufs)]
        self.i += 1
        return b


_UID = [0]


class Phase:
    def __init__(self, nc):
        self.nc = nc
        self.es = ExitStack()
        self.S = Sched(nc)
        self.n = 0

    def sb(self, shape, dt, name=None):
        _UID[0] += 1
        name = "%s_%d" % (name or "t", _UID[0])
        return Buf(self.es.enter_context(self.nc.sbuf_tensor(name, shape, dt)), name)

    def ps(self, shape, dt, name=None):
        _UID[0] += 1
        name = "%s_%d" % (name or "p", _UID[0])
        return Buf(self.es.enter_context(self.nc.psum_tensor(name, shape, dt)), name)

    def ring(self, n, shape, dt, name, psum=False):
        return Ring([(self.ps if psum else self.sb)(shape, dt, name) for _ in range(n)])

    def close(self):
        self.S.emit()
        self.es.close()


def build_program(stop=None, debug=False):
    nc = bass.Bass("TRN2", target_bir_lowering=False)

    def din(name, shape, dt=F32):
        return nc.dram_tensor(name, list(shape), dt, kind="ExternalInput").ap()

    def dscr(name, shape, dt=BF16):
        if debug:
            return nc.dram_tensor(name, list(shape), dt, kind="ExternalOutput").ap()
        return nc.dram_tensor(name, list(shape), dt).ap()

    I = {}
    I["x"] = din("x", [T, D])
    I["c"] = din("c", [128, 8])
    I["w_mod"] = din("w_mod", [L, D, 6 * D])
    I["b_mod"] = din("b_mod", [L, 6 * D])
    I["g_norm_mix"] = din("g_norm_mix", [L, D])
    I["g_norm_ffn"] = din("g_norm_ffn", [L, D])
    I["w_in"] = din("w_in", [L, D, INW])
    for nm in ("diff_lambda_q1", "diff_lambda_k1", "diff_lambda_q2", "diff_lambda_k2"):
        I[nm] = din(nm, [L, 64])
    I["diff_subln_g"] = din("diff_subln_g", [L, 128])
    I["chunk_rel_bias"] = din("chunk_rel_bias", [L, 8, 513])
    I["mla_q_norm_g"] = din("mla_q_norm_g", [L, 384])
    I["mla_w_q_b"] = din("mla_w_q_b", [L, 384, 768])
    I["mla_kv_norm_g"] = din("mla_kv_norm_g", [L, 256])
    I["mla_w_kv_b"] = din("mla_w_kv_b", [L, 256, 1024])
    I["w_branch_diff"] = din("w_branch_diff", [L, 512, D])
    I["w_branch_chunk"] = din("w_branch_chunk", [L, 512, D])
    I["w_branch_mla"] = din("w_branch_mla", [L, 512, D])
    I["w_out"] = din("w_out", [L, D, D])
    I["w_router"] = din("w_router", [D, 16])
    I["router_bias"] = din("router_bias", [1, 16])
    I["w_exp_gate"] = din("w_exp_gate", [L, 16, D, 512])
    I["w_exp_up"] = din("w_exp_up", [L, 16, D, 512])
    I["w_exp_down"] = din("w_exp_down", [L, 16 * 512, D])
    I["g_final"] = din("g_final", [1, D])
    C_ident = din("k_ident", [128, 128], BF16)
    C_identf = din("k_identf", [128, 128], F32)
    C_anti = din("k_anti", [128, 128], BF16)
    C_sel = din("k_sel", [16, 16 * 128], F32)
    C_cos = din("k_cos", [128, T], F32)
    C_sin = din("k_sin", [128, T], F32)
    C_augq = din("k_augq", [4, 4, T], BF16)
    C_augk = din("k_augk", [4, 4, T], BF16)
    C_corr = din("k_corr", [4, 128, 64], F32)
    out_d = nc.dram_tensor("out", [T, D], F32, kind="ExternalOutput").ap()

    xs = dscr("xs", [T, D], F32)
    modB_d = dscr("modB", [L, 128, 6 * D], F32)
    hT_d = dscr("hT", [8, 128, T])
    qA = dscr("qA", [8, 64, T]); kA = dscr("kA", [8, 64, T]); vA = dscr("vA", [T, 512])
    qB = dscr("qB", [8, 64, T]); kB = dscr("kB", [8, 64, T]); vB = dscr("vB", [T, 512])
    qC = dscr("qC", [8, 96, T]); kC = dscr("kC", [8, 96, T]); vC = dscr("vC", [T, 512])
    yT_d = dscr("yT", [12, 128, T])
    h2T_d = dscr("h2T", [8, 128, T])
    combT_d = dscr("combT", [16, T], F32)
    HE_d = dscr("HE", [NT, 128, 64, 128])
    E_d = dscr("Ebias", [8, 768], F32)
    lam_d = dscr("lam", [1, 4], F32)

    def rstd_from_ssq(S, P, st, n, lnexp=False):
        S.op("dve", lambda e: e.tensor_scalar(out=st[:, 1:2], in0=st[:, 0:1], scalar1=1.0 / n, scalar2=EPS,
                                              op0=ALU.mult, op1=ALU.add), reads=[st], writes=[st])
        if lnexp:
            S.op("act", lambda e: e.activation(out=st[:, 1:2], in_=st[:, 1:2], func=AF.Ln), reads=[st], writes=[st])
            S.op("act", lambda e: e.activation(out=st[:, 1:2], in_=st[:, 1:2], func=AF.Exp, scale=-0.5), reads=[st], writes=[st])
            return
        S.op("act", lambda e: e.activation(out=st[:, 1:2], in_=st[:, 1:2], func=AF.Sqrt), reads=[st], writes=[st])
        S.op("dve", lambda e: e.reciprocal(out=st[:, 1:2], in_=st[:, 1:2]), reads=[st], writes=[st])

    def norm_tile(S, xt, AB, BB, junk, st, tmp):
        S.op("act", lambda e: e.activation(out=junk[:], in_=xt[:], func=AF.Square, accum_out=st[:, 0:1]),
             reads=[xt], writes=[junk, st])
        rstd_from_ssq(S, None, st, D)
        S.op("dve", lambda e: e.scalar_tensor_tensor(out=tmp[:], in0=xt[:], scalar=st[:, 1:2], in1=AB[:],
                                                     op0=ALU.mult, op1=ALU.mult), reads=[xt, st, AB], writes=[tmp])

    def phase_mod(l):
        P = Phase(nc); S = P.S
        cl = P.sb([128, 8], F32, "cl"); cact = P.sb([128, 8], F32, "cact")
        cB = P.sb([128, 8, 128], F32, "cB")
        wr = P.ring(3, [128, 8, 512], F32, "wmod")
        bb = P.ring(2, [128, 512], F32, "bb")
        gb = P.ring(2, [128, 512], F32, "gb")
        ob = P.ring(2, [128, 512], F32, "ob")
        pp = P.ring(2, [128, 512], F32, "pm", psum=True)
        dmod = Buf(modB_d, "modB_d")
        S.dma("sp", "cl", lambda e: e.dma_start(out=cl[:], in_=I["c"]), writes=[cl])
        S.op("act", lambda e: e.activation(out=cact[:], in_=cl[:], func=AF.Silu), reads=[cl], writes=[cact])
        for k in range(8):
            S.op("dve", lambda e, k=k: e.tensor_copy(out=cB[:, k, :], in_=cact[:, k:k + 1].to_broadcast([128, 128])),
                 reads=[cact], writes=[cB])
        for j in range(12):
            w = wr.next(); b = bb.next(); o = ob.next(); p = pp.next()
            S.dma("sp", w.name, lambda e, w=w, j=j: e.dma_start(
                out=w[:], in_=I["w_mod"][l][:, j * 512:(j + 1) * 512].rearrange("(k p) n -> p k n", p=128)), writes=[w])
            S.dma("sp", b.name, lambda e, b=b, j=j: e.dma_start(
                out=b[:], in_=I["b_mod"][l, j * 512:(j + 1) * 512].partition_broadcast(128)), writes=[b])
            for k in range(8):
                S.op("pe", lambda e, w=w, p=p, k=k: e.matmul(p[:], lhsT=cB[:, k, :], rhs=w[:, k, :],
                                                               start=(k == 0), stop=(k == 7)), reads=[cB, w], writes=[p])
            S.op("dve", lambda e, o=o, p=p, b=b: e.tensor_tensor(out=o[:], in0=p[:], in1=b[:], op=ALU.add),
                 reads=[p, b], writes=[o])
            if j // 2 in (1, 4):
                g = gb.next()
                gsrc = I["g_norm_mix"] if j // 2 == 1 else I["g_norm_ffn"]
                c0 = (j % 2) * 512
                S.dma("sp", g.name, lambda e, g=g, gsrc=gsrc, c0=c0: e.dma_start(
                    out=g[:], in_=gsrc[l, c0:c0 + 512].partition_broadcast(128)), writes=[g])
                S.op("dve", lambda e, o=o, g=g: e.scalar_tensor_tensor(out=o[:], in0=o[:], scalar=1.0, in1=g[:],
                                                                        op0=ALU.add, op1=ALU.mult), reads=[o, g], writes=[o])
            S.dma("sp", "st" + o.name, lambda e, o=o, j=j: e.dma_start(out=modB_d[l][:, j * 512:(j + 1) * 512], in_=o[:]),
                  reads=[o], writes=[dmod])
        P.close()

    def phase_lam(l):
        P = Phase(nc); S = P.S
        a = P.sb([1, 4, 64], F32, "lama"); pr = P.sb([1, 2, 64], F32, "lampr"); s2 = P.sb([1, 4], F32, "lams")
        for i, nm in enumerate(("diff_lambda_q1", "diff_lambda_k1", "diff_lambda_q2", "diff_lambda_k2")):
            S.dma("sp", "la%d" % i, lambda e, i=i, nm=nm: e.dma_start(out=a[:, i, :], in_=I[nm][l:l + 1, :]), writes=[a])
        S.op("dve", lambda e: e.tensor_tensor(out=pr[:, 0, :], in0=a[:, 0, :], in1=a[:, 1, :], op=ALU.mult), reads=[a], writes=[pr])
        S.op("dve", lambda e: e.tensor_tensor(out=pr[:, 1, :], in0=a[:, 2, :], in1=a[:, 3, :], op=ALU.mult), reads=[a, pr], writes=[pr])
        S.op("dve", lambda e: e.tensor_reduce(out=s2[:, 0:2], in_=pr[:], axis=AX.X, op=ALU.add), reads=[pr], writes=[s2])
        S.op("act", lambda e: e.activation(out=s2[:, 0:2], in_=s2[:, 0:2], func=AF.Exp), reads=[s2], writes=[s2])
        lam_init = 0.8 - 0.6 * math.exp(-0.3 * l)
        S.op("dve", lambda e: e.tensor_tensor(out=s2[:, 2:3], in0=s2[:, 0:1], in1=s2[:, 1:2], op=ALU.subtract), reads=[s2], writes=[s2])
        S.op("dve", lambda e: e.tensor_scalar(out=s2[:, 3:4], in0=s2[:, 2:3], scalar1=lam_init, scalar2=-1.0,
                                              op0=ALU.add, op1=ALU.mult), reads=[s2], writes=[s2])
        dl = Buf(lam_d, "lam_d")
        S.dma("sp", "lst", lambda e: e.dma_start(out=lam_d[0:1, l:l + 1], in_=s2[:, 3:4]), reads=[s2], writes=[dl])
        P.close()

    def transpose_store(S, P, hb, pT, hst, col, ident):
        for k in range(8):
            S.op("pe", lambda e, k=k: e.transpose(pT[:, k * 128:(k + 1) * 128], hb[:, k * 128:(k + 1) * 128], ident[:]),
                 reads=[hb, ident], writes=[pT])
        S.op("act", lambda e: e.activation(out=hst[:, :, col:col + 128],
                                           in_=pT[:].rearrange("p (k t) -> p k t", k=8), func=AF.Copy),
             reads=[pT], writes=[hst])

    def phase_n1(l, src):
        P = Phase(nc); S = P.S
        ident = P.sb([128, 128], BF16, "ident")
        AB = P.sb([128, D], F32, "AB"); BB = P.sb([128, D], F32, "BB")
        xr = P.ring(2, [128, D], F32, "x")
        junk = P.sb([128, D], BF16, "junk"); st = P.ring(2, [128, 2], F32, "st")
        tmp = P.ring(2, [128, D], F32, "tmp"); hbr = P.ring(2, [128, D], BF16, "hb")
        pT = P.ring(2, [128, 1024], BF16, "pT", psum=True)
        hst = P.ring(2, [128, 8, 512], BF16, "hst")
        dh = Buf(hT_d, "hT_d")
        S.dma("sp", "id", lambda e: e.dma_start(out=ident[:], in_=C_ident), writes=[ident])
        S.dma("sp", "AB", lambda e: e.dma_start(out=AB[:], in_=modB_d[l][:, 1024:2048]), writes=[AB])
        S.dma("sp", "BB", lambda e: e.dma_start(out=BB[:], in_=modB_d[l][:, 0:1024]), writes=[BB])
        for tb in range(N1_LIMIT):
            hs = hst.next()
            for tt in range(4):
                t = tb * 4 + tt
                xt = xr.next(); s_ = st.next(); tm = tmp.next(); hb = hbr.next(); p = pT.next()
                S.dma("sp", xt.name, lambda e, xt=xt, t=t: e.dma_start(out=xt[:], in_=src[t * 128:(t + 1) * 128, :]), writes=[xt])
                norm_tile(S, xt, AB, BB, junk, s_, tm)
                S.op("pool", lambda e, hb=hb, tm=tm: e.tensor_tensor(out=hb[:], in0=tm[:], in1=BB[:], op=ALU.add),
                     reads=[tm, BB], writes=[hb])
                transpose_store(S, P, hb, p, hs, tt * 128, ident)
            S.dma("sp", "st" + hs.name, lambda e, hs=hs, tb=tb: e.dma_start(
                out=hT_d[:, :, tb * 512:(tb + 1) * 512].rearrange("k p t -> p k t"), in_=hs[:]), reads=[hs], writes=[dh])
        P.close()

    def phase_prj(l):
        P = Phase(nc); S = P.S
        win = I["w_in"][l]
        hT = [P.sb([128, 8, 512], BF16, "hT%d" % b) for b in range(NB)]
        for b in range(NB):
            S.dma("sp", "hT%d" % b, lambda e, b=b: e.dma_start(out=hT[b][:], in_=hT_d[:, :, b * 512:(b + 1) * 512].rearrange("k p t -> p k t")),
                  writes=[hT[b]])

        def hsl(src, k, c0, n):
            if isinstance(src, list):
                b = src[c0 // 512]; o = c0 % 512
                return b[:, k, o:o + n], b
            return src[:, k, c0:c0 + n], src
        wr = P.ring(2, [128, 8, 512], BF16, "w")
        pp = P.ring(8, [128, 512], F32, "pp", psum=True)
        stg = P.ring(2, [128, T], BF16, "stg")
        stv = P.ring(2, [128, 4, 512], BF16, "stv")
        evq = [0]

        def evac(dst_ap, p, dstbuf):
            evq[0] += 1
            if evq[0] % 2:
                S.op("act", lambda e: e.activation(out=dst_ap, in_=p[:], func=AF.Copy), reads=[p], writes=[dstbuf])
            else:
                S.op("dve", lambda e: e.tensor_copy(out=dst_ap, in_=p[:]), reads=[p], writes=[dstbuf])

        def load_w(c0, n):
            w = wr.next()
            S.dma("pool", w.name, lambda e: e.dma_start(
                out=w[:, :, 0:n], in_=win[:, c0:c0 + n].rearrange("(k p) n -> p k n", p=128)), writes=[w])
            return w

        def fm_group(w, wc, dsts):
            sg = stg.next()
            for tb in range(NB):
                p = pp.next()
                for k in range(8):
                    S.op("pe", lambda e, p=p, k=k, tb=tb: e.matmul(p[:], lhsT=w[:, k, wc:wc + 128],
                                                                   rhs=hT[tb][:, k, :],
                                                                   start=(k == 0), stop=(k == 7)), reads=[w, hT[tb]], writes=[p])
                evac(sg[:, tb * 512:(tb + 1) * 512], p, sg)
            for i, (dap, r0, nr) in enumerate(dsts):
                S.dma("sp", "st%s_%d" % (sg.name, i), lambda e, dap=dap, r0=r0, nr=nr: e.dma_start(out=dap, in_=sg[r0:r0 + nr, :]),
                      reads=[sg])

        def tm_group(w, dst, hsrc, nk):
            for t4 in range(NT // 4):
                sv = stv.next()
                for tt in range(4):
                    t = t4 * 4 + tt
                    p = pp.next()
                    for k in range(nk):
                        S.op("pe", lambda e, p=p, k=k, t=t: e.matmul(p[:], lhsT=hsl(hsrc, k, t * 128, 128)[0], rhs=w[:, k, 0:512],
                                                                     start=(k == 0), stop=(k == nk - 1)), reads=[w, hsl(hsrc, k, t * 128, 128)[1]], writes=[p])
                    evac(sv[:, tt, :], p, sv)
                S.dma("sp", "st" + sv.name, lambda e, sv=sv, t4=t4: e.dma_start(
                    out=dst[t4 * 512:(t4 + 1) * 512, :].rearrange("(a p) n -> p a n", p=128), in_=sv[:]), reads=[sv])

        for base, dst in ((0, qA), (512, kA)):
            w = load_w(base, 512)
            for g in range(4):
                fm_group(w, g * 128, [(dst[2 * g], 0, 64), (dst[2 * g + 1], 64, 64)])
        w = load_w(1024, 512); tm_group(w, vA, hT, 8)
        for base, dst in ((1536, qB), (2048, kB)):
            w = load_w(base, 512)
            for g in range(4):
                fm_group(w, g * 128, [(dst[2 * g], 0, 64), (dst[2 * g + 1], 64, 64)])
        w = load_w(2560, 512); tm_group(w, vB, hT, 8)

        onesf = P.sb([128, 128], F32, "onesf")
        S.op("pool", lambda e: e.memset(onesf[:], 1.0), writes=[onesf])
        mqn = P.sb([128, 3, T], BF16, "mqn"); ckvn = P.sb([128, 2, T], BF16, "ckvn")
        gq = P.sb([128, 3], F32, "gq"); gkv = P.sb([128, 2], F32, "gkv")
        S.dma("sp", "gq", lambda e: e.dma_start(out=gq[:], in_=I["mla_q_norm_g"][l].rearrange("(c p) -> p c", p=128),
                                                allow_slow_non_contiguous=True), writes=[gq])
        S.dma("sp", "gkv", lambda e: e.dma_start(out=gkv[:], in_=I["mla_kv_norm_g"][l].rearrange("(c p) -> p c", p=128),
                                                 allow_slow_non_contiguous=True), writes=[gkv])
        latf = P.ring(1, [128, 3, 512], F32, "latf"); latsq = P.ring(1, [128, 3, 512], F32, "latsq")
        rsB = P.ring(2, [128, 512], F32, "rsB")

        def latent(c0, nch, gvec, dstn):
            w = load_w(c0, nch * 128)
            for tb in range(NB):
                lf = latf.next(); lq = latsq.next(); rs = rsB.next()
                for c in range(nch):
                    p = pp.next()
                    for k in range(8):
                        S.op("pe", lambda e, p=p, k=k, c=c, tb=tb: e.matmul(p[:], lhsT=w[:, k, c * 128:(c + 1) * 128],
                                                                            rhs=hT[tb][:, k, :],
                                                                            start=(k == 0), stop=(k == 7)), reads=[w, hT[tb]], writes=[p])
                    S.op("act", lambda e, p=p, c=c, lf=lf: e.activation(out=lf[:, c, :], in_=p[:], func=AF.Copy), reads=[p], writes=[lf])
                    S.op("dve", lambda e, c=c, lf=lf, lq=lq: e.tensor_tensor(out=lq[:, c, :], in0=lf[:, c, :], in1=lf[:, c, :], op=ALU.mult),
                         reads=[lf], writes=[lq])
                p = pp.next()
                for c in range(nch):
                    S.op("pe", lambda e, p=p, c=c, lq=lq: e.matmul(p[:], lhsT=onesf[:], rhs=lq[:, c, :], start=(c == 0), stop=(c == nch - 1)),
                         reads=[onesf, lq], writes=[p])
                S.op("dve", lambda e, p=p, rs=rs: e.tensor_scalar(out=rs[:], in0=p[:], scalar1=1.0 / (nch * 128), scalar2=EPS,
                                                                  op0=ALU.mult, op1=ALU.add), reads=[p], writes=[rs])
                S.op("act", lambda e, rs=rs: e.activation(out=rs[:], in_=rs[:], func=AF.Sqrt), reads=[rs], writes=[rs])
                S.op("dve", lambda e, rs=rs: e.reciprocal(out=rs[:], in_=rs[:]), reads=[rs], writes=[rs])
                for c in range(nch):
                    S.op("dve", lambda e, c=c, lf=lf, rs=rs, tb=tb: e.scalar_tensor_tensor(
                        out=dstn[:, c, tb * 512:(tb + 1) * 512], in0=lf[:, c, :], scalar=gvec[:, c:c + 1], in1=rs[:],
                        op0=ALU.mult, op1=ALU.mult), reads=[lf, rs, gvec], writes=[dstn])

        latent(3072, 3, gq, mqn)
        latent(3456, 2, gkv, ckvn)

        cosr = P.ring(2, [128, 512], F32, "cos"); sinr = P.ring(2, [128, 512], F32, "sin")
        wkr = P.sb([128, 8, 64], BF16, "wkr")
        kr0 = 3456 + 256
        for (d0, s0, n) in ((0, kr0, 32), (32, kr0 + 16, 16), (48, kr0, 16)):
            S.dma("pool", "wkr%d" % d0, lambda e, d0=d0, s0=s0, n=n: e.dma_start(
                out=wkr[:, :, d0:d0 + n], in_=win[:, s0:s0 + n].rearrange("(k p) n -> p k n", p=128)), writes=[wkr])
        wqb = I["mla_w_q_b"][l]
        wkvb = I["mla_w_kv_b"][l]
        wqn = P.sb([128, 3, 512], BF16, "wqn"); wqr = P.sb([128, 3, 256], BF16, "wqr"); wqs = P.sb([128, 3, 256], BF16, "wqs")
        wkn = P.sb([128, 2, 512], BF16, "wkn"); wvc = P.sb([128, 2, 512], BF16, "wvc")

        def wsrc(ap2, c0, n):
            return ap2[:, c0:c0 + n].rearrange("(k p) n -> p k n", p=128)
        for h in range(8):
            S.dma("pool", "wqn", lambda e, h=h: e.dma_start(out=wqn[:, :, h * 64:(h + 1) * 64], in_=wsrc(wqb, h * 96, 64)), writes=[wqn])
            S.dma("pool", "wqr", lambda e, h=h: e.dma_start(out=wqr[:, :, h * 32:(h + 1) * 32], in_=wsrc(wqb, h * 96 + 64, 32)), writes=[wqr])
            S.dma("pool", "wqs", lambda e, h=h: e.dma_start(out=wqs[:, :, h * 32:h * 32 + 16], in_=wsrc(wqb, h * 96 + 80, 16)), writes=[wqs])
            S.dma("pool", "wqs", lambda e, h=h: e.dma_start(out=wqs[:, :, h * 32 + 16:h * 32 + 32], in_=wsrc(wqb, h * 96 + 64, 16)), writes=[wqs])
            S.dma("pool", "wkn", lambda e, h=h: e.dma_start(out=wkn[:, :, h * 64:(h + 1) * 64], in_=wsrc(wkvb, h * 128, 64)), writes=[wkn])
            S.dma("pool", "wvc", lambda e, h=h: e.dma_start(out=wvc[:, :, h * 64:(h + 1) * 64], in_=wsrc(wkvb, h * 128 + 64, 64)), writes=[wvc])

        t1r = P.ring(2, [128, 512], F32, "t1"); t2r = P.ring(2, [128, 512], F32, "t2")

        def rope_group(lhs_r, lhs_s, src, nk, nrows, sg):
            for tb in range(NB):
                co = cosr.next(); si = sinr.next()
                S.dma("sp", co.name, lambda e, co=co, tb=tb: e.dma_start(out=co[:], in_=C_cos[:, tb * 512:(tb + 1) * 512]), writes=[co])
                S.dma("sp", si.name, lambda e, si=si, tb=tb: e.dma_start(out=si[:], in_=C_sin[:, tb * 512:(tb + 1) * 512]), writes=[si])
                pr = pp.next(); ps_ = pp.next()
                for k in range(nk):
                    S.op("pe", lambda e, k=k, tb=tb, pr=pr: e.matmul(pr[0:nrows, :], lhsT=lhs_r(k), rhs=hsl(src, k, tb * 512, 512)[0],
                                                                     start=(k == 0), stop=(k == nk - 1)), reads=[hsl(src, k, tb * 512, 512)[1], wkr, wqr], writes=[pr])
                for k in range(nk):
                    S.op("pe", lambda e, k=k, tb=tb, ps_=ps_: e.matmul(ps_[0:nrows, :], lhsT=lhs_s(k), rhs=hsl(src, k, tb * 512, 512)[0],
                                                                       start=(k == 0), stop=(k == nk - 1)), reads=[hsl(src, k, tb * 512, 512)[1], wkr, wqs], writes=[ps_])
                t1 = t1r.next(); t2 = t2r.next()
                S.op("dve", lambda e, t1=t1, pr=pr, co=co: e.tensor_tensor(out=t1[0:nrows, :], in0=pr[0:nrows, :], in1=co[0:nrows, :], op=ALU.mult),
                     reads=[pr, co], writes=[t1])
                S.op("dve", lambda e, t2=t2, ps_=ps_, si=si: e.tensor_tensor(out=t2[0:nrows, :], in0=ps_[0:nrows, :], in1=si[0:nrows, :], op=ALU.mult),
                     reads=[ps_, si], writes=[t2])
                S.op("pool", lambda e, t1=t1, t2=t2, tb=tb: e.tensor_tensor(out=sg[0:nrows, tb * 512:(tb + 1) * 512], in0=t1[0:nrows, :],
                                                                           in1=t2[0:nrows, :], op=ALU.add), reads=[t1, t2], writes=[sg])

        sg = stg.next()
        rope_group(lambda k: wkr[:, k, 0:32], lambda k: wkr[:, k, 32:64], hT, 8, 32, sg)
        for h in range(8):
            S.dma("sp", "stkr%d" % h, lambda e, h=h, sg=sg: e.dma_start(out=kC[h, 0:32, :], in_=sg[0:32, :]), reads=[sg])
        for g in range(2):
            sg = stg.next()
            rope_group(lambda k, g=g: wqr[:, k, g * 128:(g + 1) * 128], lambda k, g=g: wqs[:, k, g * 128:(g + 1) * 128], mqn, 3, 128, sg)
            for i in range(4):
                S.dma("sp", "stqr%d" % i, lambda e, g=g, i=i, sg=sg: e.dma_start(out=qC[4 * g + i, 0:32, :], in_=sg[32 * i:32 * i + 32, :]), reads=[sg])

        def fm_group2(w, wc, src, nk, dsts):
            sg = stg.next()
            for tb in range(NB):
                p = pp.next()
                for k in range(nk):
                    S.op("pe", lambda e, p=p, k=k, tb=tb: e.matmul(p[:], lhsT=w[:, k, wc:wc + 128], rhs=src[:, k, tb * 512:(tb + 1) * 512],
                                                                   start=(k == 0), stop=(k == nk - 1)), reads=[w, src], writes=[p])
                evac(sg[:, tb * 512:(tb + 1) * 512], p, sg)
            for i, (dap, r0, nr) in enumerate(dsts):
                S.dma("sp", "st%s_%d" % (sg.name, i), lambda e, dap=dap, r0=r0, nr=nr, sg=sg: e.dma_start(out=dap, in_=sg[r0:r0 + nr, :]), reads=[sg])
        for g in range(4):
            fm_group2(wqn, g * 128, mqn, 3, [(qC[2 * g, 32:96, :], 0, 64), (qC[2 * g + 1, 32:96, :], 64, 64)])
        for g in range(4):
            fm_group2(wkn, g * 128, ckvn, 2, [(kC[2 * g, 32:96, :], 0, 64), (kC[2 * g + 1, 32:96, :], 64, 64)])
        tm_group(wvc, vC, ckvn, 2)
        P.close()

    def phase_att(l, kind):
        P = Phase(nc); S = P.S
        ident = P.sb([128, 128], BF16, "ident")
        S.dma("sp", "id", lambda e: e.dma_start(out=ident[:], in_=C_ident), writes=[ident])
        if kind == "A":
            KR, DV, NH, NM = 68, 128, 4, 2
            qd, kd, vd = qA, kA, vA
            scale = 0.125
        elif kind == "B":
            KR, DV, NH, NM = 64, 64, 8, 1
            qd, kd, vd = qB, kB, vB
            scale = 0.125
        else:
            KR, DV, NH, NM = 96, 64, 8, 1
            qd, kd, vd = qC, kC, vC
            scale = 96.0 ** -0.5
        NMAP = NH * NM
        qr = P.ring(4, [128, T], BF16, "qT"); kr = P.ring(4, [128, T], BF16, "kT")
        vr = P.ring(3, [128, NT, DV + 1], BF16, "v")
        for v in vr.bufs:
            S.op("pool", lambda e, v=v: e.memset(v[:, :, DV:DV + 1], 1.0), writes=[v])
        pS = P.ring(3, [128, 512], F32, "pS", psum=True)
        pO = [P.ps([128, 512], F32, "pO") for _ in range(4)]
        pTt = P.ps([128, 1024], BF16, "pTt")
        ptr = P.ring(4, [128, 512], BF16, "pt")
        st = P.ring(4, [128, 4], F32, "st")
        o1n = P.sb([128, 4, 128], F32, "o1n")
        af = P.ring(2, [128, 128], F32, "af")
        yb = P.ring(12, [128, 128], BF16, "yb")
        junk = P.sb([128, 128], BF16, "junk")
        yst = P.ring(2, [128, 512], BF16, "yst")
        dy = Buf(yT_d, "yT_d")
        if kind == "A":
            neglam = P.sb([128, 1], F32, "neglam")
            S.dma("sp", "nl", lambda e: e.dma_start(out=neglam[:], in_=lam_d[0, l:l + 1].partition_broadcast(128)), writes=[neglam])
            gsub = P.sb([128, 128], F32, "gsub")
            S.dma("sp", "gs", lambda e: e.dma_start(out=gsub[:], in_=I["diff_subln_g"][l, :].partition_broadcast(128)), writes=[gsub])
            lam_init = 0.8 - 0.6 * math.exp(-0.3 * l)
            S.op("dve", lambda e: e.tensor_scalar(out=gsub[:], in0=gsub[:], scalar1=1.0 - lam_init, scalar2=None, op0=ALU.mult),
                 reads=[gsub], writes=[gsub])
            corr = P.sb([128, 4, 64], F32, "corr")
            S.dma("sp", "corr", lambda e: e.dma_start(out=corr[:], in_=C_corr.rearrange("h p q -> p h q")), writes=[corr])
        if kind == "B":
            anti = P.sb([128, 128], BF16, "anti")
            S.dma("sp", "anti", lambda e: e.dma_start(out=anti[:], in_=C_anti), writes=[anti])
            dE = Buf(E_d, "E_d")
            rel = I["chunk_rel_bias"][l]
            S.dma("sp", "E1", lambda e: e.dma_start(out=E_d[:, 0:384], in_=rel[:, 129:513]), writes=[dE])
            e1 = P.sb([8, 1], F32, "e1"); e2 = P.sb([8, 384], F32, "e2")
            S.dma("sp", "E2a", lambda e: e.dma_start(out=e1[:], in_=rel[:, 512:513], allow_slow_non_contiguous=True), writes=[e1])
            S.op("dve", lambda e: e.tensor_copy(out=e2[:], in_=e1[:, 0:1].to_broadcast([8, 384])), reads=[e1], writes=[e2])
            S.dma("sp", "E2", lambda e: e.dma_start(out=E_d[:, 384:768], in_=e2[:]), reads=[e2], writes=[dE])
            bf = P.sb([128, 640], F32, "bf")
            bhi = P.sb([128, 8, 640], BF16, "bhi"); blo = P.sb([128, 8, 640], BF16, "blo")
            bh32 = P.sb([128, 640], F32, "bh32")
            for h in range(8):
                S.dma("sp", "bf", lambda e, h=h: e.dma_start(out=bf[:], in_=bass.AP(E_d.tensor, h * 768, [[1, 128], [1, 640]])),
                      reads=[dE], writes=[bf])
                S.op("dve", lambda e: e.tensor_scalar(out=bf[:], in0=bf[:], scalar1=1.0 / scale, scalar2=None, op0=ALU.mult), reads=[bf], writes=[bf])
                S.op("dve", lambda e, h=h: e.tensor_copy(out=bhi[:, h, :], in_=bf[:]), reads=[bf], writes=[bhi])
                S.op("dve", lambda e, h=h: e.tensor_copy(out=bh32[:], in_=bhi[:, h, :]), reads=[bhi], writes=[bh32])
                S.op("dve", lambda e, h=h: e.tensor_tensor(out=blo[:, h, :], in0=bf[:], in1=bh32[:], op=ALU.subtract), reads=[bf, bh32], writes=[blo])

        def load_map(m):
            h = m // NM
            q = qr.next(); k = kr.next()
            nd = 64 if kind != "C" else 96
            S.dma("sp", q.name, lambda e: e.dma_start(out=q[0:nd, :], in_=qd[m]), writes=[q])
            S.dma("sp", k.name, lambda e: e.dma_start(out=k[0:nd, :], in_=kd[m]), writes=[k])
            if kind == "A":
                S.dma("sp", q.name, lambda e: e.dma_start(out=q[64:68, :], in_=C_augq[h]), writes=[q])
                S.dma("sp", k.name, lambda e: e.dma_start(out=k[64:68, :], in_=C_augk[h]), writes=[k])
            return q, k

        def load_v(h):
            v = vr.next()
            S.dma("sp", v.name, lambda e: e.dma_start(out=v[:, :, 0:DV], in_=vd[:, h * DV:(h + 1) * DV].rearrange("(t p) d -> p t d", p=128)),
                  writes=[v])
            return v

        LA = 2
        jobs = []
        for h in range(NH):
            for Qb in range(NB):
                for c in range(NM):
                    if kind == "B":
                        kts = list(range(max(0, 4 * Qb - 4), 4 * Qb + 4))
                    else:
                        kts = list(range(0, 4 * Qb + 4))
                    for kt in kts:
                        jobs.append((h, Qb, c, kt, kt == kts[-1]))
        cur = {"h": None, "v": None, "maps": None}
        pre = {}

        def stage1(job):
            h, Qb, c, kt, last = job
            if cur["h"] != h:
                cur["h"] = h
                if h not in pre:
                    pre[h] = (load_v(h), [load_map(h * NM + cc) for cc in range(NM)])
                cur["v"], cur["maps"] = pre.pop(h)
                if h + 1 < NH:
                    pre[h + 1] = (load_v(h + 1), [load_map((h + 1) * NM + cc) for cc in range(NM)])
            v = cur["v"]
            q, k = cur["maps"][c]
            if kind == "B":
                jlo = max(kt, 4 * Qb); jhi = min(kt + 4, 4 * Qb + 3)
            else:
                jlo = max(kt, 4 * Qb); jhi = 4 * Qb + 3
            c0 = (jlo - 4 * Qb) * 128; c1 = (jhi - 4 * Qb + 1) * 128
            ps_ = pS.next(); pt = ptr.next()
            lastmm = (kind != "B")
            S.op("pe", lambda e: e.matmul(ps_[:, c0:c1], lhsT=k[0:KR, kt * 128:(kt + 1) * 128], rhs=q[0:KR, Qb * 512 + c0:Qb * 512 + c1],
                                          start=True, stop=lastmm), reads=[q, k], writes=[ps_])
            if kind == "B":
                b0 = (jlo - kt) * 128; b1 = (jhi - kt + 1) * 128
                S.op("pe", lambda e: e.matmul(ps_[:, c0:c1], lhsT=anti[:], rhs=bhi[:, h, b0:b1], start=False, stop=False), reads=[anti, bhi], writes=[ps_])
                S.op("pe", lambda e: e.matmul(ps_[:, c0:c1], lhsT=anti[:], rhs=blo[:, h, b0:b1], start=False, stop=True), reads=[anti, blo], writes=[ps_])
            S.op("act", lambda e: e.activation(out=pt[:, c0:c1], in_=ps_[:, c0:c1], func=AF.Exp, scale=scale), reads=[ps_], writes=[pt])
            if kt >= 4 * Qb:
                m0 = (kt - 4 * Qb) * 128
                if kind == "A":
                    S.op("pool", lambda e: e.tensor_tensor(out=pt[0:64, m0:m0 + 64], in0=pt[0:64, m0:m0 + 64], in1=corr[0:64, h, :], op=ALU.mult),
                         reads=[pt, corr], writes=[pt])
                    S.op("pool", lambda e: e.tensor_tensor(out=pt[64:128, m0 + 64:m0 + 128], in0=pt[64:128, m0 + 64:m0 + 128], in1=corr[64:128, h, :], op=ALU.mult),
                         reads=[pt, corr], writes=[pt])
                S.op("pool", lambda e: e.memset(pt[64:128, m0:m0 + 64], 0.0), writes=[pt])
            if kind == "B" and kt + 4 <= 4 * Qb + 3:
                m1 = (kt + 4 - 4 * Qb) * 128 + 64
                S.op("pool", lambda e: e.memset(pt[0:64, m1:m1 + 64], 0.0), writes=[pt])
            return (job, pt, v, jlo, jhi)

        def stage2(rec):
            (h, Qb, c, kt, last), pt, v, jlo, jhi = rec
            for j in range(jlo, jhi + 1):
                jj = j - 4 * Qb
                first = (kt == (max(0, j - 4) if kind == "B" else 0))
                S.op("pe", lambda e, jj=jj, first=first, j=j: e.matmul(
                    pO[jj][:, 0:DV + 1], lhsT=pt[:, jj * 128:(jj + 1) * 128], rhs=v[:, kt, :], start=first, stop=(kt == j)),
                    reads=[pt, v], writes=[pO[jj]])
            if not last:
                return None
            ys = None
            defer = []
            for jj in range(4):
                s_ = st.next()
                S.op("dve", lambda e, s_=s_, jj=jj: e.reciprocal(out=s_[:, 0:1], in_=pO[jj][:, DV:DV + 1]), reads=[pO[jj]], writes=[s_])
                if kind == "A":
                    if c == 0:
                        S.op("dve", lambda e, s_=s_, jj=jj: e.tensor_scalar(out=o1n[:, jj, :], in0=pO[jj][:, 0:DV], scalar1=s_[:, 0:1], scalar2=None,
                                                                            op0=ALU.mult), reads=[pO[jj], s_], writes=[o1n])
                        continue
                    a = af.next(); y = yb.next()
                    S.op("dve", lambda e, s_=s_: e.tensor_tensor(out=s_[:, 1:2], in0=s_[:, 0:1], in1=neglam[:], op=ALU.mult), reads=[s_, neglam], writes=[s_])
                    S.op("dve", lambda e, s_=s_, jj=jj, a=a: e.scalar_tensor_tensor(out=a[:], in0=pO[jj][:, 0:DV], scalar=s_[:, 1:2], in1=o1n[:, jj, :],
                                                                                    op0=ALU.mult, op1=ALU.add), reads=[pO[jj], s_, o1n], writes=[a])
                    s2 = st.next()
                    S.op("act", lambda e, a=a, s2=s2: e.activation(out=junk[:], in_=a[:], func=AF.Square, accum_out=s2[:, 0:1]), reads=[a], writes=[junk, s2])
                    rstd_from_ssq(S, P, s2, 128, lnexp=True)
                    S.op("dve", lambda e, a=a, s2=s2, y=y: e.scalar_tensor_tensor(out=y[:], in0=a[:], scalar=s2[:, 1:2], in1=gsub[:],
                                                                                  op0=ALU.mult, op1=ALU.mult), reads=[a, s2, gsub], writes=[y])
                    defer.append(lambda y=y, jj=jj: S.op("pe", lambda e: e.transpose(pTt[:, jj * 128:(jj + 1) * 128], y[:], ident[:]), reads=[y, ident], writes=[pTt]))
                else:
                    y = yb.next()
                    S.op("dve", lambda e, s_=s_, jj=jj, y=y: e.tensor_scalar(out=y[:, 0:DV], in0=pO[jj][:, 0:DV], scalar1=s_[:, 0:1], scalar2=None,
                                                                             op0=ALU.mult), reads=[pO[jj], s_], writes=[y])
                    defer.append(lambda y=y, jj=jj: S.op("pe", lambda e: e.transpose(pTt[0:DV, jj * 128:(jj + 1) * 128], y[:, 0:DV], ident[:]), reads=[y, ident], writes=[pTt]))
            if kind == "A":
                if c == 1:
                    def fin_a():
                        ys = yst.next()
                        S.op("act", lambda e: e.activation(out=ys[:], in_=pTt[:, 0:512], func=AF.Copy), reads=[pTt], writes=[ys])
                        S.dma("sp", "st" + ys.name, lambda e: e.dma_start(out=yT_d[h, :, Qb * 512:(Qb + 1) * 512], in_=ys[:]),
                              reads=[ys], writes=[dy])
                    defer.append(fin_a)
            else:
                def fin_bc():
                    ys2 = yst.next()
                    S.op("act", lambda e: e.activation(out=ys2[0:DV, :], in_=pTt[0:DV, 0:512], func=AF.Copy), reads=[pTt], writes=[ys2])
                    ch = (4 if kind == "B" else 8) + h // 2
                    r0 = (h % 2) * 64
                    S.dma("sp", "st" + ys2.name, lambda e: e.dma_start(
                        out=yT_d[ch, r0:r0 + 64, Qb * 512:(Qb + 1) * 512], in_=ys2[0:64, :]), reads=[ys2], writes=[dy])
                defer.append(fin_bc)
            return defer

        pend = []
        dq = []

        def tick():
            for d in dq:
                d[0] -= 1
            while dq and dq[0][0] <= 0:
                for fn in dq.pop(0)[1]:
                    fn()

        def run2(rec):
            d = stage2(rec)
            tick()
            if d:
                dq.append([3, d])

        for job in jobs:
            pend.append(stage1(job))
            if len(pend) > LA:
                run2(pend.pop(0))
        while pend:
            run2(pend.pop(0))
        while dq:
            for fn in dq.pop(0)[1]:
                fn()
        P.close()

    def phase_mrg(l, xsrc):
        P = Phase(nc); S = P.S
        ident = P.sb([128, 128], F32, "identf")
        S.dma("sp", "id", lambda e: e.dma_start(out=ident[:], in_=C_identf), writes=[ident])
        wbr = [P.sb([128, 4, D], BF16, "wbr%d" % i) for i in range(3)]
        wg = [P.sb([128, 8, 512], BF16, "wg%d" % i) for i in range(6)]

        def ld_wbr(i):
            nm = ("w_branch_diff", "w_branch_chunk", "w_branch_mla")[i]
            S.dma("pool", "wbr%d" % i, lambda e: e.dma_start(out=wbr[i][:], in_=I[nm][l].rearrange("(k p) n -> p k n", p=128)), writes=[wbr[i]])

        def ld_wg(i):
            S.dma("pool", "wg%d" % i, lambda e: e.dma_start(
                out=wg[i][:], in_=I["w_in"][l][:, 3744 + i * 512:3744 + (i + 1) * 512].rearrange("(k p) n -> p k n", p=128)), writes=[wg[i]])
        for i in range(3):
            ld_wg(2 * i); ld_wbr(i)
        for i in range(3):
            ld_wg(2 * i + 1)
        wo = P.sb([128, 8, D], BF16, "wo")
        S.dma("pool", "wo", lambda e: e.dma_start(out=wo[:], in_=I["w_out"][l].rearrange("(k p) n -> p k n", p=128)), writes=[wo])
        wrt = P.sb([128, 8, 16], F32, "wrt")
        S.dma("sp", "wrt", lambda e: e.dma_start(out=wrt[:], in_=I["w_router"].rearrange("(k p) n -> p k n", p=128)), writes=[wrt])
        rbias = P.sb([128, 16], F32, "rbias")
        S.dma("sp", "rb", lambda e: e.dma_start(out=rbias[:], in_=I["router_bias"][0, :].partition_broadcast(128)), writes=[rbias])
        gtB = P.sb([128, D], F32, "gtB"); A2 = P.sb([128, D], F32, "A2"); B2 = P.sb([128, D], F32, "B2")
        S.dma("sp", "gtB", lambda e: e.dma_start(out=gtB[:], in_=modB_d[l][:, 2048:3072]), writes=[gtB])
        S.dma("sp", "A2", lambda e: e.dma_start(out=A2[:], in_=modB_d[l][:, 4096:5120]), writes=[A2])
        S.dma("sp", "B2", lambda e: e.dma_start(out=B2[:], in_=modB_d[l][:, 3072:4096]), writes=[B2])
        yr = P.ring(2, [128, 12, 512], BF16, "y"); hr = P.ring(2, [128, 8, 512], BF16, "h")
        pz = P.ring(8, [128, 512], F32, "pz", psum=True); pg = pz
        sgr = P.ring(2, [128, 512], F32, "sg"); mr = P.ring(2, [128, 512], F32, "m"); tr = P.ring(2, [128, 512], F32, "t")
        mT = P.ring(1, [128, 8, 512], BF16, "mT")
        xr = P.ring(2, [128, D], F32, "x"); tmp = P.ring(1, [128, D], F32, "tmp")
        h2f = P.ring(2, [128, D], F32, "h2f")
        junk = P.sb([128, D], BF16, "junk"); st = P.ring(2, [128, 2], F32, "st")
        h2Tf = P.ring(2, [128, 8, 128], F32, "h2Tf")
        hst = P.ring(1, [128, 8, 512], BF16, "hst")
        rt = P.ring(4, [128, 8, 16], F32, "rt")
        cst = P.ring(2, [16, 512], F32, "cst")
        dxs = Buf(xs, "xs"); dh2 = Buf(h2T_d, "h2T_d"); dcb = Buf(combT_d, "combT_d")
        yh = {}
        xl = {}

        def load_yh(tb):
            y = yr.next(); h = hr.next()
            S.dma("sp", y.name, lambda e: e.dma_start(out=y[:], in_=yT_d[:, :, tb * 512:(tb + 1) * 512].rearrange("k p t -> p k t")), writes=[y])
            S.dma("sp", h.name, lambda e: e.dma_start(out=h[:], in_=hT_d[:, :, tb * 512:(tb + 1) * 512].rearrange("k p t -> p k t")), writes=[h])
            yh[tb] = (y, h)

        def load_x(t_):
            xt = xr.next()
            S.dma("sp", xt.name, lambda e: e.dma_start(out=xt[:], in_=xsrc[t_ * 128:(t_ + 1) * 128, :]), writes=[xt])
            xl[t_] = xt

        load_yh(0)
        tail = []
        for tb in range(NB):
            y, h = yh.pop(tb)
            m_T = mT.next(); cs = cst.next()
            for f in range(8):
                if f >= 1 and tail:
                    tail.pop(0)()
                m = mr.next()
                for br in range(3):
                    g = pg.next(); z = pz.next(); sg = sgr.next()
                    for k in range(8):
                        S.op("pe", lambda e, g=g, k=k, br=br, f=f, h=h: e.matmul(g[:], lhsT=wg[br * 2 + f // 4][:, k, (f % 4) * 128:(f % 4 + 1) * 128], rhs=h[:, k, :],
                                                                               start=(k == 0), stop=(k == 7)), reads=[wg[br * 2 + f // 4], h], writes=[g])
                    for c4 in range(4):
                        S.op("pe", lambda e, z=z, c4=c4, br=br, f=f, y=y: e.matmul(z[:], lhsT=wbr[br][:, c4, f * 128:(f + 1) * 128], rhs=y[:, br * 4 + c4, :],
                                                                                 start=(c4 == 0), stop=(c4 == 3)), reads=[wbr[br], y], writes=[z])
                    S.op("act", lambda e, g=g, sg=sg: e.activation(out=sg[:], in_=g[:], func=AF.Sigmoid), reads=[g], writes=[sg])
                    if br == 0:
                        S.op("dve", lambda e, z=z, sg=sg, m=m: e.tensor_tensor(out=m[:], in0=z[:], in1=sg[:], op=ALU.mult), reads=[z, sg], writes=[m])
                    else:
                        t = tr.next()
                        S.op("dve", lambda e, z=z, sg=sg, t=t: e.tensor_tensor(out=t[:], in0=z[:], in1=sg[:], op=ALU.mult), reads=[z, sg], writes=[t])
                        if br == 1:
                            S.op("pool", lambda e, m=m, t=t: e.tensor_tensor(out=m[:], in0=m[:], in1=t[:], op=ALU.add), reads=[m, t], writes=[m])
                        else:
                            S.op("pool", lambda e, m=m, t=t, f=f, m_T=m_T: e.tensor_tensor(out=m_T[:, f, :], in0=m[:], in1=t[:], op=ALU.add),
                                 reads=[m, t], writes=[m_T])
            while tail:
                tail.pop(0)()
            hs = hst.next()

            def stageA(tt, tb=tb, m_T=m_T):
                t_ = tb * 4 + tt
                if t_ not in xl:
                    load_x(t_)
                xt = xl.pop(t_)
                if t_ + 1 < NT:
                    load_x(t_ + 1)
                tm = tmp.next(); s_ = st.next(); hf = h2f.next()
                py = [pz.next(), pz.next()]
                for half in range(2):
                    for k in range(8):
                        S.op("pe", lambda e, half=half, k=k: e.matmul(py[half][:], lhsT=m_T[:, k, tt * 128:(tt + 1) * 128],
                                                                      rhs=wo[:, k, half * 512:(half + 1) * 512], start=(k == 0), stop=(k == 7)),
                             reads=[m_T, wo], writes=[py[half]])
                    S.op("dve", lambda e, half=half: e.tensor_tensor(out=tm[:, half * 512:(half + 1) * 512], in0=py[half][:],
                                                                     in1=gtB[:, half * 512:(half + 1) * 512], op=ALU.mult), reads=[py[half], gtB], writes=[tm])
                S.op("pool", lambda e: e.tensor_tensor(out=xt[:], in0=tm[:], in1=xt[:], op=ALU.add), reads=[tm, xt], writes=[xt])
                S.dma("sp", "st" + xt.name, lambda e: e.dma_start(out=xs[t_ * 128:(t_ + 1) * 128, :], in_=xt[:]), reads=[xt], writes=[dxs])
                norm_tile(S, xt, A2, B2, junk, s_, tm)
                S.op("pool", lambda e: e.tensor_tensor(out=hf[:], in0=tm[:], in1=B2[:], op=ALU.add), reads=[tm, B2], writes=[hf])
                return hf

            def stageB(tt, hf, hs=hs, cs=cs):
                hTf = h2Tf.next(); r = rt.next()
                w2 = r[:, 3, :]
                pT2 = [pz.next(), pz.next()]
                for k in range(8):
                    S.op("pe", lambda e, k=k: e.matmul(pT2[k // 4][:, (k % 4) * 128:(k % 4 + 1) * 128], lhsT=hf[:, k * 128:(k + 1) * 128], rhs=ident[:],
                                                       start=True, stop=True),
                         reads=[hf, ident], writes=[pT2[k // 4]])
                for hh in range(2):
                    S.op("dve", lambda e, hh=hh: e.tensor_copy(out=hTf[:, hh * 4:(hh + 1) * 4, :], in_=pT2[hh][:].rearrange("p (k t) -> p k t", k=4)),
                         reads=[pT2[hh]], writes=[hTf])
                    S.op("act", lambda e, hh=hh: e.activation(out=hs[:, hh * 4:(hh + 1) * 4, tt * 128:(tt + 1) * 128],
                                                              in_=hTf[:, hh * 4:(hh + 1) * 4, :], func=AF.Copy),
                         reads=[hTf], writes=[hs])
                pl = pz.next()
                for k in range(8):
                    S.op("pe", lambda e, k=k: e.matmul(pl[:, 0:16], lhsT=hTf[:, k, :], rhs=wrt[:, k, :], start=(k == 0), stop=(k == 7)),
                         reads=[hTf, wrt], writes=[pl])
                sc = r[:, 0, :]; sel = r[:, 1, :]; w1 = r[:, 2, :]; w2 = r[:, 3, :]; w3 = r[:, 4, :]; msk = r[:, 5, :]; w4 = r[:, 6, :]; sm = r[:, 7, :]

                def V(fn, rd=()):
                    S.op("dve", fn, reads=[r] + list(rd), writes=[r])
                g4 = lambda ap: ap.rearrange("p (g i) -> p g i", g=4)
                S.op("act", lambda e: e.activation(out=sc, in_=pl[:, 0:16], func=AF.Sigmoid), reads=[pl], writes=[r])
                V(lambda e: e.tensor_tensor(out=sel, in0=sc, in1=rbias[:], op=ALU.add), [rbias])
                V(lambda e: e.tensor_reduce(out=sm[:, 0:4], in_=g4(sel), axis=AX.X, op=ALU.max))
                V(lambda e: e.tensor_tensor(out=g4(w1), in0=g4(sel), in1=sm[:, 0:4].unsqueeze(2).to_broadcast([128, 4, 4]), op=ALU.is_equal))
                V(lambda e: e.scalar_tensor_tensor(out=w2, in0=w1, scalar=-BIG, in1=sel, op0=ALU.mult, op1=ALU.add))
                V(lambda e: e.tensor_reduce(out=sm[:, 4:8], in_=g4(w2), axis=AX.X, op=ALU.max))
                V(lambda e: e.tensor_tensor(out=sm[:, 8:12], in0=sm[:, 0:4], in1=sm[:, 4:8], op=ALU.add))
                V(lambda e: e.tensor_reduce(out=sm[:, 12:13], in_=sm[:, 8:12], axis=AX.X, op=ALU.max))
                V(lambda e: e.tensor_tensor(out=sm[:, 4:8], in0=sm[:, 8:12], in1=sm[:, 12:13].to_broadcast([128, 4]), op=ALU.is_equal))
                V(lambda e: e.tensor_scalar(out=g4(w1), in0=sm[:, 4:8].unsqueeze(2).to_broadcast([128, 4, 4]), scalar1=-1.0, scalar2=BIG,
                                            op0=ALU.add, op1=ALU.mult))
                V(lambda e: e.tensor_tensor(out=w2, in0=w1, in1=sel, op=ALU.add))
                V(lambda e: e.tensor_reduce(out=sm[:, 13:14], in_=w2, axis=AX.X, op=ALU.max))
                V(lambda e: e.tensor_tensor(out=w3, in0=w2, in1=sm[:, 13:14].to_broadcast([128, 16]), op=ALU.is_equal))
                V(lambda e: e.scalar_tensor_tensor(out=w4, in0=w3, scalar=-BIG, in1=w2, op0=ALU.mult, op1=ALU.add))
                V(lambda e: e.tensor_reduce(out=sm[:, 14:15], in_=w4, axis=AX.X, op=ALU.max))
                V(lambda e: e.tensor_tensor(out=msk, in0=w4, in1=sm[:, 14:15].to_broadcast([128, 16]), op=ALU.is_equal))
                V(lambda e: e.tensor_tensor(out=msk, in0=msk, in1=w3, op=ALU.add))
                V(lambda e: e.tensor_tensor(out=w1, in0=sc, in1=msk, op=ALU.mult))
                V(lambda e: e.tensor_reduce(out=sm[:, 15:16], in_=w1, axis=AX.X, op=ALU.add))
                V(lambda e: e.reciprocal(out=sm[:, 15:16], in_=sm[:, 15:16]))
                V(lambda e: e.tensor_scalar(out=w2, in0=w1, scalar1=sm[:, 15:16], scalar2=None, op0=ALU.mult))

                def b2():
                    pc = pg.next()
                    S.op("pe", lambda e: e.matmul(pc[0:16, 0:128], lhsT=w2, rhs=ident[:], start=True, stop=True), reads=[r, ident], writes=[pc])
                    S.op("act", lambda e: e.activation(out=cs[:, tt * 128:(tt + 1) * 128], in_=pc[0:16, 0:128], func=AF.Copy), reads=[pc], writes=[cs])
                return b2

            if tb + 1 < NB:
                load_yh(tb + 1)
            hf0 = stageA(0)
            hf1 = stageA(1)
            b20 = stageB(0, hf0)
            hf2 = stageA(2)
            b21 = stageB(1, hf1)
            b20()
            hf3 = stageA(3)
            b22 = stageB(2, hf2)
            b21()

            def fin3(hf3=hf3, b22=b22, hs=hs, cs=cs, tb=tb, stageB=stageB):
                b23 = stageB(3, hf3)
                b22()
                return b23

            def fin4(hs=hs, cs=cs, tb=tb):
                S.dma("sp", "st" + hs.name, lambda e: e.dma_start(out=h2T_d[:, :, tb * 512:(tb + 1) * 512].rearrange("k p t -> p k t"), in_=hs[:]),
                      reads=[hs], writes=[dh2])
                S.dma("sp", "st" + cs.name, lambda e: e.dma_start(out=combT_d[:, tb * 512:(tb + 1) * 512], in_=cs[:]), reads=[cs], writes=[dcb])
            box = {}
            tail.append(lambda fin3=fin3, box=box: box.__setitem__("b23", fin3()))
            tail.append(lambda box=box, fin4=fin4: (box["b23"](), fin4()))
        while tail:
            tail.pop(0)()
        P.close()

    def phase_moe1(l):
        P = Phase(nc); S = P.S
        h2T = [P.sb([128, 8, 512], BF16, "h2T%d" % b) for b in range(NB)]
        for b in range(NB):
            S.dma("sp", "h2T%d" % b, lambda e, b=b: e.dma_start(out=h2T[b][:], in_=h2T_d[:, :, b * 512:(b + 1) * 512].rearrange("k p t -> p k t")),
                  writes=[h2T[b]])
        combT = P.sb([16, T], F32, "combT")
        S.dma("sp", "cT", lambda e: e.dma_start(out=combT[:], in_=combT_d), writes=[combT])
        sel = P.sb([16, 16 * 128], F32, "sel")
        S.dma("sp", "sel", lambda e: e.dma_start(out=sel[:], in_=C_sel), writes=[sel])
        wgr = P.ring(2, [128, 8, 512], BF16, "wg"); wur = P.ring(2, [128, 8, 512], BF16, "wu")
        pg = P.ring(3, [128, 512], F32, "pg", psum=True); pu = P.ring(3, [128, 512], F32, "pu", psum=True)
        pc = P.ring(2, [128, 512], F32, "pc", psum=True)
        cbr = P.ring(2, [128, 512], F32, "cb"); sgr = P.ring(3, [128, 512], F32, "sg"); tr = P.ring(3, [128, 512], F32, "t")
        her = P.ring(2, [128, 4, 512], BF16, "he")
        dhe = Buf(HE_d, "HE_d")
        for ex in range(16):
            wg_ = wgr.next(); wu_ = wur.next()
            S.dma("pool", wg_.name, lambda e, wg_=wg_, ex=ex: e.dma_start(out=wg_[:], in_=I["w_exp_gate"][l, ex].rearrange("(k p) n -> p k n", p=128)), writes=[wg_])
            S.dma("pool", wu_.name, lambda e, wu_=wu_, ex=ex: e.dma_start(out=wu_[:], in_=I["w_exp_up"][l, ex].rearrange("(k p) n -> p k n", p=128)), writes=[wu_])
            for tb in range(NB):
                p_c = pc.next(); cb = cbr.next(); he = her.next()
                S.op("pe", lambda e, p_c=p_c, ex=ex, tb=tb: e.matmul(p_c[:], lhsT=sel[:, ex * 128:(ex + 1) * 128], rhs=combT[:, tb * 512:(tb + 1) * 512],
                                                                      start=True, stop=True), reads=[sel, combT], writes=[p_c])
                S.op("act", lambda e, p_c=p_c, cb=cb: e.activation(out=cb[:], in_=p_c[:], func=AF.Copy), reads=[p_c], writes=[cb])
                for fc in range(4):
                    g = pg.next(); u = pu.next(); sg = sgr.next(); t = tr.next()
                    for k in range(8):
                        S.op("pe", lambda e, g=g, k=k, fc=fc, tb=tb, wg_=wg_: e.matmul(g[:], lhsT=wg_[:, k, fc * 128:(fc + 1) * 128], rhs=h2T[tb][:, k, :],
                                                                                     start=(k == 0), stop=(k == 7)), reads=[wg_, h2T[tb]], writes=[g])
                    for k in range(8):
                        S.op("pe", lambda e, u=u, k=k, fc=fc, tb=tb, wu_=wu_: e.matmul(u[:], lhsT=wu_[:, k, fc * 128:(fc + 1) * 128], rhs=h2T[tb][:, k, :],
                                                                                     start=(k == 0), stop=(k == 7)), reads=[wu_, h2T[tb]], writes=[u])
                    S.op("act", lambda e, g=g, sg=sg: e.activation(out=sg[:], in_=g[:], func=AF.Silu), reads=[g], writes=[sg])
                    S.op("dve", lambda e, u=u, sg=sg, t=t: e.tensor_tensor(out=t[:], in0=u[:], in1=sg[:], op=ALU.mult), reads=[u, sg], writes=[t])
                    S.op("dve", lambda e, t=t, cb=cb, he=he, fc=fc: e.tensor_tensor(out=he[:, fc, :], in0=t[:], in1=cb[:], op=ALU.mult), reads=[t, cb], writes=[he])
                for a4 in range(4):
                    S.dma("sp", "st" + he.name, lambda e, he=he, ex=ex, tb=tb, a4=a4: e.dma_start(
                        out=HE_d[tb * 4 + a4, :, ex * 4:(ex + 1) * 4, :],
                        in_=he[:, :, a4 * 128:(a4 + 1) * 128]), reads=[he], writes=[dhe])
        P.close()

    def phase_moe2(l):
        P = Phase(nc); S = P.S
        last = (l == L - 1)
        ident = P.sb([128, 128], BF16, "ident")
        S.dma("sp", "id", lambda e: e.dma_start(out=ident[:], in_=C_ident), writes=[ident])
        wd = [P.sb([128, 4, D], BF16, "wd%d" % i) for i in range(16)]
        for i in range(16):
            S.dma("pool", "wd%d" % i, lambda e, i=i: e.dma_start(out=wd[i][:],
                                                                 in_=I["w_exp_down"][l][i * 512:(i + 1) * 512, :].rearrange("(k p) n -> p k n", p=128)), writes=[wd[i]])
        gtB = P.sb([128, D], F32, "gtB")
        S.dma("sp", "gtB", lambda e: e.dma_start(out=gtB[:], in_=modB_d[l][:, 5120:6144]), writes=[gtB])
        AB = P.sb([128, D], F32, "AB")
        if last:
            S.dma("sp", "AB", lambda e: e.dma_start(out=AB[:], in_=I["g_final"][0, :].partition_broadcast(128)), writes=[AB])
            BB = None
        else:
            BB = P.sb([128, D], F32, "BB")
            S.dma("sp", "AB", lambda e: e.dma_start(out=AB[:], in_=modB_d[l + 1][:, 1024:2048]), writes=[AB])
            S.dma("sp", "BB", lambda e: e.dma_start(out=BB[:], in_=modB_d[l + 1][:, 0:1024]), writes=[BB])
        her = P.ring(2, [128, 64, 128], BF16, "he")
        po = [P.ps([128, 512], F32, "po") for _ in range(4)]
        pT = P.ring(2, [128, 1024], BF16, "pT", psum=True)
        xr = P.ring(2, [128, D], F32, "x"); tmp = P.ring(2, [128, D], F32, "tmp")
        junk = P.sb([128, D], BF16, "junk"); st = P.ring(2, [128, 2], F32, "st")
        hbr = P.ring(2, [128, D], BF16, "hb")
        hst = P.ring(1, [128, 8, 512], BF16, "hst")
        dxs = Buf(xs, "xs"); dh = Buf(hT_d, "hT_d")
        hcur = {"hs": None}

        loaded = {}

        def loads(t_):
            he = her.next(); xt = xr.next()
            S.dma("sp", he.name, lambda e: e.dma_start(out=he[:], in_=HE_d[t_]), writes=[he])
            S.dma("sp", xt.name, lambda e: e.dma_start(out=xt[:], in_=xs[t_ * 128:(t_ + 1) * 128, :]), writes=[xt])
            loaded[t_] = (he, xt)

        def stageA(t_):
            if t_ not in loaded:
                loads(t_)
            he, xt = loaded.pop(t_)
            if t_ + 1 < NT:
                loads(t_ + 1)
            tm = tmp.next(); s_ = st.next()
            for half in range(2):
                pp_ = po[(t_ % 2) * 2 + half]
                for c in range(64):
                    S.op("pe", lambda e, pp_=pp_, c=c, half=half: e.matmul(pp_[:], lhsT=he[:, c, :], rhs=wd[c // 4][:, c % 4, half * 512:(half + 1) * 512],
                                                                           start=(c == 0), stop=(c == 63)), reads=[he, wd[c // 4]], writes=[pp_])
                S.op("dve", lambda e, pp_=pp_, half=half: e.tensor_tensor(out=tm[:, half * 512:(half + 1) * 512], in0=pp_[:],
                                                                          in1=gtB[:, half * 512:(half + 1) * 512], op=ALU.mult), reads=[pp_, gtB], writes=[tm])
            S.op("pool", lambda e: e.tensor_tensor(out=xt[:], in0=tm[:], in1=xt[:], op=ALU.add), reads=[tm, xt], writes=[xt])
            if not last:
                S.dma("sp", "st" + xt.name, lambda e: e.dma_start(out=xs[t_ * 128:(t_ + 1) * 128, :], in_=xt[:]), reads=[xt], writes=[dxs])
            norm_tile(S, xt, AB, BB, junk, s_, tm)
            if last:
                S.dma("sp", "st" + tm.name, lambda e: e.dma_start(out=out_d[t_ * 128:(t_ + 1) * 128, :], in_=tm[:]), reads=[tm])
                return None
            hb = hbr.next()
            S.op("pool", lambda e: e.tensor_tensor(out=hb[:], in0=tm[:], in1=BB[:], op=ALU.add), reads=[tm, BB], writes=[hb])
            return hb

        def stageB(t_, hb):
            if hb is None:
                return
            if t_ % 4 == 0:
                hcur["hs"] = hst.next()
            hs = hcur["hs"]
            p = pT.next()
            transpose_store(S, P, hb, p, hs, (t_ % 4) * 128, ident)
            if t_ % 4 == 3:
                tb = t_ // 4
                S.dma("sp", "st" + hs.name, lambda e: e.dma_start(
                    out=hT_d[:, :, tb * 512:(tb + 1) * 512].rearrange("k p t -> p k t"), in_=hs[:]), reads=[hs], writes=[dh])

        prev = None
        for t_ in range(NT):
            hb = stageA(t_)
            if prev is not None:
                stageB(*prev)
            prev = (t_, hb)
        stageB(*prev)
        P.close()

    plist = []
    for l in range(L):
        plist.append(lambda l=l: phase_mod(l))
        plist.append(lambda l=l: phase_lam(l))
    plist.append(lambda: phase_n1(0, I["x"]))
    for l in range(L):
        plist.append(lambda l=l: phase_prj(l))
        for kind in ("A", "B", "C"):
            plist.append(lambda l=l, kind=kind: phase_att(l, kind))
        plist.append(lambda l=l: phase_mrg(l, I["x"] if l == 0 else xs))
        plist.append(lambda l=l: phase_moe1(l))
        plist.append(lambda l=l: phase_moe2(l))
    for i, f in enumerate(plist):
        if stop is not None and i >= stop:
            break
        f()
    return nc


def _consts():
    bf = ml_dtypes.bfloat16
    ident = np.eye(128, dtype=np.float32)
    anti = ident[::-1].copy()
    sel = np.zeros((16, 16, 128), np.float32)
    for e in range(16):
        sel[e, e, :] = 1.0
    pos = np.arange(T, dtype=np.float32)
    inv_freq = (1.0 / (10000.0 ** (np.arange(0, 32, 2, dtype=np.float32) / 32.0))).astype(np.float32)
    ang = (pos[:, None] * inv_freq[None, :]).astype(np.float32)
    cos = np.cos(ang.astype(np.float64)).astype(np.float32); sin = np.sin(ang.astype(np.float64)).astype(np.float32)
    cos32 = np.concatenate([cos, cos], axis=1).T
    sin32 = np.concatenate([-sin, sin], axis=1).T
    cos128 = np.tile(cos32, (4, 1)).astype(np.float32)
    sin128 = np.tile(sin32, (4, 1)).astype(np.float32)
    slopes = np.exp2(-8.0 / 4 * np.arange(1, 5, dtype=np.float64))
    ipos = np.arange(T)
    a_hi = (ipos // 64) * 64.0
    a_lo = (ipos % 64) * 1.0
    augq = np.zeros((4, 4, T), np.float64); augk = np.zeros((4, 4, T), np.float64)
    for h in range(4):
        s8 = slopes[h] * 8.0
        augq[h, 0] = -s8 * a_hi; augq[h, 1] = -s8 * a_lo; augq[h, 2] = 1.0; augq[h, 3] = 1.0
        augk[h, 0] = 1.0; augk[h, 1] = 1.0; augk[h, 2] = s8 * a_hi; augk[h, 3] = s8 * a_lo
    kk = np.arange(64)[:, None]; qq = np.arange(64)[None, :]
    corr = np.zeros((4, 128, 64), np.float64)
    for h in range(4):
        c = np.exp(-2.0 * slopes[h] * np.maximum(kk - qq, 0))
        corr[h, 0:64] = c; corr[h, 64:128] = c
    return {
        "k_ident": ident.astype(bf), "k_identf": ident, "k_anti": anti.astype(bf),
        "k_sel": sel.reshape(16, 16 * 128), "k_cos": np.ascontiguousarray(cos128), "k_sin": np.ascontiguousarray(sin128),
        "k_augq": augq.astype(np.float32).astype(bf), "k_augk": augk.astype(np.float32).astype(bf),
        "k_corr": corr.astype(np.float32),
    }


_NC_CACHE = {}


def kernel(**inputs):
    if "nc" not in _NC_CACHE:
        _NC_CACHE["nc"] = build_program()
    nc = _NC_CACHE["nc"]
    consts = _consts()
    shared = {}
    for k, v in inputs.items():
        if k in ("x", "c"):
            continue
        a = np.ascontiguousarray(np.asarray(v, dtype=np.float32))
        if k == "router_bias":
            a = a.reshape(1, 16)
        elif k == "g_final":
            a = a.reshape(1, D)
        elif k == "w_exp_down":
            a = a.reshape(L, 16 * 512, D)
        shared[k] = a
    shared.update(consts)
    x = np.asarray(inputs["x"], dtype=np.float32)
    c = np.asarray(inputs["c"], dtype=np.float32)
    real_cores = [0, 1, 4, 5]
    idle = {k: np.zeros_like(v) for k, v in shared.items() if not k.startswith("k_")}
    idle.update(consts)
    idle["x"] = np.zeros((T, D), np.float32)
    idle["c"] = np.zeros((128, 8), np.float32)
    in_maps = []
    for core in range(8):
        if core in real_cores:
            b = real_cores.index(core)
            m = dict(shared)
            m["x"] = np.ascontiguousarray(x[b])
            m["c"] = np.ascontiguousarray(c[b].reshape(8, 128).T)
        else:
            m = idle
        in_maps.append(m)
    res = run_bass_kernel_spmd(nc, in_maps, core_ids=list(range(8)))
    out = np.stack([np.asarray(res.results[cid]["out"], dtype=np.float32) for cid in real_cores], axis=0)
    return out
```
